# Optimizing a Trainium2 kernel written in Bass

```python
import math
import jax, jax.numpy as jnp
from jax import lax
import numpy as np

D_MODEL = 2048
BATCH = 4
SEQ = 4096
DEPTH = 4

HEAD_DIM = 64
N_MIXERS = 4
HEADS_PER_MIXER = 8
GROUP_WIDTH = HEADS_PER_MIXER * HEAD_DIM
MIX_WIDTH = N_MIXERS * GROUP_WIDTH
N_HEADS_TOTAL = N_MIXERS * HEADS_PER_MIXER
SCALE = HEAD_DIM ** -0.5

REL_BUCKETS = 32
REL_MAX_DIST = 2048

DILATED_CONFIGS = ((128, 1), (512, 4), (2048, 16))

SWA_WINDOW = 128
SWA_KV_HEADS = 2

MOBA_BLOCK = 256
MOBA_TOPK = 3

NSA_KV_HEADS = 2
NSA_CMP_BLOCK = 32
NSA_CMP_STRIDE = 16
NSA_CMP_HIDDEN = 128
NSA_SLC_BLOCK = 64
NSA_SLC_TOPN = 16
NSA_WINDOW = 512

BAND_BLOCK = 128
QUERY_CHUNK = 64

N_EXPERTS = 16
N_EXPERT_GROUPS = 4
TOPK_GROUPS = 1
TOPK_EXPERTS = 2
D_EXPERT = 512

NORM_EPS = 1e-6
NEG_INF = -1e30

KV_B = SWA_KV_HEADS * HEAD_DIM
KV_D = NSA_KV_HEADS * HEAD_DIM
IN_SIZES = (GROUP_WIDTH, GROUP_WIDTH, GROUP_WIDTH,
            GROUP_WIDTH, KV_B, KV_B,
            GROUP_WIDTH, GROUP_WIDTH, GROUP_WIDTH,
            GROUP_WIDTH, KV_D, KV_D, KV_D, KV_D, KV_D, KV_D, HEADS_PER_MIXER * 3)
IN_COLS = sum(IN_SIZES)

kernel_name = "hybrid_parallel_heads_moe"


def rmsnorm(x, g):
    xf = x.astype(jnp.float32)
    ms = jnp.mean(xf * xf, axis=-1, keepdims=True)
    return (xf * lax.rsqrt(ms + NORM_EPS)).astype(x.dtype) * g


def t5_bucket(dist):
    dist = jnp.maximum(dist, 0)
    max_exact = REL_BUCKETS // 2
    large = max_exact + (jnp.log(jnp.maximum(dist, 1).astype(jnp.float32) / max_exact)
                         / math.log(REL_MAX_DIST / max_exact) * (REL_BUCKETS - max_exact)).astype(jnp.int32)
    large = jnp.minimum(large, REL_BUCKETS - 1)
    return jnp.where(dist < max_exact, dist, large)


def masked_softmax(s, mask, sink=None):
    s = jnp.where(mask, s, NEG_INF)
    m = jnp.max(s, axis=-1, keepdims=True)
    if sink is not None:
        m = jnp.maximum(m, sink)
    e = jnp.where(mask, jnp.exp(s - m), 0.0)
    den = jnp.sum(e, axis=-1, keepdims=True)
    if sink is not None:
        den = den + jnp.exp(sink - m)
    den = jnp.maximum(den, jnp.finfo(jnp.float32).tiny)
    return e / den, (m + jnp.log(den))[..., 0]


def banded_attention(q, k, v, max_dist, tab, dist_scale, sink=None):
    b, hk, g, L, hd = q.shape
    nblk = -(-L // BAND_BLOCK)
    Lp = nblk * BAND_BLOCK
    nprev = -(-max_dist // BAND_BLOCK)
    nk = (nprev + 1) * BAND_BLOCK
    q = jnp.pad(q, ((0, 0), (0, 0), (0, 0), (0, Lp - L), (0, 0)))
    pad_kv = ((0, 0), (0, 0), (nprev * BAND_BLOCK, Lp - L), (0, 0))
    k = jnp.pad(k, pad_kv)
    v = jnp.pad(v, pad_kv)
    kidx = np.arange(nblk)[:, None] * BAND_BLOCK + np.arange(nk)[None, :]
    kb = k[:, :, kidx]
    vb = v[:, :, kidx]
    qb = q.reshape(b, hk, g, nblk, BAND_BLOCK, hd)
    rel = np.arange(BAND_BLOCK)[:, None] + nprev * BAND_BLOCK - np.arange(nk)[None, :]
    kpos = np.arange(nblk)[:, None, None] * BAND_BLOCK - nprev * BAND_BLOCK + np.arange(nk)[None, None, :]
    mask = (rel >= 0) & (rel <= max_dist) & (kpos >= 0)
    bias = tab.astype(jnp.float32)[:, :, t5_bucket(jnp.asarray(rel * dist_scale))]
    s = jnp.einsum('bhgnqd,bhnkd->bhgnqk', qb, kb).astype(jnp.float32) * SCALE + bias[None, :, :, None]
    if sink is not None:
        sink = sink.astype(jnp.float32)[None, :, :, None, None, None]
    p, lse = masked_softmax(s, mask, sink)
    o = jnp.einsum('bhgnqk,bhnkd->bhgnqd', p.astype(v.dtype), vb).reshape(b, hk, g, Lp, hd)[:, :, :, :L]
    return o, lse.reshape(b, hk, g, Lp)[:, :, :, :L]


def dilated_attention(q, k, v, tab):
    b, h, s, hd = q.shape
    outs, lses = [], []
    for window, r in DILATED_CONFIGS:
        def strided(t):
            return t.reshape(b, h, s // r, r, hd).transpose(0, 3, 1, 2, 4).reshape(b * r, h, s // r, hd)
        o, lse = banded_attention(strided(q)[:, :, None], strided(k), strided(v), window // r, tab[:, None], r)
        outs.append(o[:, :, 0].reshape(b, r, h, s // r, hd).transpose(0, 2, 3, 1, 4).reshape(b, h, s, hd))
        lses.append(lse[:, :, 0].reshape(b, r, h, s // r).transpose(0, 2, 3, 1).reshape(b, h, s))
    w = jax.nn.softmax(jnp.stack(lses, axis=0), axis=0)
    o = w[0][..., None] * outs[0] + w[1][..., None] * outs[1] + w[2][..., None] * outs[2]
    return o.astype(q.dtype)


def moba_attention(q, k, v, tab):
    b, h, s, hd = q.shape
    nb = -(-s // MOBA_BLOCK)
    sp = nb * MOBA_BLOCK
    pad = ((0, 0), (0, 0), (0, sp - s), (0, 0))
    q, k, v = jnp.pad(q, pad), jnp.pad(k, pad), jnp.pad(v, pad)
    kb = k.reshape(b, h, nb, MOBA_BLOCK, hd)
    vb = v.reshape(b, h, nb, MOBA_BLOCK, hd)
    kmean = jnp.mean(kb, axis=-2)
    nch = sp // QUERY_CHUNK
    qc = jnp.moveaxis(q.reshape(b, h, nch, QUERY_CHUNK, hd), 2, 0)
    starts = jnp.arange(nch, dtype=jnp.int32) * QUERY_CHUNK
    ksel = min(MOBA_TOPK, nb)
    bi = jnp.arange(b)[:, None, None, None]
    hi = jnp.arange(h)[None, :, None, None]
    tabf = tab.astype(jnp.float32)

    def chunk(args):
        qi, start = args
        pos = start + jnp.arange(QUERY_CHUNK, dtype=jnp.int32)
        cur = start // MOBA_BLOCK
        gate = jnp.einsum('bhqd,bhnd->bhqn', qi, kmean).astype(jnp.float32)
        gate = jnp.where(jnp.arange(nb) < cur, gate, NEG_INF)
        _, sel = lax.top_k(gate, ksel)
        ks = kb[bi, hi, sel]
        vs = vb[bi, hi, sel]
        s_sel = jnp.einsum('bhqd,bhqnkd->bhqnk', qi, ks).reshape(b, h, QUERY_CHUNK, ksel * MOBA_BLOCK)
        kpos_sel = (sel[..., None] * MOBA_BLOCK + jnp.arange(MOBA_BLOCK)).reshape(b, h, QUERY_CHUNK, ksel * MOBA_BLOCK)
        mask_sel = jnp.repeat(sel < cur, MOBA_BLOCK, axis=-1)
        ko = lax.dynamic_index_in_dim(kb, cur, axis=2, keepdims=False)
        vo = lax.dynamic_index_in_dim(vb, cur, axis=2, keepdims=False)
        s_own = jnp.einsum('bhqd,bhkd->bhqk', qi, ko)
        kpos_own = cur * MOBA_BLOCK + jnp.arange(MOBA_BLOCK, dtype=jnp.int32)
        shp = (b, h, QUERY_CHUNK, MOBA_BLOCK)
        mask_own = jnp.broadcast_to(kpos_own[None, :] <= pos[:, None], shp)
        kpos = jnp.concatenate([kpos_sel, jnp.broadcast_to(kpos_own, shp)], axis=-1)
        mask = jnp.concatenate([mask_sel, mask_own], axis=-1)
        bias = tabf[hi, t5_bucket(pos[:, None] - kpos)]
        sc = jnp.concatenate([s_sel, s_own], axis=-1).astype(jnp.float32) * SCALE + bias
        p, _ = masked_softmax(sc, mask)
        p = p.astype(v.dtype)
        o = (jnp.einsum('bhqnk,bhqnkd->bhqd', p[..., :ksel * MOBA_BLOCK].reshape(b, h, QUERY_CHUNK, ksel, MOBA_BLOCK), vs)
             + jnp.einsum('bhqk,bhkd->bhqd', p[..., ksel * MOBA_BLOCK:], vo))
        return o

    o = lax.map(chunk, (qc, starts))
    return jnp.moveaxis(o, 0, 2).reshape(b, h, sp, hd)[:, :, :s]


def nsa_attention(q, k_cmp, v_cmp, k_slc, v_slc, k_win, v_win, gates, pe_k, pe_v, w1k, w2k, w1v, w2v, tab):
    b, hk, g, s, hd = q.shape
    n_cmp = (s - NSA_CMP_BLOCK) // NSA_CMP_STRIDE + 1
    cidx = np.arange(n_cmp)[:, None] * NSA_CMP_STRIDE + np.arange(NSA_CMP_BLOCK)[None, :]

    def compress(t, pe, w1, w2):
        blk = (t[:, :, cidx] + pe).reshape(b, hk, n_cmp, NSA_CMP_BLOCK * hd)
        return jax.nn.gelu(blk @ w1) @ w2

    kc = compress(k_cmp, pe_k, w1k, w2k)
    vc = compress(v_cmp, pe_v, w1v, w2v)
    pos = np.arange(s)
    mask_c = (np.arange(n_cmp) * NSA_CMP_STRIDE + NSA_CMP_BLOCK - 1)[None, :] <= pos[:, None]
    s_c = jnp.einsum('bhgqd,bhnd->bhgqn', q, kc).astype(jnp.float32) * SCALE
    p_c, _ = masked_softmax(s_c, mask_c)
    o_cmp = jnp.einsum('bhgqn,bhnd->bhgqd', p_c.astype(vc.dtype), vc)
    n_slc = s // NSA_SLC_BLOCK
    c_start = np.arange(n_cmp) * NSA_CMP_STRIDE
    j_start = np.arange(n_slc) * NSA_SLC_BLOCK
    overlap = ((c_start[:, None] < j_start[None, :] + NSA_SLC_BLOCK)
               & (c_start[:, None] + NSA_CMP_BLOCK > j_start[None, :])).astype(np.float32)
    imp = jnp.einsum('bhgqn,nj->bhqj', p_c, jnp.asarray(overlap))
    cur = pos // NSA_SLC_BLOCK
    jj = np.arange(n_slc)[None, :]
    forced = (jj == 0) | (jj == cur[:, None]) | (jj == cur[:, None] - 1)
    imp = jnp.where(forced, -NEG_INF, jnp.where(jj <= cur[:, None], imp, NEG_INF))
    n_sel = min(NSA_SLC_TOPN, n_slc)
    _, sel = lax.top_k(imp, n_sel)
    ksb = k_slc.reshape(b, hk, n_slc, NSA_SLC_BLOCK, hd)
    vsb = v_slc.reshape(b, hk, n_slc, NSA_SLC_BLOCK, hd)
    nch = s // QUERY_CHUNK
    qc = jnp.moveaxis(q.reshape(b, hk, g, nch, QUERY_CHUNK, hd), 3, 0)
    selc = jnp.moveaxis(sel.reshape(b, hk, nch, QUERY_CHUNK, n_sel), 2, 0)
    starts = jnp.arange(nch, dtype=jnp.int32) * QUERY_CHUNK
    bi = jnp.arange(b)[:, None, None, None]
    hi = jnp.arange(hk)[None, :, None, None]
    tab_t = tab.astype(jnp.float32).transpose(0, 2, 1)

    def chunk(args):
        qi, si, start = args
        pos_q = start + jnp.arange(QUERY_CHUNK, dtype=jnp.int32)
        kk = ksb[bi, hi, si]
        vv = vsb[bi, hi, si]
        nkeys = n_sel * NSA_SLC_BLOCK
        sc = jnp.einsum('bhgqd,bhqnkd->bhgqnk', qi, kk).reshape(b, hk, g, QUERY_CHUNK, nkeys).astype(jnp.float32) * SCALE
        kpos = (si[..., None] * NSA_SLC_BLOCK + jnp.arange(NSA_SLC_BLOCK)).reshape(b, hk, QUERY_CHUNK, nkeys)
        mask = (kpos <= pos_q[:, None])[:, :, None]
        bias = jnp.moveaxis(tab_t[hi, t5_bucket(pos_q[:, None] - kpos)], -1, 2)
        p, _ = masked_softmax(sc + bias, mask)
        return jnp.einsum('bhgqnk,bhqnkd->bhgqd',
                          p.reshape(b, hk, g, QUERY_CHUNK, n_sel, NSA_SLC_BLOCK).astype(vv.dtype), vv)

    o_slc = jnp.moveaxis(lax.map(chunk, (qc, selc, starts)), 0, 3).reshape(b, hk, g, s, hd)
    o_win, _ = banded_attention(q, k_win, v_win, NSA_WINDOW - 1, tab, 1)
    gates = gates.astype(q.dtype)
    return gates[..., 0:1] * o_cmp + gates[..., 1:2] * o_slc + gates[..., 2:3] * o_win


def _heads(t, n):
    b, s, _ = t.shape
    return t.reshape(b, s, n, HEAD_DIM).transpose(0, 2, 1, 3)


def _seq(o):
    b, n, s, d = o.shape
    return o.transpose(0, 2, 1, 3).reshape(b, s, n * d)


def token_mixer(h, w_in_l, rel_bias, sink_l, pe_k, pe_v, w1k, w2k, w1v, w2v, mix_g, w_out_l):
    b, s, _ = h.shape
    H = HEADS_PER_MIXER
    proj = h @ w_in_l
    offs = [int(o) for o in np.cumsum(IN_SIZES)[:-1]]
    (q_a, k_a, v_a, q_b, k_b, v_b, q_c, k_c, v_c,
     q_d, k_dc, v_dc, k_ds, v_ds, k_dw, v_dw, g_d) = jnp.split(proj, offs, axis=-1)
    tabs = [rel_bias[:, i * H:(i + 1) * H].T for i in range(N_MIXERS)]
    o_a = dilated_attention(_heads(q_a, H), _heads(k_a, H), _heads(v_a, H), tabs[0])
    gb = H // SWA_KV_HEADS
    o_b, _ = banded_attention(_heads(q_b, H).reshape(b, SWA_KV_HEADS, gb, s, HEAD_DIM),
                              _heads(k_b, SWA_KV_HEADS), _heads(v_b, SWA_KV_HEADS),
                              SWA_WINDOW - 1, tabs[1].reshape(SWA_KV_HEADS, gb, REL_BUCKETS), 1,
                              sink_l.reshape(SWA_KV_HEADS, gb))
    o_b = o_b.reshape(b, H, s, HEAD_DIM)
    o_c = moba_attention(_heads(q_c, H), _heads(k_c, H), _heads(v_c, H), tabs[2])
    gd = H // NSA_KV_HEADS
    gates = jax.nn.sigmoid(g_d.reshape(b, s, H, 3)).transpose(0, 2, 1, 3).reshape(b, NSA_KV_HEADS, gd, s, 3)
    kvh = lambda t: _heads(t, NSA_KV_HEADS)
    o_d = nsa_attention(_heads(q_d, H).reshape(b, NSA_KV_HEADS, gd, s, HEAD_DIM),
                        kvh(k_dc), kvh(v_dc), kvh(k_ds), kvh(v_ds), kvh(k_dw), kvh(v_dw), gates,
                        pe_k, pe_v, w1k, w2k, w1v, w2v, tabs[3].reshape(NSA_KV_HEADS, gd, REL_BUCKETS))
    o_d = o_d.reshape(b, H, s, HEAD_DIM)
    outs = [rmsnorm(_seq(o), mix_g[i]) for i, o in enumerate((o_a, o_b, o_c, o_d))]
    return jnp.concatenate(outs, axis=-1) @ w_out_l


def moe_ffn(h, router_w, router_bias, w_gate, w_up, w_down):
    b, s, d = h.shape
    t = h.reshape(b * s, d)
    aff = jax.nn.sigmoid((t @ router_w).astype(jnp.float32))
    score = aff + router_bias.astype(jnp.float32)
    per_group = N_EXPERTS // N_EXPERT_GROUPS
    group_score = lax.top_k(score.reshape(-1, N_EXPERT_GROUPS, per_group), 2)[0].sum(-1)
    _, gsel = lax.top_k(group_score, TOPK_GROUPS)
    gmask = jnp.any(jnp.arange(N_EXPERT_GROUPS)[None, None, :] == gsel[..., None], axis=1)
    emask = jnp.repeat(gmask, per_group, axis=-1)
    _, esel = lax.top_k(jnp.where(emask, score, NEG_INF), TOPK_EXPERTS)
    w = jnp.take_along_axis(aff, esel, axis=-1)
    w = w / jnp.sum(w, axis=-1, keepdims=True)
    combine = jnp.sum((esel[..., None] == jnp.arange(N_EXPERTS)) * w[..., None], axis=1).astype(t.dtype)
    y = jnp.zeros_like(t)
    for e in range(N_EXPERTS):
        he = jax.nn.silu(t @ w_gate[e]) * (t @ w_up[e])
        y = y + combine[:, e:e + 1] * (he @ w_down[e])
    return y.reshape(b, s, d)


def setup_inputs(seed: int = 0) -> dict:
    key = jax.random.key(seed)
    ks = jax.random.split(key, 24)
    f32 = jnp.float32

    def nrm(k, shape, sc):
        return jax.random.normal(k, shape, f32) * sc

    D, E, F = D_MODEL, N_EXPERTS, D_EXPERT
    cin = NSA_CMP_BLOCK * HEAD_DIM
    return {
        "x": nrm(ks[0], (BATCH, SEQ, D), 1.0),
        "c": nrm(ks[1], (BATCH, D), 1.0),
        "rel_bias": nrm(ks[2], (REL_BUCKETS, N_HEADS_TOTAL), 0.5),
        "router_w": nrm(ks[3], (D, E), D ** -0.5),
        "router_bias": nrm(ks[4], (E,), 0.01),
        "norm1_g": 1.0 + nrm(ks[5], (DEPTH, D), 0.02),
        "norm2_g": 1.0 + nrm(ks[6], (DEPTH, D), 0.02),
        "ada_w": nrm(ks[7], (DEPTH, D, 6 * D), 0.3 * D ** -0.5),
        "ada_b": nrm(ks[8], (DEPTH, 6 * D), 0.02),
        "w_in": nrm(ks[9], (DEPTH, D, IN_COLS), D ** -0.5),
        "nsa_pe_k": nrm(ks[10], (DEPTH, NSA_CMP_BLOCK, HEAD_DIM), 0.5),
        "nsa_pe_v": nrm(ks[11], (DEPTH, NSA_CMP_BLOCK, HEAD_DIM), 0.5),
        "nsa_cmp_w1_k": nrm(ks[12], (DEPTH, cin, NSA_CMP_HIDDEN), cin ** -0.5),
        "nsa_cmp_w2_k": nrm(ks[13], (DEPTH, NSA_CMP_HIDDEN, HEAD_DIM), NSA_CMP_HIDDEN ** -0.5),
        "nsa_cmp_w1_v": nrm(ks[14], (DEPTH, cin, NSA_CMP_HIDDEN), cin ** -0.5),
        "nsa_cmp_w2_v": nrm(ks[15], (DEPTH, NSA_CMP_HIDDEN, HEAD_DIM), NSA_CMP_HIDDEN ** -0.5),
        "sinks": nrm(ks[16], (DEPTH, HEADS_PER_MIXER), 0.5),
        "mix_norm_g": 1.0 + nrm(ks[17], (DEPTH, N_MIXERS, GROUP_WIDTH), 0.02),
        "w_out": nrm(ks[18], (DEPTH, MIX_WIDTH, D), MIX_WIDTH ** -0.5),
        "exp_w_gate": nrm(ks[19], (DEPTH, E, D, F), D ** -0.5),
        "exp_w_up": nrm(ks[20], (DEPTH, E, D, F), D ** -0.5),
        "exp_w_down": nrm(ks[21], (DEPTH, E, F, D), F ** -0.5),
        "final_g": 1.0 + nrm(ks[22], (D,), 0.02),
    }


def reference(x, c, rel_bias, router_w, router_bias, norm1_g, norm2_g, ada_w, ada_b, w_in,
              nsa_pe_k, nsa_pe_v, nsa_cmp_w1_k, nsa_cmp_w2_k, nsa_cmp_w1_v, nsa_cmp_w2_v,
              sinks, mix_norm_g, w_out, exp_w_gate, exp_w_up, exp_w_down, final_g):
    c_act = jax.nn.silu(c)
    for l in range(DEPTH):
        mod = c_act @ ada_w[l] + ada_b[l]
        sh1, sc1, g1, sh2, sc2, g2 = jnp.split(mod, 6, axis=-1)
        h = rmsnorm(x, norm1_g[l]) * (1.0 + sc1[:, None]) + sh1[:, None]
        x = x + g1[:, None] * token_mixer(h, w_in[l], rel_bias, sinks[l], nsa_pe_k[l], nsa_pe_v[l],
                                          nsa_cmp_w1_k[l], nsa_cmp_w2_k[l], nsa_cmp_w1_v[l], nsa_cmp_w2_v[l],
                                          mix_norm_g[l], w_out[l])
        h = rmsnorm(x, norm2_g[l]) * (1.0 + sc2[:, None]) + sh2[:, None]
        x = x + g2[:, None] * moe_ffn(h, router_w, router_bias, exp_w_gate[l], exp_w_up[l], exp_w_down[l])
    return rmsnorm(x, final_g)
```

```python
import contextlib
import math
import numpy as np
import concourse.bass as bass
import concourse.mybir as mybir
from concourse.bass_utils import run_bass_kernel_spmd

F32 = mybir.dt.float32
BF16 = mybir.dt.bfloat16
AF = mybir.ActivationFunctionType
ALU = mybir.AluOpType
AX = mybir.AxisListType

D = 2048
KC = D // 128
HD = 64
NEG = -30000.0
IN_SIZES = (512, 512, 512, 512, 128, 128, 512, 512, 512, 512, 128, 128, 128, 128, 128, 128, 24)
IN_OFFS = [0] + list(np.cumsum(IN_SIZES))
(QA, KA, VA, QB, KB_, VB, QC, KC_, VC, QD, KDC, VDC, KDS, VDS, KDW, VDW, GD) = range(17)
FT_ORDER = [QA, KA, QB, KB_, QC, KC_, QD, KDC, VDC, KDS, KDW]
TM_ORDER = [VA, VB, VC, VDS, VDW, GD]
FT_COLS = sum(IN_SIZES[i] for i in FT_ORDER)
TM_COLS = sum(IN_SIZES[i] for i in TM_ORDER)
FT_OFF = {}
_o = 0
for _i in FT_ORDER:
    FT_OFF[_i] = _o
    _o += IN_SIZES[_i]
TM_OFF = {}
_o = 0
for _i in TM_ORDER:
    TM_OFF[_i] = _o
    _o += IN_SIZES[_i]


class Region:
    __slots__ = ("name", "last_w", "readers")

    def __init__(self, name):
        self.name = name
        self.last_w = None
        self.readers = {}


class Prog:
    ENG = ("pe", "act", "dve", "pool", "sp")
    NDMA = 88

    def __init__(self, nc):
        self.nc = nc
        self.ops = {e: [] for e in self.ENG}
        self.cnt = {e: 0 for e in self.ENG}
        self.seen = {e: {} for e in self.ENG}
        self.dma_cnt = [0] * self.NDMA
        self.dma_rr = 0
        self.stack = contextlib.ExitStack()
        self.nreg = 0
        self.out_events = []

    def sbt(self, name, shape, dtype):
        self.nuniq = getattr(self, "nuniq", 0) + 1
        return self.nc.sbuf_tensor(f"{name}_{self.nuniq}", list(shape), dtype)

    def region(self, name=None):
        self.nreg += 1
        return Region(name or f"r{self.nreg}")

    def sb(self, name, shape, dtype):
        t = self.stack.enter_context(self.nc.sbuf_tensor(name, list(shape), dtype))
        return t

    def ps(self, name, shape, dtype=F32):
        return self.stack.enter_context(self.nc.psum_tensor(name, list(shape), dtype))

    def dram(self, name, shape, dtype, kind="Internal"):
        return self.nc.dram_tensor(name, list(shape), dtype, kind=kind).ap()

    def _deps(self, reads, writes, pe_acc=False, eng=None):
        deps = {}

        def add(ev):
            if ev is None:
                return
            k, v = ev
            if deps.get(k, 0) < v:
                deps[k] = v
        for r in reads:
            add(r.last_w)
        for w in writes:
            if not (pe_acc and w.last_w is not None and w.last_w[0] == eng):
                add(w.last_w)
            for k, v in w.readers.items():
                add((k, v))
        return deps

    def _commit(self, ev, reads, writes):
        for r in reads:
            k, v = ev
            if r.readers.get(k, 0) < v:
                r.readers[k] = v
        for w in writes:
            w.last_w = ev
            w.readers = {}

    def _waits(self, eng, deps):
        waits = []
        for k, v in deps.items():
            if self.seen[eng].get(k, 0) < v:
                self.seen[eng][k] = v
                waits.append((k, v))
        return waits

    def op(self, eng, fn, reads=(), writes=(), pe_acc=False):
        deps = self._deps(reads, writes, pe_acc, eng)
        waits = self._waits(eng, deps)
        self.cnt[eng] += 1
        ev = (eng, self.cnt[eng])
        self.ops[eng].append((waits, fn, (eng, 1)))
        self._commit(ev, reads, writes)
        return ev

    def dma(self, q, out, in_, reads=(), writes=(), store=False, **kw):
        if store:
            deps = self._deps(reads, ())
            for w in writes:
                for k, v in w.readers.items():
                    if deps.get(k, 0) < v:
                        deps[k] = v
            reg = reads[0]
        else:
            deps = self._deps(reads, writes)
            reg = writes[0]
        waits = self._waits(q, deps)
        if not hasattr(self, "reg_sem"):
            self.reg_sem = {}
        if id(reg) not in self.reg_sem:
            self.reg_sem[id(reg)] = len(self.reg_sem) % self.NDMA
        j = self.reg_sem[id(reg)]
        self.dma_cnt[j] += 16
        ev = (("dma", j), self.dma_cnt[j])
        self.ops[q].append((waits, lambda e: e.dma_start(out=out, in_=in_, **kw), (("dma", j), 16)))
        self._commit(ev, reads, writes)
        return ev

    def barrier(self):
        evs = {}
        for e in self.ENG:
            if self.cnt[e]:
                evs[e] = self.cnt[e]
        for j in range(self.NDMA):
            if self.dma_cnt[j]:
                evs[("dma", j)] = self.dma_cnt[j]
        for e in self.ENG:
            waits = self._waits(e, {k: v for k, v in evs.items() if k != e})
            if waits:
                self.ops[e].append((waits, None, None))

    def finish(self):
        nc = self.nc
        sems = {}
        for e in self.ENG:
            sems[e] = self.stack.enter_context(nc.semaphore("sem_" + e))
        for j in range(self.NDMA):
            sems[("dma", j)] = self.stack.enter_context(nc.semaphore(f"sem_dma{j}"))
        final = []
        for e in self.ENG:
            if self.cnt[e] and e != "sp":
                final.append((e, self.cnt[e]))
        for j in range(self.NDMA):
            if self.dma_cnt[j]:
                final.append((("dma", j), self.dma_cnt[j]))
        ops = self.ops
        with nc.Block() as block:
            def run(engobj, name):
                for waits, fn, inc in ops[name]:
                    for k, v in waits:
                        engobj.wait_ge(sems[k], v)
                    if fn is None:
                        continue
                    ins = fn(engobj)
                    ins.then_inc(sems[inc[0]], inc[1])

            @block.tensor
            def _(t):
                run(t, "pe")

            @block.scalar
            def _(s):
                run(s, "act")

            @block.vector
            def _(v):
                run(v, "dve")

            @block.gpsimd
            def _(g):
                run(g, "pool")

            @block.sync
            def _(sp):
                run(sp, "sp")
                for k, v in final:
                    sp.wait_ge(sems[k], v)
        self.stack.close()


class Ctx:
    pass


def t5_bucket_np(dist):
    dist = np.maximum(dist, 0)
    max_exact = 16
    with np.errstate(divide="ignore"):
        large = max_exact + (np.log(np.maximum(dist, 1).astype(np.float32) / np.float32(max_exact))
                             / np.float32(math.log(2048 / max_exact)) * np.float32(32 - max_exact)).astype(np.int32)
    large = np.minimum(large, 31)
    return np.where(dist < max_exact, dist, large)


def setup_ctx(p, consts_ap):
    cx = Ctx()
    cx.banks = [p.ps(f"bank{i}", [128, 512], F32) for i in range(8)]
    cx.bank_r = [p.region(f"bank{i}") for i in range(8)]
    cx.ident_f = p.sb("ident_f", [128, 128], F32)
    cx.ident_b = p.sb("ident_b", [128, 128], BF16)
    cx.r_ident = p.region("ident")
    p.dma("sp", cx.ident_f[:], consts_ap[0:128, 0:128], writes=[cx.r_ident])
    p.op("dve", lambda e: e.tensor_copy(out=cx.ident_b[:], in_=cx.ident_f[:]), reads=[cx.r_ident], writes=[cx.r_ident])
    return cx


def stage_mod(p, cx, c_ap, ada_w, ada_b, modD, NL, NB):
    with contextlib.ExitStack() as st:
        def sb(name, shape, dt):
            return st.enter_context(p.sbt(name, list(shape), dt))
        cT = sb("m_cT", [128, KC, NB], F32)
        r_cT = p.region()
        for b in range(NB):
            p.dma("sp", cT[:, :, b], c_ap[b].rearrange("(k p) -> p k", p=128), writes=[r_cT], allow_slow_non_contiguous=True) \
                if False else p.dma("sp", cT[:, :, b:b + 1], c_ap[b:b + 1, :].rearrange("b (k p) -> p k b", p=128), writes=[r_cT],
                                    allow_slow_non_contiguous=True)
        p.op("act", lambda e: e.activation(out=cT[:], in_=cT[:], func=AF.Silu), reads=[r_cT], writes=[r_cT])
        wt = [sb(f"m_w{i}", [128, KC, 512], F32) for i in range(2)]
        r_w = [p.region() for _ in range(2)]
        bt = [sb(f"m_b{i}", [NB, 512], F32) for i in range(2)]
        r_b = [p.region() for _ in range(2)]
        ot = [sb(f"m_o{i}", [NB, 512], F32) for i in range(2)]
        r_o = [p.region() for _ in range(2)]
        it = 0
        for l in range(NL):
            for cc in range(6 * D // 512):
                i = it % 2
                it += 1
                p.dma("sp", wt[i][:], ada_w[l, :, cc * 512:(cc + 1) * 512].rearrange("(k p) c -> p k c", p=128), writes=[r_w[i]])
                for b in range(NB):
                    p.dma("sp", bt[i][b:b + 1, :], ada_b[l:l + 1, cc * 512:(cc + 1) * 512], writes=[r_b[i]])
                bk = cx.banks[i]
                for k in range(KC):
                    p.op("pe", lambda e, k=k, i=i, bk=bk: e.matmul(bk[0:NB, :], cT[:, k, :], wt[i][:, k, :], start=(k == 0), stop=(k == KC - 1)),
                         reads=[r_cT, r_w[i]], writes=[cx.bank_r[i]], pe_acc=(k > 0))
                p.op("dve", lambda e, i=i, bk=bk: e.tensor_tensor(out=ot[i][:], in0=bk[0:NB, :], in1=bt[i][:], op=ALU.add),
                     reads=[cx.bank_r[i], r_b[i]], writes=[r_o[i]])
                p.dma("sp", modD[l, :, cc * 512:(cc + 1) * 512], ot[i][:], reads=[r_o[i]], writes=[cx.r_modD], store=True)
        p.barrier()


def load_cols(p, sbt, r, src_row):
    p.dma("sp", sbt[:].rearrange("p (k o) -> p k o", o=1), src_row.rearrange("o (k p) -> p k o", p=128), writes=[r],
          allow_slow_non_contiguous=True)


def stage_norm_T(p, cx, x_src, tok0, ntok, g_row, sc_row, sh_row, hT, hcol0, tag, r_x, r_hT):
    with contextlib.ExitStack() as st:
        def sb(name, shape, dt):
            return st.enter_context(p.sbt(tag + name, list(shape), dt))
        gc = sb("gc", [128, KC], F32)
        sc = sb("sc", [128, KC], F32)
        sh = sb("sh", [128, KC], F32)
        r_c = p.region()
        load_cols(p, gc, r_c, g_row)
        load_cols(p, sc, r_c, sc_row)
        load_cols(p, sh, r_c, sh_row)
        p.op("dve", lambda e: e.scalar_tensor_tensor(out=sc[:], in0=sc[:], scalar=1.0, in1=gc[:], op0=ALU.add, op1=ALU.mult),
             reads=[r_c], writes=[r_c])
        xt = [sb(f"xt{i}", [128, D], F32) for i in range(2)]
        r_xt = [p.region() for _ in range(2)]
        junk = sb("junk", [128, D], BF16)
        r_junk = p.region()
        ss = [sb(f"ss{i}", [128, 1], F32) for i in range(2)]
        r_ss = [p.region() for _ in range(2)]
        xn = [sb(f"xn{i}", [128, D], F32) for i in range(4)]
        r_xn = [p.region() for _ in range(4)]
        hs = [sb(f"hs{i}", [128, KC, 512], BF16) for i in range(2)]
        r_hs = [p.region() for _ in range(2)]
        nblk = ntok // 512
        it = 0
        for blk in range(nblk):
            hb = blk % 2
            for tt in range(4):
                i = it % 2
                it += 1
                t0 = tok0 + blk * 512 + tt * 128
                p.dma("sp", xt[i][:], x_src[t0:t0 + 128, :], reads=[r_x], writes=[r_xt[i]])
                p.op("act", lambda e, i=i: e.activation(out=junk[:], in_=xt[i][:], func=AF.Square, accum_out=ss[i][:]),
                     reads=[r_xt[i]], writes=[r_junk, r_ss[i]])
                p.op("dve", lambda e, i=i: e.tensor_scalar(out=ss[i][:], in0=ss[i][:], scalar1=1.0 / D, scalar2=1e-6, op0=ALU.mult, op1=ALU.add),
                     reads=[r_ss[i]], writes=[r_ss[i]])
                p.op("act", lambda e, i=i: e.activation(out=ss[i][:], in_=ss[i][:], func=AF.Sqrt), reads=[r_ss[i]], writes=[r_ss[i]])
                p.op("dve", lambda e, i=i: e.reciprocal(out=ss[i][:], in_=ss[i][:]), reads=[r_ss[i]], writes=[r_ss[i]])
                p.op("dve", lambda e, i=i, tt=tt: e.tensor_scalar(out=xn[tt][:], in0=xt[i][:], scalar1=ss[i][:, 0:1], scalar2=None, op0=ALU.mult),
                     reads=[r_ss[i], r_xt[i]], writes=[r_xn[tt]])
            for j in range(KC):
                bk = 4 + j % 4
                for tt in range(4):
                    p.op("pe", lambda e, j=j, tt=tt, bk=bk: e.transpose(
                        out=cx.banks[bk][:, tt * 128:(tt + 1) * 128], in_=xn[tt][:, j * 128:(j + 1) * 128], identity=cx.ident_f[:]),
                        reads=[r_xn[tt], cx.r_ident], writes=[cx.bank_r[bk]], pe_acc=(tt > 0))
                p.op("act", lambda e, j=j, bk=bk, hb=hb: e.activation(
                    out=hs[hb][:, j, :], in_=cx.banks[bk][:], func=AF.Identity, scale=sc[:, j:j + 1], bias=sh[:, j:j + 1]),
                    reads=[cx.bank_r[bk], r_c], writes=[r_hs[hb]])
            c0 = hcol0 + blk * 512
            p.dma("pool", hT[:, c0:c0 + 512].rearrange("(k p) t -> p k t", p=128), hs[hb][:], reads=[r_hs[hb]], writes=[r_hT], store=True)
        p.barrier()


def stage_proj(p, cx, w_in_l, hT, FT, TM, ntok, r_hT, r_FT, r_TM):
    with contextlib.ExitStack() as st:
        def sb(name, shape, dt):
            return st.enter_context(p.sbt("B" + name, list(shape), dt))
        wg = sb("wg", [128, KC, 1024], BF16)
        r_wg = p.region()
        hb = [sb(f"hb{i}", [128, KC, 512], BF16) for i in range(2)]
        r_hb = [p.region() for _ in range(2)]
        og = [sb(f"og{i}", [128, 512], BF16) for i in range(2)]
        r_og = [p.region() for _ in range(2)]
        nblk = ntok // 512
        chunks = []
        for seg in FT_ORDER:
            for c in range(IN_SIZES[seg] // 128):
                chunks.append((IN_OFFS[seg] + c * 128, seg in (QA, QB, QC, QD)))
        ngrp = (len(chunks) + 7) // 8
        it = 0
        ib = 0
        for g in range(ngrp):
            gch = chunks[g * 8:(g + 1) * 8]
            for j, (c0, isq) in enumerate(gch):
                p.dma("pool", wg[:, :, j * 128:(j + 1) * 128], w_in_l[:, c0:c0 + 128].rearrange("(k p) c -> p k c", p=128), writes=[r_wg])
            for blk in range(nblk):
                i = ib % 2
                ib += 1
                p.dma("sp", hb[i][:], hT[:, blk * 512:(blk + 1) * 512].rearrange("(k p) t -> p k t", p=128), reads=[r_hT], writes=[r_hb[i]])
                for j, (c0, isq) in enumerate(gch):
                    bk = it % 4
                    o = it % 2
                    it += 1
                    for k in range(KC):
                        p.op("pe", lambda e, j=j, k=k, i=i, bk=bk: e.matmul(cx.banks[bk][:], wg[:, k, j * 128:(j + 1) * 128], hb[i][:, k, :],
                                                                        start=(k == 0), stop=(k == KC - 1)),
                             reads=[r_wg, r_hb[i]], writes=[cx.bank_r[bk]], pe_acc=(k > 0))
                    p.op("act", lambda e, bk=bk, o=o, isq=isq: e.activation(out=og[o][:], in_=cx.banks[bk][:], func=AF.Copy, scale=(0.125 if isq else 1.0)),
                         reads=[cx.bank_r[bk]], writes=[r_og[o]])
                    row = (g * 8 + j) * 128
                    p.dma("sp", FT[row:row + 128, blk * 512:(blk + 1) * 512], og[o][:], reads=[r_og[o]], writes=[r_FT], store=True)
        wt = sb("wt", [128, KC, TM_COLS], BF16)
        r_wt = p.region()
        for seg in TM_ORDER:
            p.dma("pool", wt[:, :, TM_OFF[seg]:TM_OFF[seg] + IN_SIZES[seg]],
                  w_in_l[:, IN_OFFS[seg]:IN_OFFS[seg] + IN_SIZES[seg]].rearrange("(k p) c -> p k c", p=128), writes=[r_wt])
        ot = [sb(f"ot{i}", [128, TM_COLS], BF16) for i in range(2)]
        r_ot = [p.region() for _ in range(2)]
        cgs = [(0, 512), (512, 512), (1024, TM_COLS - 1024)]
        itt = 0
        for blk in range(nblk):
            i = ib % 2
            ib += 1
            p.dma("sp", hb[i][:], hT[:, blk * 512:(blk + 1) * 512].rearrange("(k p) t -> p k t", p=128), reads=[r_hT], writes=[r_hb[i]])
            for tt in range(4):
                o = itt % 2
                itt += 1
                for (c0, cn) in cgs:
                    bk = it % 4
                    it += 1
                    for k in range(KC):
                        p.op("pe", lambda e, k=k, i=i, bk=bk, tt=tt, c0=c0, cn=cn: e.matmul(
                            cx.banks[bk][:, 0:cn], hb[i][:, k, tt * 128:(tt + 1) * 128], wt[:, k, c0:c0 + cn], start=(k == 0), stop=(k == KC - 1)),
                            reads=[r_wt, r_hb[i]], writes=[cx.bank_r[bk]], pe_acc=(k > 0))
                    p.op("dve", lambda e, bk=bk, o=o, c0=c0, cn=cn: e.tensor_copy(out=ot[o][:, c0:c0 + cn], in_=cx.banks[bk][:, 0:cn]),
                         reads=[cx.bank_r[bk]], writes=[r_ot[o]])
                t0 = blk * 512 + tt * 128
                p.dma("sp", TM[t0:t0 + 128, :], ot[o][:], reads=[r_ot[o]], writes=[r_TM], store=True)
        p.barrier()


W_STRIP = 3072
NSA_ONLY = None
DBG = None
OFF_STRIP = 384


def attn_core(p, cx, pT, r_pT, S, QT, r_q, KT, r_k, Kc, V, r_v, nv, eb, r_eb, clamp, ktiles_fn, skip_fn, out_cb, cmask=None, r_cm=None):
    steps = []
    nqc = S // 512
    for qc in range(nqc):
        kts = ktiles_fn(qc)
        for kt in kts:
            steps.append((qc, kt))
    used = {}
    for qc in range(nqc):
        for tq in range(4):
            used[(qc, tq)] = [kt for kt in ktiles_fn(qc) if not skip_fn(kt, qc * 4 + tq)]

    def qk(s):
        qc, kt = steps[s]
        bk = s % 2
        p.op("pe", lambda e, qc=qc, kt=kt, bk=bk: e.matmul(cx.banks[bk][:], KT[0:Kc, kt * 128:(kt + 1) * 128], QT[0:Kc, qc * 512:(qc + 1) * 512],
                                                         start=True, stop=True),
             reads=[r_q, r_k], writes=[cx.bank_r[bk]])
    qk(0)
    for s, (qc, kt) in enumerate(steps):
        if s + 1 < len(steps):
            qk(s + 1)
        bk = s % 2
        pb = s % 3
        p.op("act", lambda e, bk=bk, pb=pb: e.activation(out=pT[pb][:], in_=cx.banks[bk][:], func=AF.Exp), reads=[cx.bank_r[bk]], writes=[r_pT[pb]])
        if eb is not None:
            b = qc * 512 - kt * 128 + OFF_STRIP
            if clamp and b > 2048:
                b = 2048
            p.op("dve", lambda e, pb=pb, b=b: e.tensor_tensor(out=pT[pb][:], in0=pT[pb][:], in1=eb[:, b:b + 512], op=ALU.mult),
                 reads=[r_eb, r_pT[pb]], writes=[r_pT[pb]])
        if cmask is not None:
            p.op("dve", lambda e, pb=pb, kt=kt, qc=qc: e.tensor_tensor(out=pT[pb][:], in0=pT[pb][:], in1=cmask[:, kt, qc * 512:(qc + 1) * 512], op=ALU.mult),
                 reads=[r_cm, r_pT[pb]], writes=[r_pT[pb]])
        par = qc % 2
        for tq in range(4):
            ul = used[(qc, tq)]
            if kt not in ul:
                continue
            ob = 2 + tq
            oc = 0
            first = (kt == ul[0])
            last = (kt == ul[-1])
            p.op("pe", lambda e, pb=pb, tq=tq, kt=kt, ob=ob, oc=oc, first=first, last=last: e.matmul(
                cx.banks[ob][:, oc:oc + nv], pT[pb][:, tq * 128:(tq + 1) * 128], V[:, kt, 0:nv], start=first, stop=last),
                reads=[r_pT[pb], r_v], writes=[cx.bank_r[ob]], pe_acc=True)
            if last:
                out_cb(qc * 4 + tq, cx.banks[ob][:, oc:oc + nv], cx.bank_r[ob])


def stage_attn(p, cx, cst, FT, TM, OD, S, b, rel_strips, sinks_l, nsa_w, r_FT, r_TM, r_OD):
    col0 = b * S
    NKT = S // 128
    with contextlib.ExitStack() as st:
        def sb(name, shape, dt):
            return st.enter_context(p.sbt("C" + name, list(shape), dt))
        QT = sb("QT", [128, S], BF16)
        KT = sb("KT", [128, S], BF16)
        V = sb("V", [128, NKT, 129], BF16)
        r_q, r_k, r_v = p.region(), p.region(), p.region()
        pT = [sb(f"pT{i}", [128, 512], BF16) for i in range(3)]
        r_pT = [p.region() for _ in range(3)]
        sbias = sb("sbias", [128, W_STRIP], F32)
        smult = sb("smult", [128, W_STRIP], F32)
        eb = sb("eb", [128, W_STRIP], BF16)
        eb2 = None
        r_sb, r_sm, r_eb, r_eb2 = p.region(), p.region(), p.region(), None
        osb = [sb(f"osb{i}", [128, 64], F32) for i in range(2)]
        r_osb = [p.region() for _ in range(2)]
        rden = [sb(f"rden{i}", [128, 1], F32) for i in range(2)]
        r_rd = [p.region() for _ in range(2)]
        esink = sb("esink", [128, 8], F32)
        r_es = p.region()
        p.dma("sp", esink[:], bcast_rows(sinks_l, 128), writes=[r_es])
        p.op("act", lambda e: e.activation(out=esink[:], in_=esink[:], func=AF.Exp), reads=[r_es], writes=[r_es])
        p.op("pool", lambda e: e.memset(V[:, :, 64:65], 1.0), writes=[r_v])
        cnt = [0]

        def load_eb(strip_idx, mult_idx, dst, r_dst):
            p.dma("sp", sbias[:], rel_strips[strip_idx], writes=[r_sb])
            p.dma("sp", smult[:], cst["mults"][mult_idx], writes=[r_sm])
            p.op("act", lambda e: e.activation(out=sbias[:], in_=sbias[:], func=AF.Exp), reads=[r_sb], writes=[r_sb])
            p.op("dve", lambda e: e.tensor_tensor(out=dst[:], in0=sbias[:], in1=smult[:], op=ALU.mult), reads=[r_sb, r_sm], writes=[r_dst])

        def load_ft(dst, rows, seg, idx, width, r_dst):
            r0 = FT_OFF[seg] + idx * width
            p.dma("sp", dst[rows[0]:rows[0] + width, :], FT[r0:r0 + width, col0:col0 + S], reads=[r_FT], writes=[r_dst])

        def load_v(seg, idx, r_dst, c_dst=0):
            c0 = TM_OFF[seg] + idx * 64
            p.dma("sp", V[:, :, c_dst:c_dst + 64], TM[col0:col0 + S, c0:c0 + 64].rearrange("(t p) c -> p t c", p=128), reads=[r_TM], writes=[r_dst])

        def simple_out(mixer, h, extra_den=None):
            def cb(qt, ps, r_bank):
                i = cnt[0] % 2
                cnt[0] += 1
                if extra_den is not None:
                    p.op("dve", lambda e, i=i: e.tensor_tensor(out=rden[i][:], in0=ps[:, 64:65], in1=extra_den, op=ALU.add),
                         reads=[r_bank, r_es], writes=[r_rd[i]])
                    p.op("dve", lambda e, i=i: e.reciprocal(out=rden[i][:], in_=rden[i][:]), reads=[r_rd[i]], writes=[r_rd[i]])
                else:
                    p.op("dve", lambda e, i=i: e.reciprocal(out=rden[i][:], in_=ps[:, 64:65]), reads=[r_bank], writes=[r_rd[i]])
                p.op("dve", lambda e, i=i: e.tensor_scalar(out=osb[i][:], in0=ps[:, 0:64], scalar1=rden[i][:, 0:1], scalar2=None, op0=ALU.mult),
                     reads=[r_bank, r_rd[i]], writes=[r_osb[i]])
                t0 = col0 + qt * 128
                c0 = mixer * 512 + h * 64
                p.dma("pool", OD[t0:t0 + 128, c0:c0 + 64], osb[i][:], reads=[r_osb[i]], writes=[r_OD], store=True)
            return cb

        causal_skip = lambda kt, qt: kt > qt
        for h in range(8):
            load_ft(QT, (0,), QA, h, 64, r_q)
            load_ft(KT, (0,), KA, h, 64, r_k)
            load_v(VA, h, r_v)
            load_eb(0 * 8 + h, 0, eb, r_eb)
            attn_core(p, cx, pT, r_pT, S, QT, r_q, KT, r_k, 64, V, r_v, 65, eb, r_eb, False,
                      lambda qc: list(range(max(0, (qc * 512 - 2048) // 128), min(NKT, qc * 4 + 4))), causal_skip, simple_out(0, h))
        for h in range(8):
            load_ft(QT, (0,), QB, h, 64, r_q)
            if h % 4 == 0:
                load_ft(KT, (0,), KB_, h // 4, 64, r_k)
                load_v(VB, h // 4, r_v)
            load_eb(1 * 8 + h, 1, eb, r_eb)
            attn_core(p, cx, pT, r_pT, S, QT, r_q, KT, r_k, 64, V, r_v, 65, eb, r_eb, False,
                      lambda qc: list(range(max(0, qc * 4 - 1), min(NKT, qc * 4 + 4))), causal_skip, simple_out(1, h, extra_den=esink[:, h:h + 1]))
        stage_moba(p, cx, cst, sb, S, col0, FT, QT, KT, V, r_q, r_k, r_v, pT, r_pT, eb, r_eb, load_ft, load_v, load_eb, simple_out, causal_skip, r_FT)
        stage_nsa(p, cx, cst, sb, S, col0, FT, TM, OD, QT, KT, V, r_q, r_k, r_v, pT, r_pT, eb, r_eb, eb2, r_eb2, load_ft, load_v, load_eb,
                  causal_skip, nsa_w, r_FT, r_TM, r_OD, rden, r_rd, osb, r_osb, cnt)
        p.barrier()


def bcast_rows(ap1d, n):
    return bass.AP(tensor=ap1d.tensor, offset=ap1d.offset, ap=[[0, n]] + [list(x) for x in ap1d.ap])


def stage_moba(p, cx, cst, sb, S, col0, FT, QT, KT, V, r_q, r_k, r_v, pT, r_pT, eb, r_eb, load_ft, load_v, load_eb, simple_out, causal_skip, r_FT):
    NKT = S // 128
    nblk = S // 256
    kmf = sb("kmf", [64, 16], F32)
    kmb = sb("kmb", [64, 16], BF16)
    r_km = p.region()
    mv = sb("mv", [128, 16, 16], F32)
    own = sb("own", [128, 16, 16], F32)
    r_mc = p.region()
    p.dma("sp", mv[:].rearrange("p a b -> p (a b)"), bcast_rows(cst["moba_valid"], 128), writes=[r_mc])
    p.dma("sp", own[:].rearrange("p a b -> p (a b)"), bcast_rows(cst["moba_own"], 128), writes=[r_mc])
    gs = sb("gs", [128, 16], F32)
    m8 = sb("m8", [128, 8], F32)
    sel = sb("sel", [128, 16], F32)
    wide = sb("wide", [128, 128], F32)
    r_gs, r_m8, r_sel, r_wide = p.region(), p.region(), p.region(), p.region()
    p.op("pool", lambda e: e.memset(wide[:], 0.0), writes=[r_wide])
    p.op("pool", lambda e: e.memset(kmf[:], 0.0), writes=[r_km])
    p.dma("sp", KT[64:80, :], cst["moba_koh"], writes=[r_k])
    for h in range(8):
        load_ft(QT, (0,), QC, h, 64, r_q)
        load_ft(KT, (0,), KC_, h, 64, r_k)
        load_v(VC, h, r_v)
        load_eb(2 * 8 + h, 2, eb, r_eb)
        p.op("dve", lambda e: e.tensor_reduce(out=kmf[:, 0:nblk], in_=KT[0:64, :].rearrange("p (n k) -> p n k", k=256), axis=AX.X, op=ALU.add),
             reads=[r_k], writes=[r_km])
        p.op("dve", lambda e: e.tensor_scalar(out=kmb[:], in0=kmf[:], scalar1=1.0 / 256, scalar2=None, op0=ALU.mult), reads=[r_km], writes=[r_km])
        for qt in range(NKT):
            cur = qt // 2
            p.op("pe", lambda e, qt=qt: e.matmul(cx.banks[6][:, 0:16], QT[0:64, qt * 128:(qt + 1) * 128], kmb[:], start=True, stop=True),
                 reads=[r_q, r_km], writes=[cx.bank_r[6]])
            p.op("dve", lambda e, cur=cur: e.tensor_tensor(out=gs[:], in0=cx.banks[6][:, 0:16], in1=mv[:, cur, :], op=ALU.add),
                 reads=[cx.bank_r[6], r_mc], writes=[r_gs])
            p.op("dve", lambda e: e.max(out=m8[:], in_=gs[:]), reads=[r_gs], writes=[r_m8])
            p.op("dve", lambda e: e.tensor_scalar(out=m8[:, 2:3], in0=m8[:, 2:3], scalar1=-1e29, scalar2=None, op0=ALU.max), reads=[r_m8], writes=[r_m8])
            p.op("dve", lambda e: e.tensor_scalar(out=sel[:], in0=gs[:], scalar1=m8[:, 2:3], scalar2=None, op0=ALU.is_ge), reads=[r_gs, r_m8], writes=[r_sel])
            p.op("dve", lambda e, cur=cur: e.tensor_tensor(out=sel[:], in0=sel[:], in1=own[:, cur, :], op=ALU.max), reads=[r_sel, r_mc], writes=[r_sel])
            p.op("dve", lambda e: e.tensor_scalar(out=wide[:, 64:80], in0=sel[:], scalar1=-1.0, scalar2=-NEG, op0=ALU.add, op1=ALU.mult),
                 reads=[r_sel], writes=[r_wide])
            p.op("pe", lambda e: e.transpose(out=cx.banks[7][:, 0:128], in_=wide[:], identity=cx.ident_f[:]), reads=[r_wide, cx.r_ident], writes=[cx.bank_r[7]])
            p.op("act", lambda e, qt=qt: e.activation(out=QT[64:80, qt * 128:(qt + 1) * 128], in_=cx.banks[7][64:80, 0:128], func=AF.Copy),
                 reads=[cx.bank_r[7]], writes=[r_q])
        attn_core(p, cx, pT, r_pT, S, QT, r_q, KT, r_k, 80, V, r_v, 65, eb, r_eb, True,
                  lambda qc: list(range(0, min(NKT, qc * 4 + 4))), causal_skip, simple_out(2, h))


def stage_nsa(p, cx, cst, sb, S, col0, FT, TM, OD, QT, KT, V, r_q, r_k, r_v, pT, r_pT, eb, r_eb, eb2, r_eb2, load_ft, load_v, load_eb,
              causal_skip, nsa_w, r_FT, r_TM, r_OD, rden, r_rd, osb, r_osb, cnt):
    NKT = S // 128
    ncmp = (S - 32) // 16 + 1
    w1k, w2k, w1v, w2v, pek, pev = nsa_w
    w1 = [sb(f"w1_{i}", [64, 32, 128], BF16) for i in range(2)]
    w2 = [sb(f"w2_{i}", [128, 64], BF16) for i in range(2)]
    peT = [sb(f"peT{i}", [64, 32], BF16) for i in range(2)]
    r_w = p.region()
    for i, (a, b2, c) in enumerate(((w1k, w2k, pek), (w1v, w2v, pev))):
        p.dma("pool", w1[i][:], a.rearrange("(t d) m -> d t m", d=64), writes=[r_w])
        p.dma("pool", w2[i][:], b2, writes=[r_w])
        p.dma("pool", peT[i][:], c.rearrange("t d -> d t"), writes=[r_w], allow_slow_non_contiguous=True)
    src = sb("csrc", [64, S], BF16)
    r_src = p.region()
    hbias = sb("hbias", [128, 1], F32)
    r_hb = p.region()
    xs = sb("xs", [128, 256], F32)
    u = sb("u", [128, 256], F32)
    hid = sb("hid", [128, 256], BF16)
    r_xs, r_u, r_hid = p.region(), p.region(), p.region()
    KTc = sb("KTc", [64, 256], BF16)
    Vc = sb("Vc", [128, 2, 129], BF16)
    r_ktc, r_vc = p.region(), p.region()
    cmask = sb("cmask", [128, 2, S], BF16)
    r_cm = p.region()
    p.dma("sp", cmask[:], cst["cmp_mask"].rearrange("(t p) s -> p t s", p=128), writes=[r_cm])
    gsig = sb("gsig", [128, NKT, 24], F32)
    r_g = p.region()
    p.dma("sp", gsig[:], TM[col0:col0 + S, TM_OFF[GD]:TM_OFF[GD] + 24].rearrange("(t p) c -> p t c", p=128), reads=[r_TM], writes=[r_g]) \
        if False else None
    gtmp = sb("gtmp", [128, NKT, 24], BF16)
    p.dma("sp", gtmp[:], TM[col0:col0 + S, TM_OFF[GD]:TM_OFF[GD] + 24].rearrange("(t p) c -> p t c", p=128), reads=[r_TM], writes=[r_g])
    p.op("act", lambda e: e.activation(out=gsig[:], in_=gtmp[:], func=AF.Sigmoid), reads=[r_g], writes=[r_g])
    imp = sb("imp", [128, NKT, 64], F32)
    r_imp = p.region()
    oacc = [sb(f"oacc{g}", [128, NKT, 64], F32) for g in range(4)]
    r_oacc = [p.region() for _ in range(4)]
    selmul = sb("selmul", [128, 64], F32)
    seladd = sb("seladd", [128, 64], F32)
    r_sc = p.region()
    selT = sb("selT", [128, S], BF16)
    r_selT = p.region()
    v1 = sb("v1", [128, 64], F32)
    v2 = sb("v2", [128, 64], F32)
    m8a = sb("m8a", [128, 8], F32)
    m8b = sb("m8b", [128, 8], F32)
    wide = sb("nwide", [128, 128], F32)
    r_v1, r_v2, r_m8a, r_m8b, r_wide = p.region(), p.region(), p.region(), p.region(), p.region()
    p.op("pool", lambda e: e.memset(wide[:], 0.0), writes=[r_wide])
    p.op("pool", lambda e: e.memset(hid[:], 0.0), writes=[r_hid])
    p.op("pool", lambda e: e.memset(KTc[:], 0.0), writes=[r_ktc])
    p.op("pool", lambda e: e.memset(Vc[:, :, 64:65], 1.0), writes=[r_vc])
    p.dma("sp", Vc[:, :, 65:129], cst["overlap"].rearrange("(t p) c -> p t c", p=128), writes=[r_vc])
    tmp = [sb(f"ntmp{i}", [128, 64], F32) for i in range(2)]
    r_tmp = [p.region() for _ in range(2)]

    def compress(i, seg, kvh):
        r0 = FT_OFF[seg] + kvh * 64
        p.dma("sp", src[:], FT[r0:r0 + 64, col0:col0 + S], reads=[r_FT], writes=[r_src])
        sv = src[:].rearrange("p (n s) -> p n s", s=16)
        for t in range(32):
            p.op("pe", lambda e, t=t: e.matmul(cx.banks[6][:, 0:ncmp], w1[i][:, t, :], sv[:, t // 16:t // 16 + ncmp, t % 16], start=(t == 0), stop=(t == 31)),
                 reads=[r_w, r_src], writes=[cx.bank_r[6]], pe_acc=(t > 0))
        for t in range(32):
            p.op("pe", lambda e, t=t: e.matmul(cx.banks[7][:, 0:1], w1[i][:, t, :], peT[i][:, t:t + 1], start=(t == 0), stop=(t == 31)),
                 reads=[r_w], writes=[cx.bank_r[7]], pe_acc=(t > 0))
        p.op("dve", lambda e: e.tensor_copy(out=hbias[:], in_=cx.banks[7][:, 0:1]), reads=[cx.bank_r[7]], writes=[r_hb])
        p.op("act", lambda e: e.activation(out=xs[:, 0:ncmp], in_=cx.banks[6][:, 0:ncmp], func=AF.Identity, bias=hbias[:, 0:1]),
             reads=[cx.bank_r[6], r_hb], writes=[r_xs])
        p.op("dve", lambda e: e.tensor_tensor(out=u[:, 0:ncmp], in0=xs[:, 0:ncmp], in1=xs[:, 0:ncmp], op=ALU.mult), reads=[r_xs], writes=[r_u])
        p.op("dve", lambda e: e.tensor_scalar(out=u[:, 0:ncmp], in0=u[:, 0:ncmp], scalar1=0.044715, scalar2=1.0, op0=ALU.mult, op1=ALU.add), reads=[r_u], writes=[r_u])
        p.op("dve", lambda e: e.tensor_tensor(out=u[:, 0:ncmp], in0=u[:, 0:ncmp], in1=xs[:, 0:ncmp], op=ALU.mult), reads=[r_u, r_xs], writes=[r_u])
        p.op("act", lambda e: e.activation(out=u[:, 0:ncmp], in_=u[:, 0:ncmp], func=AF.Sigmoid, scale=2.0 * 0.7978845608028654), reads=[r_u], writes=[r_u])
        p.op("dve", lambda e: e.tensor_tensor(out=hid[:, 0:ncmp], in0=u[:, 0:ncmp], in1=xs[:, 0:ncmp], op=ALU.mult), reads=[r_u, r_xs], writes=[r_hid])

    for kvh in range(2):
        compress(0, KDC, kvh)
        p.op("pe", lambda e: e.matmul(cx.banks[6][0:64, 0:256], w2[0][:], hid[:], start=True, stop=True), reads=[r_w, r_hid], writes=[cx.bank_r[6]])
        p.op("act", lambda e: e.activation(out=KTc[:, 0:ncmp], in_=cx.banks[6][0:64, 0:ncmp], func=AF.Copy), reads=[cx.bank_r[6]], writes=[r_ktc])
        compress(1, VDC, kvh)
        for nt in range((ncmp + 127) // 128):
            p.op("pe", lambda e, nt=nt: e.matmul(cx.banks[7][:, 0:64], hid[:, nt * 128:(nt + 1) * 128], w2[1][:], start=True, stop=True),
                 reads=[r_w, r_hid], writes=[cx.bank_r[7]])
            p.op("act", lambda e, nt=nt: e.activation(out=Vc[:, nt, 0:64], in_=cx.banks[7][:, 0:64], func=AF.Copy), reads=[cx.bank_r[7]], writes=[r_vc])
        p.op("pool", lambda e: e.memset(imp[:], 0.0), writes=[r_imp])
        nct = (ncmp + 127) // 128
        for g in range(4):
            h = kvh * 4 + g
            load_ft(QT, (0,), QD, h, 64, r_q)

            def cmp_cb(qt, ps, r_bank, g=g, h=h):
                i = cnt[0] % 2
                cnt[0] += 1
                p.op("dve", lambda e, i=i: e.tensor_scalar(out=rden[i][:], in0=ps[:, 64:65], scalar1=1e-30, scalar2=None, op0=ALU.max), reads=[r_bank], writes=[r_rd[i]])
                p.op("dve", lambda e, i=i: e.reciprocal(out=rden[i][:], in_=rden[i][:]), reads=[r_rd[i]], writes=[r_rd[i]])
                p.op("dve", lambda e, i=i, qt=qt: e.scalar_tensor_tensor(out=imp[:, qt, :], in0=ps[:, 65:129], scalar=rden[i][:, 0:1], in1=imp[:, qt, :],
                                                                       op0=ALU.mult, op1=ALU.add), reads=[r_bank, r_rd[i], r_imp], writes=[r_imp])
                p.op("dve", lambda e, i=i: e.tensor_scalar(out=tmp[i][:], in0=ps[:, 0:64], scalar1=rden[i][:, 0:1], scalar2=None, op0=ALU.mult),
                     reads=[r_bank, r_rd[i]], writes=[r_tmp[i]])
                p.op("dve", lambda e, i=i, qt=qt: e.tensor_scalar(out=oacc[g][:, qt, :], in0=tmp[i][:], scalar1=(gsig[:, qt, h * 3:h * 3 + 1] if NSA_ONLY in (None, 0) else 0.0), scalar2=None, op0=ALU.mult),
                     reads=[r_tmp[i], r_g], writes=[r_oacc[g]])
            attn_core(p, cx, pT, r_pT, S, QT, r_q, KTc, r_ktc, 64, Vc, r_vc, 129, None, None, False,
                      lambda qc: list(range(nct)), lambda kt, qt: False, cmp_cb, cmask=cmask, r_cm=r_cm)
        for qt in range(NKT):
            p.dma("sp", selmul[:], cst["sel_mul"][qt * 128:(qt + 1) * 128, :], writes=[r_sc])
            p.dma("sp", seladd[:], cst["sel_add"][qt * 128:(qt + 1) * 128, :], writes=[r_sc])
            p.op("dve", lambda e, qt=qt: e.tensor_tensor(out=v1[:], in0=imp[:, qt, :], in1=selmul[:], op=ALU.mult), reads=[r_imp, r_sc], writes=[r_v1])
            p.op("dve", lambda e, qt=qt: e.tensor_tensor(out=v1[:], in0=v1[:], in1=seladd[:], op=ALU.add), reads=[r_v1, r_sc], writes=[r_v1])
            p.op("dve", lambda e: e.max(out=m8a[:], in_=v1[:]), reads=[r_v1], writes=[r_m8a])
            p.op("dve", lambda e: e.match_replace(out=v2[:], in_to_replace=m8a[:], in_values=v1[:], imm_value=-1e30), reads=[r_v1, r_m8a], writes=[r_v2])
            p.op("dve", lambda e: e.max(out=m8b[:], in_=v2[:]), reads=[r_v2], writes=[r_m8b])
            p.op("dve", lambda e: e.tensor_scalar(out=m8b[:, 7:8], in0=m8b[:, 7:8], scalar1=-1e29, scalar2=None, op0=ALU.max), reads=[r_m8b], writes=[r_m8b])
            p.op("dve", lambda e: e.tensor_scalar(out=v2[:], in0=v1[:], scalar1=m8b[:, 7:8], scalar2=None, op0=ALU.is_ge), reads=[r_v1, r_m8b], writes=[r_v2])
            p.op("dve", lambda e: e.tensor_scalar(out=wide[:, 64:128], in0=v2[:], scalar1=-1.0, scalar2=-NEG, op0=ALU.add, op1=ALU.mult), reads=[r_v2], writes=[r_wide])
            p.op("pe", lambda e: e.transpose(out=cx.banks[7][:, 0:128], in_=wide[:], identity=cx.ident_f[:]), reads=[r_wide, cx.r_ident], writes=[cx.bank_r[7]])
            p.op("act", lambda e, qt=qt: e.activation(out=selT[64:128, qt * 128:(qt + 1) * 128], in_=cx.banks[7][64:128, 0:128], func=AF.Copy),
                 reads=[cx.bank_r[7]], writes=[r_selT])
        for g in range(4):
            h = kvh * 4 + g
            load_ft(QT, (0,), QD, h, 64, r_q)
            p.op("act", lambda e: e.activation(out=QT[64:128, :], in_=selT[64:128, :], func=AF.Copy), reads=[r_selT], writes=[r_q])
            for br in (1, 2):
                if br == 1:
                    load_ft(KT, (0,), KDS, kvh, 64, r_k)
                    p.dma("sp", KT[64:128, :], cst["nsa_koh"], writes=[r_k])
                    load_v(VDS, kvh, r_v)
                    load_eb(3 * 8 + h, 2, eb, r_eb)
                    kfn = lambda qc: list(range(0, min(NKT, qc * 4 + 4)))
                    Kc, clamp = 128, True
                else:
                    load_ft(KT, (0,), KDW, kvh, 64, r_k)
                    load_v(VDW, kvh, r_v)
                    load_eb(3 * 8 + h, 3, eb, r_eb)
                    kfn = lambda qc: list(range(max(0, qc * 4 - 4), min(NKT, qc * 4 + 4)))
                    Kc, clamp = 64, False

                def br_cb(qt, ps, r_bank, g=g, h=h, br=br):
                    if NSA_ONLY is not None and NSA_ONLY != br:
                        return
                    i = cnt[0] % 2
                    cnt[0] += 1
                    p.op("dve", lambda e, i=i: e.reciprocal(out=rden[i][:], in_=ps[:, 64:65]), reads=[r_bank], writes=[r_rd[i]])
                    p.op("dve", lambda e, i=i: e.tensor_scalar(out=tmp[i][:], in0=ps[:, 0:64], scalar1=rden[i][:, 0:1], scalar2=None, op0=ALU.mult),
                         reads=[r_bank, r_rd[i]], writes=[r_tmp[i]])
                    p.op("dve", lambda e, i=i, qt=qt: e.scalar_tensor_tensor(out=oacc[g][:, qt, :], in0=tmp[i][:], scalar=gsig[:, qt, h * 3 + br:h * 3 + br + 1],
                                                                           in1=oacc[g][:, qt, :], op0=ALU.mult, op1=ALU.add),
                         reads=[r_tmp[i], r_g, r_oacc[g]], writes=[r_oacc[g]])
                if DBG is not None and h == 4 and br == 2:
                    r_dbg = p.region()
                    p.dma("sp", DBG[0][:, 0:S], QT[:], reads=[r_q], writes=[r_dbg])
                    p.dma("sp", DBG[1][:, 0:S], KT[:], reads=[r_k], writes=[r_dbg])
                    p.dma("sp", DBG[2][:, 0:S // 128 * 129], V[:].rearrange("p t c -> p (t c)"), reads=[r_v], writes=[r_dbg])
                    p.dma("sp", DBG[3][:, 0:W_STRIP], eb[:], reads=[r_eb], writes=[r_dbg])
                attn_core(p, cx, pT, r_pT, S, QT, r_q, KT, r_k, Kc, V, r_v, 65, eb, r_eb, clamp, kfn, causal_skip, br_cb)
            c0 = 3 * 512 + h * 64
            p.dma("pool", OD[col0:col0 + S, c0:c0 + 64].rearrange("(t p) c -> p t c", p=128), oacc[g][:], reads=[r_oacc[g]], writes=[r_OD], store=True)


def make_consts(S, rel_bias):
    cst = {}
    j = np.arange(128)[:, None]
    m = np.arange(W_STRIP)[None, :]
    d = m - OFF_STRIP - j
    dd = np.maximum(d, 0)
    bucket = t5_bucket_np(dd)
    strips = np.ascontiguousarray(np.transpose(rel_bias[bucket], (2, 0, 1))).astype(np.float32)
    causal = (d >= 0)
    multA = ((d >= 0) & (d <= 128)).astype(np.float32) + ((d >= 0) & (d % 4 == 0) & (d <= 512)) + ((d >= 0) & (d % 16 == 0) & (d <= 2048))
    mults = np.stack([multA, (causal & (d <= 127)), causal, (causal & (d <= 511))]).astype(np.float32)
    cst["mults"] = mults
    nblk = 16
    cur = np.arange(16)[:, None]
    n = np.arange(16)[None, :]
    cst["moba_valid"] = np.where(n < cur, 0.0, -1e30).astype(np.float32).reshape(-1)
    cst["moba_own"] = (n == cur).astype(np.float32).reshape(-1)
    import ml_dtypes
    bf = ml_dtypes.bfloat16
    key = np.arange(S)[None, :]
    cst["moba_koh"] = (key // 256 == np.arange(16)[:, None]).astype(bf)
    cst["nsa_koh"] = (key // 64 == np.arange(64)[:, None]).astype(bf)
    ncmp = (S - 32) // 16 + 1
    nn = np.arange(256)[:, None]
    cst["cmp_mask"] = ((nn * 16 + 31 <= key) & (nn < ncmp)).astype(bf)
    c_start = nn * 16
    j_start = np.arange(64)[None, :] * 64
    cst["overlap"] = ((c_start < j_start + 64) & (c_start + 32 > j_start) & (nn < ncmp)).astype(bf)
    pos = np.arange(S)[:, None]
    curq = pos // 64
    jj = np.arange(64)[None, :]
    forced = (jj == 0) | (jj == curq) | (jj == curq - 1)
    valid = jj <= curq
    cst["sel_mul"] = (valid & ~forced).astype(np.float32)
    cst["sel_add"] = np.where(forced, 1e30, np.where(valid, 0.0, -1e30)).astype(np.float32)
    return cst, strips


def stage_mix_out(p, cx, OD, x_src, x_dst, mixg_row, w_out_l, modD, l, NB, S, r_OD, r_x):
    with contextlib.ExitStack() as st:
        def sb(name, shape, dt):
            return st.enter_context(p.sbt("E" + name, list(shape), dt))
        wo = sb("wo", [128, KC, D], BF16)
        r_wo = p.region()
        for k4 in range(4):
            p.dma("pool", wo[:, k4 * 4:(k4 + 1) * 4, :], w_out_l[k4 * 512:(k4 + 1) * 512, :].rearrange("(k p) c -> p k c", p=128), writes=[r_wo])
        mg = sb("mg", [128, KC], F32)
        r_mg = p.region()
        load_cols(p, mg, r_mg, mixg_row)
        g1b = sb("g1b", [128, D], F32)
        r_g1 = p.region()
        ot = [sb(f"ot{i}", [128, D], F32) for i in range(2)]
        r_ot = [p.region() for _ in range(2)]
        junk = sb("junk", [128, 512], BF16)
        r_junk = p.region()
        ss = [sb(f"ss{i}", [128, 4], F32) for i in range(2)]
        r_ss = [p.region() for _ in range(2)]
        on = sb("on", [128, D], F32)
        r_on = p.region()
        cT = [sb(f"cT{i}", [128, KC, 128], BF16) for i in range(2)]
        r_cT = [p.region() for _ in range(2)]
        xt = [sb(f"xt{i}", [128, D], F32) for i in range(2)]
        r_xt = [p.region() for _ in range(2)]
        x1 = [sb(f"x1{i}", [128, D], F32) for i in range(2)]
        r_x1 = [p.region() for _ in range(2)]
        it = 0
        for b in range(NB):
            p.dma("sp", g1b[:], bcast_rows(modD[l, b, 2 * D:3 * D], 128), reads=[cx.r_modD], writes=[r_g1])
            for tt in range(S // 128):
                i = it % 2
                it += 1
                t0 = b * S + tt * 128
                p.dma("sp", ot[i][:], OD[t0:t0 + 128, :], reads=[r_OD], writes=[r_ot[i]])
                p.dma("sp", xt[i][:], x_src[t0:t0 + 128, :], reads=[r_x], writes=[r_xt[i]])
                for m in range(4):
                    p.op("act", lambda e, i=i, m=m: e.activation(out=junk[:], in_=ot[i][:, m * 512:(m + 1) * 512], func=AF.Square, accum_out=ss[i][:, m:m + 1]),
                         reads=[r_ot[i]], writes=[r_junk, r_ss[i]])
                p.op("dve", lambda e, i=i: e.tensor_scalar(out=ss[i][:], in0=ss[i][:], scalar1=1.0 / 512, scalar2=1e-6, op0=ALU.mult, op1=ALU.add),
                     reads=[r_ss[i]], writes=[r_ss[i]])
                p.op("act", lambda e, i=i: e.activation(out=ss[i][:], in_=ss[i][:], func=AF.Sqrt), reads=[r_ss[i]], writes=[r_ss[i]])
                p.op("dve", lambda e, i=i: e.reciprocal(out=ss[i][:], in_=ss[i][:]), reads=[r_ss[i]], writes=[r_ss[i]])
                for m in range(4):
                    p.op("dve", lambda e, i=i, m=m: e.tensor_scalar(out=on[:, m * 512:(m + 1) * 512], in0=ot[i][:, m * 512:(m + 1) * 512],
                                                                  scalar1=ss[i][:, m:m + 1], scalar2=None, op0=ALU.mult),
                         reads=[r_ot[i], r_ss[i]], writes=[r_on])
                for j4 in range(4):
                    bk = 4 + j4
                    for jj in range(4):
                        j = j4 * 4 + jj
                        p.op("pe", lambda e, j=j, jj=jj, bk=bk: e.transpose(out=cx.banks[bk][:, jj * 128:(jj + 1) * 128], in_=on[:, j * 128:(j + 1) * 128],
                                                                          identity=cx.ident_f[:]),
                             reads=[r_on, cx.r_ident], writes=[cx.bank_r[bk]], pe_acc=(jj > 0))
                    for jj in range(4):
                        j = j4 * 4 + jj
                        p.op("act", lambda e, j=j, jj=jj, bk=bk, i=i: e.activation(out=cT[i][:, j, :], in_=cx.banks[bk][:, jj * 128:(jj + 1) * 128],
                                                                             func=AF.Copy, scale=mg[:, j:j + 1]),
                             reads=[cx.bank_r[bk], r_mg], writes=[r_cT[i]])
                for n in range(4):
                    for k in range(KC):
                        p.op("pe", lambda e, n=n, k=k, i=i: e.matmul(cx.banks[n][:], cT[i][:, k, :], wo[:, k, n * 512:(n + 1) * 512], start=(k == 0), stop=(k == KC - 1)),
                             reads=[r_cT[i], r_wo], writes=[cx.bank_r[n]], pe_acc=(k > 0))
                    p.op("dve", lambda e, n=n, i=i: e.tensor_tensor(out=x1[i][:, n * 512:(n + 1) * 512], in0=cx.banks[n][:], in1=g1b[:, n * 512:(n + 1) * 512], op=ALU.mult),
                         reads=[cx.bank_r[n], r_g1], writes=[r_x1[i]])
                    p.op("dve", lambda e, n=n, i=i: e.tensor_tensor(out=x1[i][:, n * 512:(n + 1) * 512], in0=x1[i][:, n * 512:(n + 1) * 512],
                                                                  in1=xt[i][:, n * 512:(n + 1) * 512], op=ALU.add),
                         reads=[r_x1[i], r_xt[i]], writes=[r_x1[i]])
                p.dma("pool", x_dst[t0:t0 + 128, :], x1[i][:], reads=[r_x1[i]], writes=[r_x], store=True)
        p.barrier()


def stage_moe(p, cx, h2T, NT, router_w, router_bias, wg_l, wu_l, wd_l, yacc, r_h2T, r_y):
    NTL = NT // 128
    with contextlib.ExitStack() as st:
        def sb(name, shape, dt):
            return st.enter_context(p.sbt("M" + name, list(shape), dt))
        rwb = sb("rwb", [128, KC, 16], BF16)
        r_rw = p.region()
        p.dma("pool", rwb[:], router_w.rearrange("(k p) e -> p k e", p=128), writes=[r_rw])
        rb = sb("rb", [128, 16], F32)
        p.dma("sp", rb[:], bcast_rows(router_bias, 128), writes=[r_rw])
        comb = sb("comb", [128, NTL, 16], F32)
        r_comb = p.region()
        hb = [sb(f"hb{i}", [128, KC, 512], BF16) for i in range(2)]
        r_hb = [p.region() for _ in range(2)]
        aff = sb("aff", [128, 16], F32)
        sc = sb("sc", [128, 16], F32)
        t16 = sb("t16", [128, 16], F32)
        m1 = sb("m1", [128, 4], F32)
        m2 = sb("m2", [128, 4], F32)
        gm = sb("gm", [128, 1], F32)
        e1 = sb("e1", [128, 1], F32)
        r_aff, r_sc, r_t16, r_m1, r_m2, r_gm, r_e1 = (p.region() for _ in range(7))
        ib = 0

        def v3(t):
            return t[:].rearrange("p (g k) -> p g k", k=4)
        for blk in range(NT // 512):
            i = ib % 2
            ib += 1
            p.dma("sp", hb[i][:], h2T[:, blk * 512:(blk + 1) * 512].rearrange("(k p) t -> p k t", p=128), reads=[r_h2T], writes=[r_hb[i]])
            for tt in range(4):
                tl = blk * 4 + tt
                for k in range(KC):
                    p.op("pe", lambda e, k=k, i=i, tt=tt: e.matmul(cx.banks[6][:, 0:16], hb[i][:, k, tt * 128:(tt + 1) * 128], rwb[:, k, :], start=(k == 0), stop=(k == KC - 1)),
                         reads=[r_hb[i], r_rw], writes=[cx.bank_r[6]], pe_acc=(k > 0))
                p.op("act", lambda e: e.activation(out=aff[:], in_=cx.banks[6][:, 0:16], func=AF.Sigmoid), reads=[cx.bank_r[6]], writes=[r_aff])
                p.op("dve", lambda e: e.tensor_tensor(out=sc[:], in0=aff[:], in1=rb[:], op=ALU.add), reads=[r_aff, r_rw], writes=[r_sc])
                p.op("dve", lambda e: e.tensor_reduce(out=m1[:], in_=v3(sc), axis=AX.X, op=ALU.max), reads=[r_sc], writes=[r_m1])
                p.op("dve", lambda e: e.tensor_tensor(out=v3(t16), in0=v3(sc), in1=m1[:].unsqueeze(2).to_broadcast([128, 4, 4]), op=ALU.is_equal),
                     reads=[r_sc, r_m1], writes=[r_t16])
                p.op("dve", lambda e: e.scalar_tensor_tensor(out=t16[:], in0=t16[:], scalar=-1e30, in1=sc[:], op0=ALU.mult, op1=ALU.add),
                     reads=[r_t16, r_sc], writes=[r_t16])
                p.op("dve", lambda e: e.tensor_reduce(out=m2[:], in_=v3(t16), axis=AX.X, op=ALU.max), reads=[r_t16], writes=[r_m2])
                p.op("dve", lambda e: e.tensor_tensor(out=m1[:], in0=m1[:], in1=m2[:], op=ALU.add), reads=[r_m1, r_m2], writes=[r_m1])
                p.op("dve", lambda e: e.tensor_reduce(out=gm[:], in_=m1[:], axis=AX.X, op=ALU.max), reads=[r_m1], writes=[r_gm])
                p.op("dve", lambda e: e.tensor_scalar(out=m2[:], in0=m1[:], scalar1=gm[:, 0:1], scalar2=None, op0=ALU.is_ge), reads=[r_m1, r_gm], writes=[r_m2])
                p.op("dve", lambda e: e.tensor_scalar(out=m2[:], in0=m2[:], scalar1=-1.0, scalar2=1e30, op0=ALU.add, op1=ALU.mult), reads=[r_m2], writes=[r_m2])
                p.op("dve", lambda e: e.tensor_tensor(out=v3(t16), in0=v3(sc), in1=m2[:].unsqueeze(2).to_broadcast([128, 4, 4]), op=ALU.add),
                     reads=[r_sc, r_m2], writes=[r_t16])
                p.op("dve", lambda e: e.tensor_reduce(out=e1[:], in_=t16[:], axis=AX.X, op=ALU.max), reads=[r_t16], writes=[r_e1])
                p.op("dve", lambda e: e.tensor_scalar(out=sc[:], in0=t16[:], scalar1=e1[:, 0:1], scalar2=None, op0=ALU.is_equal), reads=[r_t16, r_e1], writes=[r_sc])
                p.op("dve", lambda e: e.scalar_tensor_tensor(out=t16[:], in0=sc[:], scalar=-1e30, in1=t16[:], op0=ALU.mult, op1=ALU.add),
                     reads=[r_sc, r_t16], writes=[r_t16])
                p.op("dve", lambda e: e.tensor_reduce(out=e1[:], in_=t16[:], axis=AX.X, op=ALU.max), reads=[r_t16], writes=[r_e1])
                p.op("dve", lambda e: e.tensor_scalar(out=t16[:], in0=t16[:], scalar1=e1[:, 0:1], scalar2=None, op0=ALU.is_equal), reads=[r_t16, r_e1], writes=[r_t16])
                p.op("dve", lambda e: e.tensor_tensor(out=sc[:], in0=sc[:], in1=t16[:], op=ALU.add), reads=[r_sc, r_t16], writes=[r_sc])
                p.op("dve", lambda e: e.tensor_tensor(out=sc[:], in0=sc[:], in1=aff[:], op=ALU.mult), reads=[r_sc, r_aff], writes=[r_sc])
                p.op("dve", lambda e: e.tensor_reduce(out=e1[:], in_=sc[:], axis=AX.X, op=ALU.add), reads=[r_sc], writes=[r_e1])
                p.op("dve", lambda e: e.reciprocal(out=e1[:], in_=e1[:]), reads=[r_e1], writes=[r_e1])
                p.op("dve", lambda e, tl=tl: e.tensor_scalar(out=comb[:, tl, :], in0=sc[:], scalar1=e1[:, 0:1], scalar2=None, op0=ALU.mult),
                     reads=[r_sc, r_e1], writes=[r_comb])
        wgu = [sb(f"wgu{i}", [128, KC, 1024], BF16) for i in range(2)]
        wd = [sb(f"wd{i}", [128, 4, D], BF16) for i in range(2)]
        r_wgu = [p.region() for _ in range(2)]
        r_wd = [p.region() for _ in range(2)]
        sg = [sb(f"sg{i}", [128, 512], F32) for i in range(2)]
        he = [sb(f"he{i}", [128, 512], F32) for i in range(2)]
        heT = [sb(f"heT{i}", [128, 4, 128], BF16) for i in range(2)]
        yt = [sb(f"yt{i}", [128, D], F32) for i in range(2)]
        r_sg = [p.region() for _ in range(2)]
        r_he = [p.region() for _ in range(2)]
        r_heT = [p.region() for _ in range(2)]
        r_yt = [p.region() for _ in range(2)]
        it = 0
        for ex in range(16):
            w = ex % 2
            p.dma("pool", wgu[w][:, :, 0:512], wg_l[ex].rearrange("(k p) f -> p k f", p=128), writes=[r_wgu[w]])
            p.dma("pool", wgu[w][:, :, 512:1024], wu_l[ex].rearrange("(k p) f -> p k f", p=128), writes=[r_wgu[w]])
            p.dma("pool", wd[w][:], wd_l[ex].rearrange("(k p) c -> p k c", p=128), writes=[r_wd[w]])
            for blk in range(NT // 512):
                i = ib % 2
                ib += 1
                p.dma("sp", hb[i][:], h2T[:, blk * 512:(blk + 1) * 512].rearrange("(k p) t -> p k t", p=128), reads=[r_h2T], writes=[r_hb[i]])
                for tt in range(4):
                    tl = blk * 4 + tt
                    o = it % 2
                    it += 1
                    for half in range(2):
                        for k in range(KC):
                            p.op("pe", lambda e, k=k, i=i, tt=tt, w=w, half=half: e.matmul(
                                cx.banks[half][:], hb[i][:, k, tt * 128:(tt + 1) * 128], wgu[w][:, k, half * 512:(half + 1) * 512], start=(k == 0), stop=(k == KC - 1)),
                                reads=[r_hb[i], r_wgu[w]], writes=[cx.bank_r[half]], pe_acc=(k > 0))
                    p.op("act", lambda e, o=o: e.activation(out=sg[o][:], in_=cx.banks[0][:], func=AF.Silu), reads=[cx.bank_r[0]], writes=[r_sg[o]])
                    p.op("dve", lambda e, o=o, tl=tl, ex=ex: e.scalar_tensor_tensor(out=he[o][:], in0=cx.banks[1][:], scalar=comb[:, tl, ex:ex + 1], in1=sg[o][:],
                                                                               op0=ALU.mult, op1=ALU.mult),
                         reads=[cx.bank_r[1], r_comb, r_sg[o]], writes=[r_he[o]])
                    bt = 2 + (it % 2)
                    for kk in range(4):
                        p.op("pe", lambda e, kk=kk, o=o, bt=bt: e.transpose(out=cx.banks[bt][:, kk * 128:(kk + 1) * 128], in_=he[o][:, kk * 128:(kk + 1) * 128],
                                                                          identity=cx.ident_f[:]),
                             reads=[r_he[o], cx.r_ident], writes=[cx.bank_r[bt]], pe_acc=(kk > 0))
                    p.op("act", lambda e, o=o, bt=bt: e.activation(out=heT[o][:].rearrange("p k t -> p (k t)"), in_=cx.banks[bt][:], func=AF.Copy),
                         reads=[cx.bank_r[bt]], writes=[r_heT[o]])
                    for n in range(4):
                        for kk in range(4):
                            p.op("pe", lambda e, n=n, kk=kk, o=o, w=w: e.matmul(cx.banks[4 + n][:], heT[o][:, kk, :], wd[w][:, kk, n * 512:(n + 1) * 512],
                                                                              start=(kk == 0), stop=(kk == 3)),
                                 reads=[r_heT[o], r_wd[w]], writes=[cx.bank_r[4 + n]], pe_acc=(kk > 0))
                        if n % 2 == 0:
                            p.op("act", lambda e, n=n, o=o: e.activation(out=yt[o][:, n * 512:(n + 1) * 512], in_=cx.banks[4 + n][:], func=AF.Copy),
                                 reads=[cx.bank_r[4 + n]], writes=[r_yt[o]])
                        else:
                            p.op("dve", lambda e, n=n, o=o: e.tensor_copy(out=yt[o][:, n * 512:(n + 1) * 512], in_=cx.banks[4 + n][:]),
                                 reads=[cx.bank_r[4 + n]], writes=[r_yt[o]])
                    if ex == 0:
                        p.dma("pool", yacc[tl * 128:(tl + 1) * 128, :], yt[o][:], reads=[r_yt[o]], writes=[r_y], store=True)
                    else:
                        p.dma("pool", yacc[tl * 128:(tl + 1) * 128, :], yt[o][:], reads=[r_yt[o]], writes=[r_y], accum_op=ALU.add, store=True)
        p.barrier()


def stage_resid(p, cx, xs, yacc, modD, l, goff, NB, S, r_x, r_y, final_g=None, out=None, r_out=None):
    with contextlib.ExitStack() as st:
        def sb(name, shape, dt):
            return st.enter_context(p.sbt("R" + name, list(shape), dt))
        gb = sb("gb", [128, D], F32)
        r_gb = p.region()
        fg = sb("fg", [128, D], F32)
        r_fg = p.region()
        if final_g is not None:
            p.dma("sp", fg[:], bcast_rows(final_g, 128), writes=[r_fg])
        xt = [sb(f"xt{i}", [128, D], F32) for i in range(2)]
        yt = [sb(f"yt{i}", [128, D], F32) for i in range(2)]
        r_xt = [p.region() for _ in range(2)]
        r_yt = [p.region() for _ in range(2)]
        junk = sb("junk", [128, D], BF16)
        r_junk = p.region()
        ss = [sb(f"ss{i}", [128, 1], F32) for i in range(2)]
        r_ss = [p.region() for _ in range(2)]
        it = 0
        for b in range(NB):
            p.dma("sp", gb[:], bcast_rows(modD[l, b, goff:goff + D], 128), reads=[cx.r_modD], writes=[r_gb])
            for tt in range(S // 128):
                i = it % 2
                it += 1
                t0 = b * S + tt * 128
                p.dma("sp", xt[i][:], xs[t0:t0 + 128, :], reads=[r_x], writes=[r_xt[i]])
                p.dma("sp", yt[i][:], yacc[t0:t0 + 128, :], reads=[r_y], writes=[r_yt[i]])
                p.op("dve", lambda e, i=i: e.tensor_tensor(out=yt[i][:], in0=yt[i][:], in1=gb[:], op=ALU.mult), reads=[r_yt[i], r_gb], writes=[r_yt[i]])
                p.op("dve", lambda e, i=i: e.tensor_tensor(out=xt[i][:], in0=xt[i][:], in1=yt[i][:], op=ALU.add), reads=[r_yt[i], r_xt[i]], writes=[r_xt[i]])
                if final_g is None:
                    p.dma("pool", xs[t0:t0 + 128, :], xt[i][:], reads=[r_xt[i]], writes=[r_x], store=True)
                else:
                    p.op("act", lambda e, i=i: e.activation(out=junk[:], in_=xt[i][:], func=AF.Square, accum_out=ss[i][:]),
                         reads=[r_xt[i]], writes=[r_junk, r_ss[i]])
                    p.op("dve", lambda e, i=i: e.tensor_scalar(out=ss[i][:], in0=ss[i][:], scalar1=1.0 / D, scalar2=1e-6, op0=ALU.mult, op1=ALU.add),
                         reads=[r_ss[i]], writes=[r_ss[i]])
                    p.op("act", lambda e, i=i: e.activation(out=ss[i][:], in_=ss[i][:], func=AF.Sqrt), reads=[r_ss[i]], writes=[r_ss[i]])
                    p.op("dve", lambda e, i=i: e.reciprocal(out=ss[i][:], in_=ss[i][:]), reads=[r_ss[i]], writes=[r_ss[i]])
                    p.op("dve", lambda e, i=i: e.scalar_tensor_tensor(out=yt[i][:], in0=xt[i][:], scalar=ss[i][:, 0:1], in1=fg[:], op0=ALU.mult, op1=ALU.mult),
                         reads=[r_xt[i], r_ss[i], r_fg], writes=[r_yt[i]])
                    p.dma("pool", out[t0:t0 + 128, :], yt[i][:], reads=[r_yt[i]], writes=[r_out], store=True)
        p.barrier()


CONST_SPECS = None


def build_program(NB, S, NL):
    NT = NB * S
    nc = bass.Bass("TRN2", target_bir_lowering=False)
    p = Prog(nc)

    def ext(name, shape, dt=F32):
        return nc.dram_tensor(name, list(shape), dt, kind="ExternalInput").ap()

    def internal(name, shape, dt=F32):
        return nc.dram_tensor(name, list(shape), dt, kind="Internal").ap()
    consts = ext("consts", [128, 128])
    x = ext("x", [NT, D])
    c = ext("c", [NB, D])
    strips = ext("strips", [32, 128, W_STRIP])
    cst = {"mults": ext("mults", [4, 128, W_STRIP]), "moba_valid": ext("moba_valid", [256]), "moba_own": ext("moba_own", [256]),
           "moba_koh": ext("moba_koh", [16, S], BF16), "nsa_koh": ext("nsa_koh", [64, S], BF16), "cmp_mask": ext("cmp_mask", [256, S], BF16),
           "overlap": ext("overlap", [256, 64], BF16), "sel_mul": ext("sel_mul", [S, 64]), "sel_add": ext("sel_add", [S, 64])}
    router_w = ext("router_w", [D, 16])
    router_bias = ext("router_bias", [16])
    norm1_g = ext("norm1_g", [NL, D])
    norm2_g = ext("norm2_g", [NL, D])
    ada_w = ext("ada_w", [NL, D, 6 * D])
    ada_b = ext("ada_b", [NL, 6 * D])
    w_in = ext("w_in", [NL, D, 5144])
    pek = ext("nsa_pe_k", [NL, 32, 64])
    pev = ext("nsa_pe_v", [NL, 32, 64])
    w1k = ext("nsa_cmp_w1_k", [NL, 2048, 128])
    w2k = ext("nsa_cmp_w2_k", [NL, 128, 64])
    w1v = ext("nsa_cmp_w1_v", [NL, 2048, 128])
    w2v = ext("nsa_cmp_w2_v", [NL, 128, 64])
    sinks = ext("sinks", [NL, 8])
    mixg = ext("mix_norm_g", [NL, D])
    w_out = ext("w_out", [NL, D, D])
    wg = ext("exp_w_gate", [NL, 16, D, 512])
    wu = ext("exp_w_up", [NL, 16, D, 512])
    wd = ext("exp_w_down", [NL, 16, 512, D])
    final_g = ext("final_g", [D])
    y = nc.dram_tensor("y", [NT, D], F32, kind="ExternalOutput").ap()
    modD = internal("modD", [NL, NB, 6 * D])
    xs = internal("xs", [NT, D])
    hT = internal("hT", [D, NT], BF16)
    FT = internal("FT", [FT_COLS, NT], BF16)
    TM = internal("TM", [NT, TM_COLS], BF16)
    OD = internal("OD", [NT, D])
    yacc = internal("yacc", [NT, D])
    cx = setup_ctx(p, consts)
    cx.r_modD = p.region("modD")
    r_x, r_hT, r_FT, r_TM, r_OD, r_y, r_out = (p.region() for _ in range(7))
    stage_mod(p, cx, c, ada_w, ada_b, modD, NL, NB)
    for l in range(NL):
        x_src = x if l == 0 else xs
        for b in range(NB):
            stage_norm_T(p, cx, x_src, b * S, S, norm1_g[l:l + 1, :], modD[l, b:b + 1, D:2 * D], modD[l, b:b + 1, 0:D], hT, b * S, "A", r_x, r_hT)
        stage_proj(p, cx, w_in[l], hT, FT, TM, NT, r_hT, r_FT, r_TM)
        for b in range(NB):
            stage_attn(p, cx, cst, FT, TM, OD, S, b, strips, sinks[l], (w1k[l], w2k[l], w1v[l], w2v[l], pek[l], pev[l]), r_FT, r_TM, r_OD)
        stage_mix_out(p, cx, OD, x_src, xs, mixg[l:l + 1, :], w_out[l], modD, l, NB, S, r_OD, r_x)
        for b in range(NB):
            stage_norm_T(p, cx, xs, b * S, S, norm2_g[l:l + 1, :], modD[l, b:b + 1, 4 * D:5 * D], modD[l, b:b + 1, 3 * D:4 * D], hT, b * S, "N", r_x, r_hT)
        stage_moe(p, cx, hT, NT, router_w, router_bias, wg[l], wu[l], wd[l], yacc, r_hT, r_y)
        last = (l == NL - 1)
        stage_resid(p, cx, xs, yacc, modD, l, 5 * D, NB, S, r_x, r_y, final_g=(final_g if last else None), out=y, r_out=r_out)
    p.finish()
    return nc, p


N_CORES = 4


def kernel(**inputs):
    import ml_dtypes
    B, S, _ = inputs["x"].shape
    NL = inputs["ada_w"].shape[0]
    NB = B // N_CORES
    nc, prog = build_program(NB, S, NL)
    f32 = lambda a: np.ascontiguousarray(np.asarray(a), dtype=np.float32)
    cst, strips = make_consts(S, f32(inputs["rel_bias"]))
    shared = {"consts": np.eye(128, dtype=np.float32), "strips": strips, **cst}
    for k in ("router_w", "router_bias", "norm1_g", "norm2_g", "ada_w", "ada_b", "w_in", "nsa_pe_k", "nsa_pe_v", "nsa_cmp_w1_k", "nsa_cmp_w2_k",
              "nsa_cmp_w1_v", "nsa_cmp_w2_v", "sinks", "w_out", "exp_w_gate", "exp_w_up", "exp_w_down", "final_g"):
        shared[k] = f32(inputs[k])
    shared["mix_norm_g"] = f32(inputs["mix_norm_g"]).reshape(NL, D)
    xin = f32(inputs["x"])
    cin = f32(inputs["c"])
    in_maps = []
    for ci in range(N_CORES):
        m = dict(shared)
        m["x"] = xin[ci * NB:(ci + 1) * NB].reshape(NB * S, D)
        m["c"] = cin[ci * NB:(ci + 1) * NB]
        in_maps.append(m)
    res = run_bass_kernel_spmd(nc, in_maps, core_ids=list(range(N_CORES)))
    out = np.concatenate([res.results[ci]["y"].reshape(NB, S, D) for ci in range(N_CORES)], axis=0)
    return out.astype(np.float32)
```

```python
import contextlib
import math
import numpy as np
import concourse.bass as bass
import concourse.mybir as mybir
from concourse.bass_utils import run_bass_kernel_spmd

F32 = mybir.dt.float32
BF16 = mybir.dt.bfloat16
AF = mybir.ActivationFunctionType
ALU = mybir.AluOpType
AX = mybir.AxisListType

D = 2048
KC = D // 128
HD = 64
NEG = -30000.0
IN_SIZES = (512, 512, 512, 512, 128, 128, 512, 512, 512, 512, 128, 128, 128, 128, 128, 128, 24)
IN_OFFS = [0] + list(np.cumsum(IN_SIZES))
(QA, KA, VA, QB, KB_, VB, QC, KC_, VC, QD, KDC, VDC, KDS, VDS, KDW, VDW, GD) = range(17)
FT_ORDER = [QA, KA, QB, KB_, QC, KC_, QD, KDC, VDC, KDS, KDW]
TM_ORDER = [VA, VB, VC, VDS, VDW, GD]
FT_COLS = sum(IN_SIZES[i] for i in FT_ORDER)
TM_COLS = sum(IN_SIZES[i] for i in TM_ORDER)
FT_OFF = {}
_o = 0
for _i in FT_ORDER:
    FT_OFF[_i] = _o
    _o += IN_SIZES[_i]
TM_OFF = {}
_o = 0
for _i in TM_ORDER:
    TM_OFF[_i] = _o
    _o += IN_SIZES[_i]


class Region:
    __slots__ = ("name", "last_w", "readers")

    def __init__(self, name):
        self.name = name
        self.last_w = None
        self.readers = {}


class Prog:
    ENG = ("pe", "act", "dve", "pool", "sp")
    NDMA = 88

    def __init__(self, nc):
        self.nc = nc
        self.ops = {e: [] for e in self.ENG}
        self.cnt = {e: 0 for e in self.ENG}
        self.seen = {e: {} for e in self.ENG}
        self.dma_cnt = [0] * self.NDMA
        self.dma_rr = 0
        self.stack = contextlib.ExitStack()
        self.nreg = 0
        self.out_events = []

    def sbt(self, name, shape, dtype):
        self.nuniq = getattr(self, "nuniq", 0) + 1
        return self.nc.sbuf_tensor(f"{name}_{self.nuniq}", list(shape), dtype)

    def region(self, name=None):
        self.nreg += 1
        return Region(name or f"r{self.nreg}")

    def sb(self, name, shape, dtype):
        t = self.stack.enter_context(self.nc.sbuf_tensor(name, list(shape), dtype))
        return t

    def ps(self, name, shape, dtype=F32):
        return self.stack.enter_context(self.nc.psum_tensor(name, list(shape), dtype))

    def dram(self, name, shape, dtype, kind="Internal"):
        return self.nc.dram_tensor(name, list(shape), dtype, kind=kind).ap()

    def _deps(self, reads, writes, pe_acc=False, eng=None):
        deps = {}

        def add(ev):
            if ev is None:
                return
            k, v = ev
            if deps.get(k, 0) < v:
                deps[k] = v
        for r in reads:
            add(r.last_w)
        for w in writes:
            if not (pe_acc and w.last_w is not None and w.last_w[0] == eng):
                add(w.last_w)
            for k, v in w.readers.items():
                add((k, v))
        return deps

    def _commit(self, ev, reads, writes):
        for r in reads:
            k, v = ev
            if r.readers.get(k, 0) < v:
                r.readers[k] = v
        for w in writes:
            w.last_w = ev
            w.readers = {}

    def _waits(self, eng, deps):
        waits = []
        for k, v in deps.items():
            if self.seen[eng].get(k, 0) < v:
                self.seen[eng][k] = v
                waits.append((k, v))
        return waits

    def op(self, eng, fn, reads=(), writes=(), pe_acc=False):
        deps = self._deps(reads, writes, pe_acc, eng)
        waits = self._waits(eng, deps)
        self.cnt[eng] += 1
        ev = (eng, self.cnt[eng])
        self.ops[eng].append((waits, fn, (eng, 1)))
        self._commit(ev, reads, writes)
        return ev

    def dma(self, q, out, in_, reads=(), writes=(), store=False, **kw):
        if store:
            deps = self._deps(reads, ())
            for w in writes:
                for k, v in w.readers.items():
                    if deps.get(k, 0) < v:
                        deps[k] = v
            reg = reads[0]
        else:
            deps = self._deps(reads, writes)
            reg = writes[0]
        waits = self._waits(q, deps)
        if not hasattr(self, "reg_sem"):
            self.reg_sem = {}
        if id(reg) not in self.reg_sem:
            self.reg_sem[id(reg)] = len(self.reg_sem) % self.NDMA
        j = self.reg_sem[id(reg)]
        self.dma_cnt[j] += 16
        ev = (("dma", j), self.dma_cnt[j])
        self.ops[q].append((waits, lambda e: e.dma_start(out=out, in_=in_, **kw), (("dma", j), 16)))
        self._commit(ev, reads, writes)
        return ev

    def barrier(self):
        evs = {}
        for e in self.ENG:
            if self.cnt[e]:
                evs[e] = self.cnt[e]
        for j in range(self.NDMA):
            if self.dma_cnt[j]:
                evs[("dma", j)] = self.dma_cnt[j]
        for e in self.ENG:
            waits = self._waits(e, {k: v for k, v in evs.items() if k != e})
            if waits:
                self.ops[e].append((waits, None, None))

    def finish(self):
        nc = self.nc
        sems = {}
        for e in self.ENG:
            sems[e] = self.stack.enter_context(nc.semaphore("sem_" + e))
        for j in range(self.NDMA):
            sems[("dma", j)] = self.stack.enter_context(nc.semaphore(f"sem_dma{j}"))
        final = []
        for e in self.ENG:
            if self.cnt[e] and e != "sp":
                final.append((e, self.cnt[e]))
        for j in range(self.NDMA):
            if self.dma_cnt[j]:
                final.append((("dma", j), self.dma_cnt[j]))
        ops = self.ops
        with nc.Block() as block:
            def run(engobj, name):
                for waits, fn, inc in ops[name]:
                    for k, v in waits:
                        engobj.wait_ge(sems[k], v)
                    if fn is None:
                        continue
                    ins = fn(engobj)
                    ins.then_inc(sems[inc[0]], inc[1])

            @block.tensor
            def _(t):
                run(t, "pe")

            @block.scalar
            def _(s):
                run(s, "act")

            @block.vector
            def _(v):
                run(v, "dve")

            @block.gpsimd
            def _(g):
                run(g, "pool")

            @block.sync
            def _(sp):
                run(sp, "sp")
                for k, v in final:
                    sp.wait_ge(sems[k], v)
        self.stack.close()


class Ctx:
    pass


def t5_bucket_np(dist):
    dist = np.maximum(dist, 0)
    max_exact = 16
    with np.errstate(divide="ignore"):
        large = max_exact + (np.log(np.maximum(dist, 1).astype(np.float32) / np.float32(max_exact))
                             / np.float32(math.log(2048 / max_exact)) * np.float32(32 - max_exact)).astype(np.int32)
    large = np.minimum(large, 31)
    return np.where(dist < max_exact, dist, large)


def setup_ctx(p, consts_ap):
    cx = Ctx()
    cx.banks = [p.ps(f"bank{i}", [128, 512], F32) for i in range(8)]
    cx.bank_r = [p.region(f"bank{i}") for i in range(8)]
    cx.ident_f = p.sb("ident_f", [128, 128], F32)
    cx.ident_b = p.sb("ident_b", [128, 128], BF16)
    cx.r_ident = p.region("ident")
    p.dma("sp", cx.ident_f[:], consts_ap[0:128, 0:128], writes=[cx.r_ident])
    p.op("dve", lambda e: e.tensor_copy(out=cx.ident_b[:], in_=cx.ident_f[:]), reads=[cx.r_ident], writes=[cx.r_ident])
    return cx


def stage_mod(p, cx, c_ap, ada_w, ada_b, modD, NL, NB):
    with contextlib.ExitStack() as st:
        def sb(name, shape, dt):
            return st.enter_context(p.sbt(name, list(shape), dt))
        cT = sb("m_cT", [128, KC, NB], F32)
        r_cT = p.region()
        for b in range(NB):
            p.dma("sp", cT[:, :, b], c_ap[b].rearrange("(k p) -> p k", p=128), writes=[r_cT], allow_slow_non_contiguous=True) \
                if False else p.dma("sp", cT[:, :, b:b + 1], c_ap[b:b + 1, :].rearrange("b (k p) -> p k b", p=128), writes=[r_cT],
                                    allow_slow_non_contiguous=True)
        p.op("act", lambda e: e.activation(out=cT[:], in_=cT[:], func=AF.Silu), reads=[r_cT], writes=[r_cT])
        wt = [sb(f"m_w{i}", [128, KC, 512], F32) for i in range(2)]
        r_w = [p.region() for _ in range(2)]
        bt = [sb(f"m_b{i}", [NB, 512], F32) for i in range(2)]
        r_b = [p.region() for _ in range(2)]
        ot = [sb(f"m_o{i}", [NB, 512], F32) for i in range(2)]
        r_o = [p.region() for _ in range(2)]
        it = 0
        for l in range(NL):
            for cc in range(6 * D // 512):
                i = it % 2
                it += 1
                p.dma("sp", wt[i][:], ada_w[l, :, cc * 512:(cc + 1) * 512].rearrange("(k p) c -> p k c", p=128), writes=[r_w[i]])
                for b in range(NB):
                    p.dma("sp", bt[i][b:b + 1, :], ada_b[l:l + 1, cc * 512:(cc + 1) * 512], writes=[r_b[i]])
                bk = cx.banks[i]
                for k in range(KC):
                    p.op("pe", lambda e, k=k, i=i, bk=bk: e.matmul(bk[0:NB, :], cT[:, k, :], wt[i][:, k, :], start=(k == 0), stop=(k == KC - 1)),
                         reads=[r_cT, r_w[i]], writes=[cx.bank_r[i]], pe_acc=(k > 0))
                p.op("dve", lambda e, i=i, bk=bk: e.tensor_tensor(out=ot[i][:], in0=bk[0:NB, :], in1=bt[i][:], op=ALU.add),
                     reads=[cx.bank_r[i], r_b[i]], writes=[r_o[i]])
                p.dma("sp", modD[l, :, cc * 512:(cc + 1) * 512], ot[i][:], reads=[r_o[i]], writes=[cx.r_modD], store=True)
        p.barrier()


def load_cols(p, sbt, r, src_row):
    p.dma("sp", sbt[:].rearrange("p (k o) -> p k o", o=1), src_row.rearrange("o (k p) -> p k o", p=128), writes=[r],
          allow_slow_non_contiguous=True)


def stage_norm_T(p, cx, x_src, tok0, ntok, g_row, sc_row, sh_row, hT, hcol0, tag, r_x, r_hT):
    with contextlib.ExitStack() as st:
        def sb(name, shape, dt):
            return st.enter_context(p.sbt(tag + name, list(shape), dt))
        gc = sb("gc", [128, KC], F32)
        sc = sb("sc", [128, KC], F32)
        sh = sb("sh", [128, KC], F32)
        r_c = p.region()
        load_cols(p, gc, r_c, g_row)
        load_cols(p, sc, r_c, sc_row)
        load_cols(p, sh, r_c, sh_row)
        p.op("dve", lambda e: e.scalar_tensor_tensor(out=sc[:], in0=sc[:], scalar=1.0, in1=gc[:], op0=ALU.add, op1=ALU.mult),
             reads=[r_c], writes=[r_c])
        xt = [sb(f"xt{i}", [128, D], F32) for i in range(2)]
        r_xt = [p.region() for _ in range(2)]
        junk = sb("junk", [128, D], BF16)
        r_junk = p.region()
        ss = [sb(f"ss{i}", [128, 1], F32) for i in range(2)]
        r_ss = [p.region() for _ in range(2)]
        xn = [sb(f"xn{i}", [128, D], F32) for i in range(4)]
        r_xn = [p.region() for _ in range(4)]
        hs = [sb(f"hs{i}", [128, KC, 512], BF16) for i in range(2)]
        r_hs = [p.region() for _ in range(2)]
        nblk = ntok // 512
        it = 0
        for blk in range(nblk):
            hb = blk % 2
            for tt in range(4):
                i = it % 2
                it += 1
                t0 = tok0 + blk * 512 + tt * 128
                p.dma("sp", xt[i][:], x_src[t0:t0 + 128, :], reads=[r_x], writes=[r_xt[i]])
                p.op("act", lambda e, i=i: e.activation(out=junk[:], in_=xt[i][:], func=AF.Square, accum_out=ss[i][:]),
                     reads=[r_xt[i]], writes=[r_junk, r_ss[i]])
                p.op("dve", lambda e, i=i: e.tensor_scalar(out=ss[i][:], in0=ss[i][:], scalar1=1.0 / D, scalar2=1e-6, op0=ALU.mult, op1=ALU.add),
                     reads=[r_ss[i]], writes=[r_ss[i]])
                p.op("act", lambda e, i=i: e.activation(out=ss[i][:], in_=ss[i][:], func=AF.Sqrt), reads=[r_ss[i]], writes=[r_ss[i]])
                p.op("dve", lambda e, i=i: e.reciprocal(out=ss[i][:], in_=ss[i][:]), reads=[r_ss[i]], writes=[r_ss[i]])
                p.op("dve", lambda e, i=i, tt=tt: e.tensor_scalar(out=xn[tt][:], in0=xt[i][:], scalar1=ss[i][:, 0:1], scalar2=None, op0=ALU.mult),
                     reads=[r_ss[i], r_xt[i]], writes=[r_xn[tt]])
            for j in range(KC):
                bk = 4 + j % 4
                for tt in range(4):
                    p.op("pe", lambda e, j=j, tt=tt, bk=bk: e.transpose(
                        out=cx.banks[bk][:, tt * 128:(tt + 1) * 128], in_=xn[tt][:, j * 128:(j + 1) * 128], identity=cx.ident_f[:]),
                        reads=[r_xn[tt], cx.r_ident], writes=[cx.bank_r[bk]], pe_acc=(tt > 0))
                p.op("act", lambda e, j=j, bk=bk, hb=hb: e.activation(
                    out=hs[hb][:, j, :], in_=cx.banks[bk][:], func=AF.Identity, scale=sc[:, j:j + 1], bias=sh[:, j:j + 1]),
                    reads=[cx.bank_r[bk], r_c], writes=[r_hs[hb]])
            c0 = hcol0 + blk * 512
            p.dma("pool", hT[:, c0:c0 + 512].rearrange("(k p) t -> p k t", p=128), hs[hb][:], reads=[r_hs[hb]], writes=[r_hT], store=True)
        p.barrier()


def stage_proj(p, cx, w_in_l, hT, FT, TM, ntok, r_hT, r_FT, r_TM):
    with contextlib.ExitStack() as st:
        def sb(name, shape, dt):
            return st.enter_context(p.sbt("B" + name, list(shape), dt))
        wg = sb("wg", [128, KC, 1024], BF16)
        r_wg = p.region()
        hb = [sb(f"hb{i}", [128, KC, 512], BF16) for i in range(2)]
        r_hb = [p.region() for _ in range(2)]
        og = [sb(f"og{i}", [128, 512], BF16) for i in range(2)]
        r_og = [p.region() for _ in range(2)]
        nblk = ntok // 512
        chunks = []
        for seg in FT_ORDER:
            for c in range(IN_SIZES[seg] // 128):
                chunks.append((IN_OFFS[seg] + c * 128, seg in (QA, QB, QC, QD)))
        ngrp = (len(chunks) + 7) // 8
        it = 0
        ib = 0
        for g in range(ngrp):
            gch = chunks[g * 8:(g + 1) * 8]
            for j, (c0, isq) in enumerate(gch):
                p.dma("pool", wg[:, :, j * 128:(j + 1) * 128], w_in_l[:, c0:c0 + 128].rearrange("(k p) c -> p k c", p=128), writes=[r_wg])
            for blk in range(nblk):
                i = ib % 2
                ib += 1
                p.dma("sp", hb[i][:], hT[:, blk * 512:(blk + 1) * 512].rearrange("(k p) t -> p k t", p=128), reads=[r_hT], writes=[r_hb[i]])
                for j, (c0, isq) in enumerate(gch):
                    bk = it % 4
                    o = it % 2
                    it += 1
                    for k in range(KC):
                        p.op("pe", lambda e, j=j, k=k, i=i, bk=bk: e.matmul(cx.banks[bk][:], wg[:, k, j * 128:(j + 1) * 128], hb[i][:, k, :],
                                                                        start=(k == 0), stop=(k == KC - 1)),
                             reads=[r_wg, r_hb[i]], writes=[cx.bank_r[bk]], pe_acc=(k > 0))
                    p.op("act", lambda e, bk=bk, o=o, isq=isq: e.activation(out=og[o][:], in_=cx.banks[bk][:], func=AF.Copy, scale=(0.125 if isq else 1.0)),
                         reads=[cx.bank_r[bk]], writes=[r_og[o]])
                    row = (g * 8 + j) * 128
                    p.dma("sp", FT[row:row + 128, blk * 512:(blk + 1) * 512], og[o][:], reads=[r_og[o]], writes=[r_FT], store=True)
        wt = sb("wt", [128, KC, TM_COLS], BF16)
        r_wt = p.region()
        for seg in TM_ORDER:
            p.dma("pool", wt[:, :, TM_OFF[seg]:TM_OFF[seg] + IN_SIZES[seg]],
                  w_in_l[:, IN_OFFS[seg]:IN_OFFS[seg] + IN_SIZES[seg]].rearrange("(k p) c -> p k c", p=128), writes=[r_wt])
        ot = [sb(f"ot{i}", [128, TM_COLS], BF16) for i in range(2)]
        r_ot = [p.region() for _ in range(2)]
        cgs = [(0, 512), (512, 512), (1024, TM_COLS - 1024)]
        itt = 0
        for blk in range(nblk):
            i = ib % 2
            ib += 1
            p.dma("sp", hb[i][:], hT[:, blk * 512:(blk + 1) * 512].rearrange("(k p) t -> p k t", p=128), reads=[r_hT], writes=[r_hb[i]])
            for tt in range(4):
                o = itt % 2
                itt += 1
                for (c0, cn) in cgs:
                    bk = it % 4
                    it += 1
                    for k in range(KC):
                        p.op("pe", lambda e, k=k, i=i, bk=bk, tt=tt, c0=c0, cn=cn: e.matmul(
                            cx.banks[bk][:, 0:cn], hb[i][:, k, tt * 128:(tt + 1) * 128], wt[:, k, c0:c0 + cn], start=(k == 0), stop=(k == KC - 1)),
                            reads=[r_wt, r_hb[i]], writes=[cx.bank_r[bk]], pe_acc=(k > 0))
                    p.op("dve", lambda e, bk=bk, o=o, c0=c0, cn=cn: e.tensor_copy(out=ot[o][:, c0:c0 + cn], in_=cx.banks[bk][:, 0:cn]),
                         reads=[cx.bank_r[bk]], writes=[r_ot[o]])
                t0 = blk * 512 + tt * 128
                p.dma("sp", TM[t0:t0 + 128, :], ot[o][:], reads=[r_ot[o]], writes=[r_TM], store=True)
        p.barrier()


W_STRIP = 3072
NSA_ONLY = None
DBG = None
OFF_STRIP = 384


def attn_core(p, cx, pT, r_pT, S, QT, r_q, KT, r_k, Kc, V, r_v, nv, eb, r_eb, clamp, ktiles_fn, skip_fn, out_cb, cmask=None, r_cm=None):
    steps = []
    nqc = S // 512
    for qc in range(nqc):
        kts = ktiles_fn(qc)
        for kt in kts:
            steps.append((qc, kt))
    used = {}
    for qc in range(nqc):
        for tq in range(4):
            used[(qc, tq)] = [kt for kt in ktiles_fn(qc) if not skip_fn(kt, qc * 4 + tq)]

    SB = (0, 1, 6, 7)
    NP = len(pT)

    def qk(s):
        qc, kt = steps[s]
        bk = SB[s % 4]
        p.op("pe", lambda e, qc=qc, kt=kt, bk=bk: e.matmul(cx.banks[bk][:], KT[0:Kc, kt * 128:(kt + 1) * 128], QT[0:Kc, qc * 512:(qc + 1) * 512],
                                                         start=True, stop=True),
             reads=[r_q, r_k], writes=[cx.bank_r[bk]])
    for s0 in range(min(3, len(steps))):
        qk(s0)
    for s, (qc, kt) in enumerate(steps):
        if s + 3 < len(steps):
            qk(s + 3)
        bk = SB[s % 4]
        pb = s % NP
        p.op("act", lambda e, bk=bk, pb=pb: e.activation(out=pT[pb][:], in_=cx.banks[bk][:], func=AF.Exp), reads=[cx.bank_r[bk]], writes=[r_pT[pb]])
        if eb is not None:
            b = qc * 512 - kt * 128 + OFF_STRIP
            if clamp and b > 2048:
                b = 2048
            p.op("dve", lambda e, pb=pb, b=b: e.tensor_tensor(out=pT[pb][:], in0=pT[pb][:], in1=eb[:, b:b + 512], op=ALU.mult),
                 reads=[r_eb, r_pT[pb]], writes=[r_pT[pb]])
        if cmask is not None:
            p.op("dve", lambda e, pb=pb, kt=kt, qc=qc: e.tensor_tensor(out=pT[pb][:], in0=pT[pb][:], in1=cmask[:, kt, qc * 512:(qc + 1) * 512], op=ALU.mult),
                 reads=[r_cm, r_pT[pb]], writes=[r_pT[pb]])
        par = qc % 2
        for tq in range(4):
            ul = used[(qc, tq)]
            if kt not in ul:
                continue
            ob = 2 + tq
            oc = 0
            first = (kt == ul[0])
            last = (kt == ul[-1])
            p.op("pe", lambda e, pb=pb, tq=tq, kt=kt, ob=ob, oc=oc, first=first, last=last: e.matmul(
                cx.banks[ob][:, oc:oc + nv], pT[pb][:, tq * 128:(tq + 1) * 128], V[:, kt, 0:nv], start=first, stop=last),
                reads=[r_pT[pb], r_v], writes=[cx.bank_r[ob]], pe_acc=True)
            if last:
                out_cb(qc * 4 + tq, cx.banks[ob][:, oc:oc + nv], cx.bank_r[ob])


def stage_eb(p, cx, rel_strips, cst, EBD, r_EBD):
    with contextlib.ExitStack() as st:
        def sb(name, shape, dt):
            return st.enter_context(p.sbt("P" + name, list(shape), dt))
        sbias = [sb(f"sbias{i}", [128, W_STRIP], F32) for i in range(2)]
        smult = sb("smult", [128, W_STRIP], F32)
        eb = [sb(f"eb{i}", [128, W_STRIP], BF16) for i in range(2)]
        r_sb = [p.region() for _ in range(2)]
        r_eb = [p.region() for _ in range(2)]
        r_sm = p.region()
        combos = [(h, 0) for h in range(8)] + [(8 + h, 1) for h in range(8)] + [(16 + h, 2) for h in range(8)] + \
                 [(24 + h, 2) for h in range(8)] + [(24 + h, 3) for h in range(8)]
        last_m = None
        for idx, (si, mi) in enumerate(combos):
            i = idx % 2
            if mi != last_m:
                p.dma("sp", smult[:], cst["mults"][mi], writes=[r_sm])
                last_m = mi
            p.dma("sp", sbias[i][:], rel_strips[si], writes=[r_sb[i]])
            p.op("act", lambda e, i=i: e.activation(out=sbias[i][:], in_=sbias[i][:], func=AF.Exp), reads=[r_sb[i]], writes=[r_sb[i]])
            p.op("dve", lambda e, i=i: e.tensor_tensor(out=eb[i][:], in0=sbias[i][:], in1=smult[:], op=ALU.mult), reads=[r_sb[i], r_sm], writes=[r_eb[i]])
            p.dma("pool", EBD[idx], eb[i][:], reads=[r_eb[i]], writes=[r_EBD], store=True)
        p.barrier()


def stage_attn(p, cx, cst, FT, TM, OD, S, b, EBD, sinks_l, nsa_w, r_FT, r_TM, r_OD):
    col0 = b * S
    NKT = S // 128
    with contextlib.ExitStack() as st:
        def sb(name, shape, dt):
            return st.enter_context(p.sbt("C" + name, list(shape), dt))
        QT = [sb(f"QT{i}", [128, S], BF16) for i in range(2)]
        KT = [sb(f"KT{i}", [128, S], BF16) for i in range(2)]
        V = [sb(f"V{i}", [128, NKT, 65], BF16) for i in range(2)]
        eb = [sb(f"eb{i}", [128, W_STRIP], BF16) for i in range(2)]
        r_q = [p.region() for _ in range(2)]
        r_k = [p.region() for _ in range(2)]
        r_v = [p.region() for _ in range(2)]
        r_eb = [p.region() for _ in range(2)]
        pT = [sb(f"pT{i}", [128, 512], BF16) for i in range(5)]
        r_pT = [p.region() for _ in range(5)]
        osb = [sb(f"osb{i}", [128, 64], F32) for i in range(8)]
        r_osb = [p.region() for _ in range(8)]
        rden = [sb(f"rden{i}", [128, 1], F32) for i in range(8)]
        r_rd = [p.region() for _ in range(8)]
        esink = sb("esink", [128, 8], F32)
        r_es = p.region()
        p.dma("sp", esink[:], bcast_rows(sinks_l, 128), writes=[r_es])
        p.op("act", lambda e: e.activation(out=esink[:], in_=esink[:], func=AF.Exp), reads=[r_es], writes=[r_es])
        for i in range(2):
            p.op("pool", lambda e, i=i: e.memset(V[i][:, :, 64:65], 1.0), writes=[r_v[i]])
        cnt = [0]
        slot = [-1]

        def nxt():
            slot[0] += 1
            return slot[0] % 2

        def load_eb(ebidx, dst, r_dst):
            p.dma("sp", dst[:], EBD[ebidx], writes=[r_dst])

        def load_ft(dst, rows, seg, idx, width, r_dst):
            r0 = FT_OFF[seg] + idx * width
            p.dma("sp", dst[rows[0]:rows[0] + width, :], FT[r0:r0 + width, col0:col0 + S], reads=[r_FT], writes=[r_dst])

        def load_v(Vt, seg, idx, r_dst):
            c0 = TM_OFF[seg] + idx * 64
            p.dma("sp", Vt[:, :, 0:64], TM[col0:col0 + S, c0:c0 + 64].rearrange("(t p) c -> p t c", p=128), reads=[r_TM], writes=[r_dst])

        def simple_out(mixer, h, extra_den=None):
            def cb(qt, ps, r_bank):
                i = cnt[0] % 8
                cnt[0] += 1
                if extra_den is not None:
                    p.op("dve", lambda e, i=i: e.tensor_tensor(out=rden[i][:], in0=ps[:, 64:65], in1=extra_den, op=ALU.add),
                         reads=[r_bank, r_es], writes=[r_rd[i]])
                    p.op("dve", lambda e, i=i: e.reciprocal(out=rden[i][:], in_=rden[i][:]), reads=[r_rd[i]], writes=[r_rd[i]])
                else:
                    p.op("dve", lambda e, i=i: e.reciprocal(out=rden[i][:], in_=ps[:, 64:65]), reads=[r_bank], writes=[r_rd[i]])
                p.op("dve", lambda e, i=i: e.tensor_scalar(out=osb[i][:], in0=ps[:, 0:64], scalar1=rden[i][:, 0:1], scalar2=None, op0=ALU.mult),
                     reads=[r_bank, r_rd[i]], writes=[r_osb[i]])
                t0 = col0 + qt * 128
                c0 = mixer * 512 + h * 64
                p.dma("pool", OD[t0:t0 + 128, c0:c0 + 64], osb[i][:], reads=[r_osb[i]], writes=[r_OD], store=True)
            return cb

        causal_skip = lambda kt, qt: kt > qt
        for h in range(8):
            i = nxt()
            load_ft(QT[i], (0,), QA, h, 64, r_q[i])
            load_ft(KT[i], (0,), KA, h, 64, r_k[i])
            load_v(V[i], VA, h, r_v[i])
            load_eb(h, eb[i], r_eb[i])
            attn_core(p, cx, pT, r_pT, S, QT[i], r_q[i], KT[i], r_k[i], 64, V[i], r_v[i], 65, eb[i], r_eb[i], False,
                      lambda qc: list(range(max(0, (qc * 512 - 2048) // 128), min(NKT, qc * 4 + 4))), causal_skip, simple_out(0, h))
        for h in range(8):
            i = nxt()
            load_ft(QT[i], (0,), QB, h, 64, r_q[i])
            load_ft(KT[i], (0,), KB_, h // 4, 64, r_k[i])
            load_v(V[i], VB, h // 4, r_v[i])
            load_eb(8 + h, eb[i], r_eb[i])
            attn_core(p, cx, pT, r_pT, S, QT[i], r_q[i], KT[i], r_k[i], 64, V[i], r_v[i], 65, eb[i], r_eb[i], False,
                      lambda qc: list(range(max(0, qc * 4 - 1), min(NKT, qc * 4 + 4))), causal_skip, simple_out(1, h, extra_den=esink[:, h:h + 1]))
        stage_moba(p, cx, cst, sb, S, col0, FT, QT, KT, V, r_q, r_k, r_v, pT, r_pT, eb, r_eb, nxt, load_ft, load_v, load_eb, simple_out, causal_skip, r_FT)
        stage_nsa(p, cx, cst, sb, S, col0, FT, TM, OD, QT, KT, V, r_q, r_k, r_v, pT, r_pT, eb, r_eb, nxt, load_ft, load_v, load_eb,
                  causal_skip, nsa_w, r_FT, r_TM, r_OD, rden, r_rd, osb, r_osb, cnt)
        p.barrier()


def bcast_rows(ap1d, n):
    return bass.AP(tensor=ap1d.tensor, offset=ap1d.offset, ap=[[0, n]] + [list(x) for x in ap1d.ap])


def stage_moba(p, cx, cst, sb, S, col0, FT, QTs, KTs, Vs, r_qs, r_ks, r_vs, pT, r_pT, ebs, r_ebs, nxt, load_ft, load_v, load_eb, simple_out, causal_skip, r_FT):
    NKT = S // 128
    nblk = S // 256
    kmf = sb("kmf", [64, 16], F32)
    kmb = sb("kmb", [64, 16], BF16)
    r_km = p.region()
    mv = sb("mv", [128, 16, 16], F32)
    own = sb("own", [128, 16, 16], F32)
    r_mc = p.region()
    p.dma("sp", mv[:].rearrange("p a b -> p (a b)"), bcast_rows(cst["moba_valid"], 128), writes=[r_mc])
    p.dma("sp", own[:].rearrange("p a b -> p (a b)"), bcast_rows(cst["moba_own"], 128), writes=[r_mc])
    gs = sb("gs", [128, 16], F32)
    m8 = sb("m8", [128, 8], F32)
    sel = sb("sel", [128, 16], F32)
    wide = sb("wide", [128, 128], F32)
    r_gs, r_m8, r_sel, r_wide = p.region(), p.region(), p.region(), p.region()
    p.op("pool", lambda e: e.memset(wide[:], 0.0), writes=[r_wide])
    p.op("pool", lambda e: e.memset(kmf[:], 0.0), writes=[r_km])
    for i in range(2):
        p.dma("sp", KTs[i][64:80, :], cst["moba_koh"], writes=[r_ks[i]])
    for h in range(8):
        i = nxt()
        QT, KT, V, eb, r_q, r_k, r_v, r_eb = QTs[i], KTs[i], Vs[i], ebs[i], r_qs[i], r_ks[i], r_vs[i], r_ebs[i]
        load_ft(QT, (0,), QC, h, 64, r_q)
        load_ft(KT, (0,), KC_, h, 64, r_k)
        load_v(V, VC, h, r_v)
        load_eb(16 + h, eb, r_eb)
        p.op("dve", lambda e, KT=KT: e.tensor_reduce(out=kmf[:, 0:nblk], in_=KT[0:64, :].rearrange("p (n k) -> p n k", k=256), axis=AX.X, op=ALU.add),
             reads=[r_k], writes=[r_km])
        p.op("dve", lambda e: e.tensor_scalar(out=kmb[:], in0=kmf[:], scalar1=1.0 / 256, scalar2=None, op0=ALU.mult), reads=[r_km], writes=[r_km])
        for qt in range(NKT):
            cur = qt // 2
            p.op("pe", lambda e, qt=qt, QT=QT: e.matmul(cx.banks[6][:, 0:16], QT[0:64, qt * 128:(qt + 1) * 128], kmb[:], start=True, stop=True),
                 reads=[r_q, r_km], writes=[cx.bank_r[6]])
            p.op("dve", lambda e, cur=cur: e.tensor_tensor(out=gs[:], in0=cx.banks[6][:, 0:16], in1=mv[:, cur, :], op=ALU.add),
                 reads=[cx.bank_r[6], r_mc], writes=[r_gs])
            p.op("dve", lambda e: e.max(out=m8[:], in_=gs[:]), reads=[r_gs], writes=[r_m8])
            p.op("dve", lambda e: e.tensor_scalar(out=m8[:, 2:3], in0=m8[:, 2:3], scalar1=-1e29, scalar2=None, op0=ALU.max), reads=[r_m8], writes=[r_m8])
            p.op("dve", lambda e: e.tensor_scalar(out=sel[:], in0=gs[:], scalar1=m8[:, 2:3], scalar2=None, op0=ALU.is_ge), reads=[r_gs, r_m8], writes=[r_sel])
            p.op("dve", lambda e, cur=cur: e.tensor_tensor(out=sel[:], in0=sel[:], in1=own[:, cur, :], op=ALU.max), reads=[r_sel, r_mc], writes=[r_sel])
            p.op("dve", lambda e: e.tensor_scalar(out=wide[:, 64:80], in0=sel[:], scalar1=-1.0, scalar2=-NEG, op0=ALU.add, op1=ALU.mult),
                 reads=[r_sel], writes=[r_wide])
            p.op("pe", lambda e: e.transpose(out=cx.banks[7][:, 0:128], in_=wide[:], identity=cx.ident_f[:]), reads=[r_wide, cx.r_ident], writes=[cx.bank_r[7]])
            p.op("act", lambda e, qt=qt, QT=QT: e.activation(out=QT[64:80, qt * 128:(qt + 1) * 128], in_=cx.banks[7][64:80, 0:128], func=AF.Copy),
                 reads=[cx.bank_r[7]], writes=[r_q])
        attn_core(p, cx, pT, r_pT, S, QT, r_q, KT, r_k, 80, V, r_v, 65, eb, r_eb, True,
                  lambda qc: list(range(0, min(NKT, qc * 4 + 4))), causal_skip, simple_out(2, h))


def stage_nsa(p, cx, cst, sb, S, col0, FT, TM, OD, QTs, KTs, Vs, r_qs, r_ks, r_vs, pT, r_pT, ebs, r_ebs, nxt, load_ft, load_v, load_eb,
              causal_skip, nsa_w, r_FT, r_TM, r_OD, rden, r_rd, osb, r_osb, cnt):
    NKT = S // 128
    ncmp = (S - 32) // 16 + 1
    w1k, w2k, w1v, w2v, pek, pev = nsa_w
    w1 = [sb(f"w1_{i}", [64, 32, 128], BF16) for i in range(2)]
    w2 = [sb(f"w2_{i}", [128, 64], BF16) for i in range(2)]
    peT = [sb(f"peT{i}", [64, 32], BF16) for i in range(2)]
    r_w = p.region()
    for i, (a, b2, c) in enumerate(((w1k, w2k, pek), (w1v, w2v, pev))):
        p.dma("pool", w1[i][:], a.rearrange("(t d) m -> d t m", d=64), writes=[r_w])
        p.dma("pool", w2[i][:], b2, writes=[r_w])
        p.dma("pool", peT[i][:], c.rearrange("t d -> d t"), writes=[r_w], allow_slow_non_contiguous=True)
    src = sb("csrc", [64, S], BF16)
    r_src = p.region()
    hbias = sb("hbias", [128, 1], F32)
    r_hb = p.region()
    xs = sb("xs", [128, 256], F32)
    u = sb("u", [128, 256], F32)
    hid = sb("hid", [128, 256], BF16)
    r_xs, r_u, r_hid = p.region(), p.region(), p.region()
    KTc = sb("KTc", [64, 256], BF16)
    Vc = sb("Vc", [128, 2, 129], BF16)
    r_ktc, r_vc = p.region(), p.region()
    cmask = sb("cmask", [128, 2, S], BF16)
    r_cm = p.region()
    p.dma("sp", cmask[:], cst["cmp_mask"].rearrange("(t p) s -> p t s", p=128), writes=[r_cm])
    gsig = sb("gsig", [128, NKT, 24], F32)
    r_g = p.region()
    p.dma("sp", gsig[:], TM[col0:col0 + S, TM_OFF[GD]:TM_OFF[GD] + 24].rearrange("(t p) c -> p t c", p=128), reads=[r_TM], writes=[r_g]) \
        if False else None
    gtmp = sb("gtmp", [128, NKT, 24], BF16)
    p.dma("sp", gtmp[:], TM[col0:col0 + S, TM_OFF[GD]:TM_OFF[GD] + 24].rearrange("(t p) c -> p t c", p=128), reads=[r_TM], writes=[r_g])
    p.op("act", lambda e: e.activation(out=gsig[:], in_=gtmp[:], func=AF.Sigmoid), reads=[r_g], writes=[r_g])
    imp = sb("imp", [128, NKT, 64], F32)
    r_imp = p.region()
    oacc = [sb(f"oacc{g}", [128, NKT, 64], F32) for g in range(4)]
    r_oacc = [p.region() for _ in range(4)]
    selmul = sb("selmul", [128, 64], F32)
    seladd = sb("seladd", [128, 64], F32)
    r_sc = p.region()
    selT = sb("selT", [128, S], BF16)
    r_selT = p.region()
    v1 = sb("v1", [128, 64], F32)
    v2 = sb("v2", [128, 64], F32)
    m8a = sb("m8a", [128, 8], F32)
    m8b = sb("m8b", [128, 8], F32)
    wide = sb("nwide", [128, 128], F32)
    r_v1, r_v2, r_m8a, r_m8b, r_wide = p.region(), p.region(), p.region(), p.region(), p.region()
    p.op("pool", lambda e: e.memset(wide[:], 0.0), writes=[r_wide])
    p.op("pool", lambda e: e.memset(hid[:], 0.0), writes=[r_hid])
    p.op("pool", lambda e: e.memset(KTc[:], 0.0), writes=[r_ktc])
    p.op("pool", lambda e: e.memset(Vc[:, :, 64:65], 1.0), writes=[r_vc])
    p.dma("sp", Vc[:, :, 65:129], cst["overlap"].rearrange("(t p) c -> p t c", p=128), writes=[r_vc])
    tmp = [sb(f"ntmp{i}", [128, 64], F32) for i in range(2)]
    r_tmp = [p.region() for _ in range(2)]

    def compress(i, seg, kvh):
        r0 = FT_OFF[seg] + kvh * 64
        p.dma("sp", src[:], FT[r0:r0 + 64, col0:col0 + S], reads=[r_FT], writes=[r_src])
        sv = src[:].rearrange("p (n s) -> p n s", s=16)
        for t in range(32):
            p.op("pe", lambda e, t=t: e.matmul(cx.banks[6][:, 0:ncmp], w1[i][:, t, :], sv[:, t // 16:t // 16 + ncmp, t % 16], start=(t == 0), stop=(t == 31)),
                 reads=[r_w, r_src], writes=[cx.bank_r[6]], pe_acc=(t > 0))
        for t in range(32):
            p.op("pe", lambda e, t=t: e.matmul(cx.banks[7][:, 0:1], w1[i][:, t, :], peT[i][:, t:t + 1], start=(t == 0), stop=(t == 31)),
                 reads=[r_w], writes=[cx.bank_r[7]], pe_acc=(t > 0))
        p.op("dve", lambda e: e.tensor_copy(out=hbias[:], in_=cx.banks[7][:, 0:1]), reads=[cx.bank_r[7]], writes=[r_hb])
        p.op("act", lambda e: e.activation(out=xs[:, 0:ncmp], in_=cx.banks[6][:, 0:ncmp], func=AF.Identity, bias=hbias[:, 0:1]),
             reads=[cx.bank_r[6], r_hb], writes=[r_xs])
        p.op("dve", lambda e: e.tensor_tensor(out=u[:, 0:ncmp], in0=xs[:, 0:ncmp], in1=xs[:, 0:ncmp], op=ALU.mult), reads=[r_xs], writes=[r_u])
        p.op("dve", lambda e: e.tensor_scalar(out=u[:, 0:ncmp], in0=u[:, 0:ncmp], scalar1=0.044715, scalar2=1.0, op0=ALU.mult, op1=ALU.add), reads=[r_u], writes=[r_u])
        p.op("dve", lambda e: e.tensor_tensor(out=u[:, 0:ncmp], in0=u[:, 0:ncmp], in1=xs[:, 0:ncmp], op=ALU.mult), reads=[r_u, r_xs], writes=[r_u])
        p.op("act", lambda e: e.activation(out=u[:, 0:ncmp], in_=u[:, 0:ncmp], func=AF.Sigmoid, scale=2.0 * 0.7978845608028654), reads=[r_u], writes=[r_u])
        p.op("dve", lambda e: e.tensor_tensor(out=hid[:, 0:ncmp], in0=u[:, 0:ncmp], in1=xs[:, 0:ncmp], op=ALU.mult), reads=[r_u, r_xs], writes=[r_hid])

    for i in range(2):
        p.dma("sp", KTs[i][64:128, :], cst["nsa_koh"], writes=[r_ks[i]])
    for kvh in range(2):
        compress(0, KDC, kvh)
        p.op("pe", lambda e: e.matmul(cx.banks[6][0:64, 0:256], w2[0][:], hid[:], start=True, stop=True), reads=[r_w, r_hid], writes=[cx.bank_r[6]])
        p.op("act", lambda e: e.activation(out=KTc[:, 0:ncmp], in_=cx.banks[6][0:64, 0:ncmp], func=AF.Copy), reads=[cx.bank_r[6]], writes=[r_ktc])
        compress(1, VDC, kvh)
        for nt in range((ncmp + 127) // 128):
            p.op("pe", lambda e, nt=nt: e.matmul(cx.banks[7][:, 0:64], hid[:, nt * 128:(nt + 1) * 128], w2[1][:], start=True, stop=True),
                 reads=[r_w, r_hid], writes=[cx.bank_r[7]])
            p.op("act", lambda e, nt=nt: e.activation(out=Vc[:, nt, 0:64], in_=cx.banks[7][:, 0:64], func=AF.Copy), reads=[cx.bank_r[7]], writes=[r_vc])
        p.op("pool", lambda e: e.memset(imp[:], 0.0), writes=[r_imp])
        nct = (ncmp + 127) // 128
        for g in range(4):
            h = kvh * 4 + g
            i = nxt()
            QT, r_q = QTs[i], r_qs[i]
            load_ft(QT, (0,), QD, h, 64, r_q)

            def cmp_cb(qt, ps, r_bank, g=g, h=h):
                i = cnt[0] % 2
                cnt[0] += 1
                p.op("dve", lambda e, i=i: e.tensor_scalar(out=rden[i][:], in0=ps[:, 64:65], scalar1=1e-30, scalar2=None, op0=ALU.max), reads=[r_bank], writes=[r_rd[i]])
                p.op("dve", lambda e, i=i: e.reciprocal(out=rden[i][:], in_=rden[i][:]), reads=[r_rd[i]], writes=[r_rd[i]])
                p.op("dve", lambda e, i=i, qt=qt: e.scalar_tensor_tensor(out=imp[:, qt, :], in0=ps[:, 65:129], scalar=rden[i][:, 0:1], in1=imp[:, qt, :],
                                                                       op0=ALU.mult, op1=ALU.add), reads=[r_bank, r_rd[i], r_imp], writes=[r_imp])
                p.op("dve", lambda e, i=i: e.tensor_scalar(out=tmp[i][:], in0=ps[:, 0:64], scalar1=rden[i][:, 0:1], scalar2=None, op0=ALU.mult),
                     reads=[r_bank, r_rd[i]], writes=[r_tmp[i]])
                p.op("dve", lambda e, i=i, qt=qt: e.tensor_scalar(out=oacc[g][:, qt, :], in0=tmp[i][:], scalar1=(gsig[:, qt, h * 3:h * 3 + 1] if NSA_ONLY in (None, 0) else 0.0), scalar2=None, op0=ALU.mult),
                     reads=[r_tmp[i], r_g], writes=[r_oacc[g]])
            attn_core(p, cx, pT, r_pT, S, QT, r_q, KTc, r_ktc, 64, Vc, r_vc, 129, None, None, False,
                      lambda qc: list(range(nct)), lambda kt, qt: False, cmp_cb, cmask=cmask, r_cm=r_cm)
        for qt in range(NKT):
            p.dma("sp", selmul[:], cst["sel_mul"][qt * 128:(qt + 1) * 128, :], writes=[r_sc])
            p.dma("sp", seladd[:], cst["sel_add"][qt * 128:(qt + 1) * 128, :], writes=[r_sc])
            p.op("dve", lambda e, qt=qt: e.tensor_tensor(out=v1[:], in0=imp[:, qt, :], in1=selmul[:], op=ALU.mult), reads=[r_imp, r_sc], writes=[r_v1])
            p.op("dve", lambda e, qt=qt: e.tensor_tensor(out=v1[:], in0=v1[:], in1=seladd[:], op=ALU.add), reads=[r_v1, r_sc], writes=[r_v1])
            p.op("dve", lambda e: e.max(out=m8a[:], in_=v1[:]), reads=[r_v1], writes=[r_m8a])
            p.op("dve", lambda e: e.match_replace(out=v2[:], in_to_replace=m8a[:], in_values=v1[:], imm_value=-1e30), reads=[r_v1, r_m8a], writes=[r_v2])
            p.op("dve", lambda e: e.max(out=m8b[:], in_=v2[:]), reads=[r_v2], writes=[r_m8b])
            p.op("dve", lambda e: e.tensor_scalar(out=m8b[:, 7:8], in0=m8b[:, 7:8], scalar1=-1e29, scalar2=None, op0=ALU.max), reads=[r_m8b], writes=[r_m8b])
            p.op("dve", lambda e: e.tensor_scalar(out=v2[:], in0=v1[:], scalar1=m8b[:, 7:8], scalar2=None, op0=ALU.is_ge), reads=[r_v1, r_m8b], writes=[r_v2])
            p.op("dve", lambda e: e.tensor_scalar(out=wide[:, 64:128], in0=v2[:], scalar1=-1.0, scalar2=-NEG, op0=ALU.add, op1=ALU.mult), reads=[r_v2], writes=[r_wide])
            p.op("pe", lambda e: e.transpose(out=cx.banks[7][:, 0:128], in_=wide[:], identity=cx.ident_f[:]), reads=[r_wide, cx.r_ident], writes=[cx.bank_r[7]])
            p.op("act", lambda e, qt=qt: e.activation(out=selT[64:128, qt * 128:(qt + 1) * 128], in_=cx.banks[7][64:128, 0:128], func=AF.Copy),
                 reads=[cx.bank_r[7]], writes=[r_selT])
        for g in range(4):
            h = kvh * 4 + g
            for br in (1, 2):
                i = nxt()
                QT, KT, V, eb, r_q, r_k, r_v, r_eb = QTs[i], KTs[i], Vs[i], ebs[i], r_qs[i], r_ks[i], r_vs[i], r_ebs[i]
                load_ft(QT, (0,), QD, h, 64, r_q)
                if br == 1:
                    p.op("act", lambda e, QT=QT: e.activation(out=QT[64:128, :], in_=selT[64:128, :], func=AF.Copy), reads=[r_selT], writes=[r_q])
                    load_ft(KT, (0,), KDS, kvh, 64, r_k)
                    load_v(V, VDS, kvh, r_v)
                    load_eb(24 + h, eb, r_eb)
                    kfn = lambda qc: list(range(0, min(NKT, qc * 4 + 4)))
                    Kc, clamp = 128, True
                else:
                    load_ft(KT, (0,), KDW, kvh, 64, r_k)
                    load_v(V, VDW, kvh, r_v)
                    load_eb(32 + h, eb, r_eb)
                    kfn = lambda qc: list(range(max(0, qc * 4 - 4), min(NKT, qc * 4 + 4)))
                    Kc, clamp = 64, False

                def br_cb(qt, ps, r_bank, g=g, h=h, br=br):
                    if NSA_ONLY is not None and NSA_ONLY != br:
                        return
                    i = cnt[0] % 2
                    cnt[0] += 1
                    p.op("dve", lambda e, i=i: e.reciprocal(out=rden[i][:], in_=ps[:, 64:65]), reads=[r_bank], writes=[r_rd[i]])
                    p.op("dve", lambda e, i=i: e.tensor_scalar(out=tmp[i][:], in0=ps[:, 0:64], scalar1=rden[i][:, 0:1], scalar2=None, op0=ALU.mult),
                         reads=[r_bank, r_rd[i]], writes=[r_tmp[i]])
                    p.op("dve", lambda e, i=i, qt=qt: e.scalar_tensor_tensor(out=oacc[g][:, qt, :], in0=tmp[i][:], scalar=gsig[:, qt, h * 3 + br:h * 3 + br + 1],
                                                                           in1=oacc[g][:, qt, :], op0=ALU.mult, op1=ALU.add),
                         reads=[r_tmp[i], r_g, r_oacc[g]], writes=[r_oacc[g]])
                attn_core(p, cx, pT, r_pT, S, QT, r_q, KT, r_k, Kc, V, r_v, 65, eb, r_eb, clamp, kfn, causal_skip, br_cb)
            c0 = 3 * 512 + h * 64
            p.dma("pool", OD[col0:col0 + S, c0:c0 + 64].rearrange("(t p) c -> p t c", p=128), oacc[g][:], reads=[r_oacc[g]], writes=[r_OD], store=True)


def make_consts(S, rel_bias):
    cst = {}
    j = np.arange(128)[:, None]
    m = np.arange(W_STRIP)[None, :]
    d = m - OFF_STRIP - j
    dd = np.maximum(d, 0)
    bucket = t5_bucket_np(dd)
    strips = np.ascontiguousarray(np.transpose(rel_bias[bucket], (2, 0, 1))).astype(np.float32)
    causal = (d >= 0)
    multA = ((d >= 0) & (d <= 128)).astype(np.float32) + ((d >= 0) & (d % 4 == 0) & (d <= 512)) + ((d >= 0) & (d % 16 == 0) & (d <= 2048))
    mults = np.stack([multA, (causal & (d <= 127)), causal, (causal & (d <= 511))]).astype(np.float32)
    cst["mults"] = mults
    nblk = 16
    cur = np.arange(16)[:, None]
    n = np.arange(16)[None, :]
    cst["moba_valid"] = np.where(n < cur, 0.0, -1e30).astype(np.float32).reshape(-1)
    cst["moba_own"] = (n == cur).astype(np.float32).reshape(-1)
    import ml_dtypes
    bf = ml_dtypes.bfloat16
    key = np.arange(S)[None, :]
    cst["moba_koh"] = (key // 256 == np.arange(16)[:, None]).astype(bf)
    cst["nsa_koh"] = (key // 64 == np.arange(64)[:, None]).astype(bf)
    ncmp = (S - 32) // 16 + 1
    nn = np.arange(256)[:, None]
    cst["cmp_mask"] = ((nn * 16 + 31 <= key) & (nn < ncmp)).astype(bf)
    c_start = nn * 16
    j_start = np.arange(64)[None, :] * 64
    cst["overlap"] = ((c_start < j_start + 64) & (c_start + 32 > j_start) & (nn < ncmp)).astype(bf)
    pos = np.arange(S)[:, None]
    curq = pos // 64
    jj = np.arange(64)[None, :]
    forced = (jj == 0) | (jj == curq) | (jj == curq - 1)
    valid = jj <= curq
    cst["sel_mul"] = (valid & ~forced).astype(np.float32)
    cst["sel_add"] = np.where(forced, 1e30, np.where(valid, 0.0, -1e30)).astype(np.float32)
    return cst, strips


def stage_mix_out(p, cx, OD, x_src, x_dst, mixg_row, w_out_l, modD, l, NB, S, r_OD, r_x):
    with contextlib.ExitStack() as st:
        def sb(name, shape, dt):
            return st.enter_context(p.sbt("E" + name, list(shape), dt))
        wo = sb("wo", [128, KC, D], BF16)
        r_wo = p.region()
        for k4 in range(4):
            p.dma("pool", wo[:, k4 * 4:(k4 + 1) * 4, :], w_out_l[k4 * 512:(k4 + 1) * 512, :].rearrange("(k p) c -> p k c", p=128), writes=[r_wo])
        mg = sb("mg", [128, KC], F32)
        r_mg = p.region()
        load_cols(p, mg, r_mg, mixg_row)
        g1b = sb("g1b", [128, D], F32)
        r_g1 = p.region()
        ot = [sb(f"ot{i}", [128, D], F32) for i in range(2)]
        r_ot = [p.region() for _ in range(2)]
        junk = sb("junk", [128, 512], BF16)
        r_junk = p.region()
        ss = [sb(f"ss{i}", [128, 4], F32) for i in range(2)]
        r_ss = [p.region() for _ in range(2)]
        on = sb("on", [128, D], F32)
        r_on = p.region()
        cT = [sb(f"cT{i}", [128, KC, 128], BF16) for i in range(2)]
        r_cT = [p.region() for _ in range(2)]
        xt = [sb(f"xt{i}", [128, D], F32) for i in range(2)]
        r_xt = [p.region() for _ in range(2)]
        x1 = [sb(f"x1{i}", [128, D], F32) for i in range(2)]
        r_x1 = [p.region() for _ in range(2)]
        it = 0
        for b in range(NB):
            p.dma("sp", g1b[:], bcast_rows(modD[l, b, 2 * D:3 * D], 128), reads=[cx.r_modD], writes=[r_g1])
            for tt in range(S // 128):
                i = it % 2
                it += 1
                t0 = b * S + tt * 128
                p.dma("sp", ot[i][:], OD[t0:t0 + 128, :], reads=[r_OD], writes=[r_ot[i]])
                p.dma("sp", xt[i][:], x_src[t0:t0 + 128, :], reads=[r_x], writes=[r_xt[i]])
                for m in range(4):
                    p.op("act", lambda e, i=i, m=m: e.activation(out=junk[:], in_=ot[i][:, m * 512:(m + 1) * 512], func=AF.Square, accum_out=ss[i][:, m:m + 1]),
                         reads=[r_ot[i]], writes=[r_junk, r_ss[i]])
                p.op("dve", lambda e, i=i: e.tensor_scalar(out=ss[i][:], in0=ss[i][:], scalar1=1.0 / 512, scalar2=1e-6, op0=ALU.mult, op1=ALU.add),
                     reads=[r_ss[i]], writes=[r_ss[i]])
                p.op("act", lambda e, i=i: e.activation(out=ss[i][:], in_=ss[i][:], func=AF.Sqrt), reads=[r_ss[i]], writes=[r_ss[i]])
                p.op("dve", lambda e, i=i: e.reciprocal(out=ss[i][:], in_=ss[i][:]), reads=[r_ss[i]], writes=[r_ss[i]])
                for m in range(4):
                    p.op("dve", lambda e, i=i, m=m: e.tensor_scalar(out=on[:, m * 512:(m + 1) * 512], in0=ot[i][:, m * 512:(m + 1) * 512],
                                                                  scalar1=ss[i][:, m:m + 1], scalar2=None, op0=ALU.mult),
                         reads=[r_ot[i], r_ss[i]], writes=[r_on])
                for j4 in range(4):
                    bk = 4 + j4
                    for jj in range(4):
                        j = j4 * 4 + jj
                        p.op("pe", lambda e, j=j, jj=jj, bk=bk: e.transpose(out=cx.banks[bk][:, jj * 128:(jj + 1) * 128], in_=on[:, j * 128:(j + 1) * 128],
                                                                          identity=cx.ident_f[:]),
                             reads=[r_on, cx.r_ident], writes=[cx.bank_r[bk]], pe_acc=(jj > 0))
                    for jj in range(4):
                        j = j4 * 4 + jj
                        p.op("act", lambda e, j=j, jj=jj, bk=bk, i=i: e.activation(out=cT[i][:, j, :], in_=cx.banks[bk][:, jj * 128:(jj + 1) * 128],
                                                                             func=AF.Copy, scale=mg[:, j:j + 1]),
                             reads=[cx.bank_r[bk], r_mg], writes=[r_cT[i]])
                for n in range(4):
                    for k in range(KC):
                        p.op("pe", lambda e, n=n, k=k, i=i: e.matmul(cx.banks[n][:], cT[i][:, k, :], wo[:, k, n * 512:(n + 1) * 512], start=(k == 0), stop=(k == KC - 1)),
                             reads=[r_cT[i], r_wo], writes=[cx.bank_r[n]], pe_acc=(k > 0))
                    p.op("dve", lambda e, n=n, i=i: e.tensor_tensor(out=x1[i][:, n * 512:(n + 1) * 512], in0=cx.banks[n][:], in1=g1b[:, n * 512:(n + 1) * 512], op=ALU.mult),
                         reads=[cx.bank_r[n], r_g1], writes=[r_x1[i]])
                    p.op("dve", lambda e, n=n, i=i: e.tensor_tensor(out=x1[i][:, n * 512:(n + 1) * 512], in0=x1[i][:, n * 512:(n + 1) * 512],
                                                                  in1=xt[i][:, n * 512:(n + 1) * 512], op=ALU.add),
                         reads=[r_x1[i], r_xt[i]], writes=[r_x1[i]])
                p.dma("pool", x_dst[t0:t0 + 128, :], x1[i][:], reads=[r_x1[i]], writes=[r_x], store=True)
        p.barrier()


def stage_moe(p, cx, h2T, NT, router_w, router_bias, wg_l, wu_l, wd_l, yacc, r_h2T, r_y):
    NTL = NT // 128
    with contextlib.ExitStack() as st:
        def sb(name, shape, dt):
            return st.enter_context(p.sbt("M" + name, list(shape), dt))
        rwb = sb("rwb", [128, KC, 16], BF16)
        r_rw = p.region()
        p.dma("pool", rwb[:], router_w.rearrange("(k p) e -> p k e", p=128), writes=[r_rw])
        rb = sb("rb", [128, 16], F32)
        p.dma("sp", rb[:], bcast_rows(router_bias, 128), writes=[r_rw])
        comb = sb("comb", [128, NTL, 16], F32)
        r_comb = p.region()
        hb = [sb(f"hb{i}", [128, KC, 512], BF16) for i in range(2)]
        r_hb = [p.region() for _ in range(2)]
        aff = sb("aff", [128, 16], F32)
        sc = sb("sc", [128, 16], F32)
        t16 = sb("t16", [128, 16], F32)
        m1 = sb("m1", [128, 4], F32)
        m2 = sb("m2", [128, 4], F32)
        gm = sb("gm", [128, 1], F32)
        e1 = sb("e1", [128, 1], F32)
        r_aff, r_sc, r_t16, r_m1, r_m2, r_gm, r_e1 = (p.region() for _ in range(7))
        ib = 0

        def v3(t):
            return t[:].rearrange("p (g k) -> p g k", k=4)
        for blk in range(NT // 512):
            i = ib % 2
            ib += 1
            p.dma("sp", hb[i][:], h2T[:, blk * 512:(blk + 1) * 512].rearrange("(k p) t -> p k t", p=128), reads=[r_h2T], writes=[r_hb[i]])
            for tt in range(4):
                tl = blk * 4 + tt
                for k in range(KC):
                    p.op("pe", lambda e, k=k, i=i, tt=tt: e.matmul(cx.banks[6][:, 0:16], hb[i][:, k, tt * 128:(tt + 1) * 128], rwb[:, k, :], start=(k == 0), stop=(k == KC - 1)),
                         reads=[r_hb[i], r_rw], writes=[cx.bank_r[6]], pe_acc=(k > 0))
                p.op("act", lambda e: e.activation(out=aff[:], in_=cx.banks[6][:, 0:16], func=AF.Sigmoid), reads=[cx.bank_r[6]], writes=[r_aff])
                p.op("dve", lambda e: e.tensor_tensor(out=sc[:], in0=aff[:], in1=rb[:], op=ALU.add), reads=[r_aff, r_rw], writes=[r_sc])
                p.op("dve", lambda e: e.tensor_reduce(out=m1[:], in_=v3(sc), axis=AX.X, op=ALU.max), reads=[r_sc], writes=[r_m1])
                p.op("dve", lambda e: e.tensor_tensor(out=v3(t16), in0=v3(sc), in1=m1[:].unsqueeze(2).to_broadcast([128, 4, 4]), op=ALU.is_equal),
                     reads=[r_sc, r_m1], writes=[r_t16])
                p.op("dve", lambda e: e.scalar_tensor_tensor(out=t16[:], in0=t16[:], scalar=-1e30, in1=sc[:], op0=ALU.mult, op1=ALU.add),
                     reads=[r_t16, r_sc], writes=[r_t16])
                p.op("dve", lambda e: e.tensor_reduce(out=m2[:], in_=v3(t16), axis=AX.X, op=ALU.max), reads=[r_t16], writes=[r_m2])
                p.op("dve", lambda e: e.tensor_tensor(out=m1[:], in0=m1[:], in1=m2[:], op=ALU.add), reads=[r_m1, r_m2], writes=[r_m1])
                p.op("dve", lambda e: e.tensor_reduce(out=gm[:], in_=m1[:], axis=AX.X, op=ALU.max), reads=[r_m1], writes=[r_gm])
                p.op("dve", lambda e: e.tensor_scalar(out=m2[:], in0=m1[:], scalar1=gm[:, 0:1], scalar2=None, op0=ALU.is_ge), reads=[r_m1, r_gm], writes=[r_m2])
                p.op("dve", lambda e: e.tensor_scalar(out=m2[:], in0=m2[:], scalar1=-1.0, scalar2=1e30, op0=ALU.add, op1=ALU.mult), reads=[r_m2], writes=[r_m2])
                p.op("dve", lambda e: e.tensor_tensor(out=v3(t16), in0=v3(sc), in1=m2[:].unsqueeze(2).to_broadcast([128, 4, 4]), op=ALU.add),
                     reads=[r_sc, r_m2], writes=[r_t16])
                p.op("dve", lambda e: e.tensor_reduce(out=e1[:], in_=t16[:], axis=AX.X, op=ALU.max), reads=[r_t16], writes=[r_e1])
                p.op("dve", lambda e: e.tensor_scalar(out=sc[:], in0=t16[:], scalar1=e1[:, 0:1], scalar2=None, op0=ALU.is_equal), reads=[r_t16, r_e1], writes=[r_sc])
                p.op("dve", lambda e: e.scalar_tensor_tensor(out=t16[:], in0=sc[:], scalar=-1e30, in1=t16[:], op0=ALU.mult, op1=ALU.add),
                     reads=[r_sc, r_t16], writes=[r_t16])
                p.op("dve", lambda e: e.tensor_reduce(out=e1[:], in_=t16[:], axis=AX.X, op=ALU.max), reads=[r_t16], writes=[r_e1])
                p.op("dve", lambda e: e.tensor_scalar(out=t16[:], in0=t16[:], scalar1=e1[:, 0:1], scalar2=None, op0=ALU.is_equal), reads=[r_t16, r_e1], writes=[r_t16])
                p.op("dve", lambda e: e.tensor_tensor(out=sc[:], in0=sc[:], in1=t16[:], op=ALU.add), reads=[r_sc, r_t16], writes=[r_sc])
                p.op("dve", lambda e: e.tensor_tensor(out=sc[:], in0=sc[:], in1=aff[:], op=ALU.mult), reads=[r_sc, r_aff], writes=[r_sc])
                p.op("dve", lambda e: e.tensor_reduce(out=e1[:], in_=sc[:], axis=AX.X, op=ALU.add), reads=[r_sc], writes=[r_e1])
                p.op("dve", lambda e: e.reciprocal(out=e1[:], in_=e1[:]), reads=[r_e1], writes=[r_e1])
                p.op("dve", lambda e, tl=tl: e.tensor_scalar(out=comb[:, tl, :], in0=sc[:], scalar1=e1[:, 0:1], scalar2=None, op0=ALU.mult),
                     reads=[r_sc, r_e1], writes=[r_comb])
        wgu = [sb(f"wgu{i}", [128, KC, 1024], BF16) for i in range(2)]
        wd = [sb(f"wd{i}", [128, 4, D], BF16) for i in range(2)]
        r_wgu = [p.region() for _ in range(2)]
        r_wd = [p.region() for _ in range(2)]
        sg = [sb(f"sg{i}", [128, 512], F32) for i in range(2)]
        he = [sb(f"he{i}", [128, 512], F32) for i in range(2)]
        heT = [sb(f"heT{i}", [128, 4, 128], BF16) for i in range(2)]
        yt = [sb(f"yt{i}", [128, D], F32) for i in range(2)]
        r_sg = [p.region() for _ in range(2)]
        r_he = [p.region() for _ in range(2)]
        r_heT = [p.region() for _ in range(2)]
        r_yt = [p.region() for _ in range(2)]
        def load_w(ex):
            w = ex % 2
            p.dma("pool", wgu[w][:, :, 0:512], wg_l[ex].rearrange("(k p) f -> p k f", p=128), writes=[r_wgu[w]])
            p.dma("pool", wgu[w][:, :, 512:1024], wu_l[ex].rearrange("(k p) f -> p k f", p=128), writes=[r_wgu[w]])
            p.dma("pool", wd[w][:], wd_l[ex].rearrange("(k p) c -> p k c", p=128), writes=[r_wd[w]])

        steps = [(ex, blk, tt) for ex in range(16) for blk in range(NT // 512) for tt in range(4)]
        hb_of = {}

        def gate_up(n):
            ex, blk, tt = steps[n]
            w = ex % 2
            if tt == 0:
                i = (ib0[0]) % 2
                ib0[0] += 1
                hb_of[(ex, blk)] = i
                p.dma("sp", hb[i][:], h2T[:, blk * 512:(blk + 1) * 512].rearrange("(k p) t -> p k t", p=128), reads=[r_h2T], writes=[r_hb[i]])
            i = hb_of[(ex, blk)]
            pr = 2 * (n % 2)
            for half in range(2):
                for k in range(KC):
                    p.op("pe", lambda e, k=k, i=i, tt=tt, w=w, half=half, pr=pr: e.matmul(
                        cx.banks[pr + half][:], hb[i][:, k, tt * 128:(tt + 1) * 128], wgu[w][:, k, half * 512:(half + 1) * 512], start=(k == 0), stop=(k == KC - 1)),
                        reads=[r_hb[i], r_wgu[w]], writes=[cx.bank_r[pr + half]], pe_acc=(k > 0))
        ib0 = [ib]
        load_w(0)
        gate_up(0)
        for n, (ex, blk, tt) in enumerate(steps):
            w = ex % 2
            if blk == 0 and tt == 0 and ex + 1 < 16:
                load_w(ex + 1)
            if n + 1 < len(steps):
                gate_up(n + 1)
            tl = blk * 4 + tt
            o = n % 2
            pr = 2 * (n % 2)
            p.op("act", lambda e, o=o, pr=pr: e.activation(out=sg[o][:], in_=cx.banks[pr][:], func=AF.Silu), reads=[cx.bank_r[pr]], writes=[r_sg[o]])
            p.op("dve", lambda e, o=o, tl=tl, ex=ex, pr=pr: e.scalar_tensor_tensor(out=he[o][:], in0=cx.banks[pr + 1][:], scalar=comb[:, tl, ex:ex + 1], in1=sg[o][:],
                                                                             op0=ALU.mult, op1=ALU.mult),
                 reads=[cx.bank_r[pr + 1], r_comb, r_sg[o]], writes=[r_he[o]])
            bt = pr
            for kk in range(4):
                p.op("pe", lambda e, kk=kk, o=o, bt=bt: e.transpose(out=cx.banks[bt][:, kk * 128:(kk + 1) * 128], in_=he[o][:, kk * 128:(kk + 1) * 128],
                                                                  identity=cx.ident_f[:]),
                     reads=[r_he[o], cx.r_ident], writes=[cx.bank_r[bt]], pe_acc=(kk > 0))
            p.op("act", lambda e, o=o, bt=bt: e.activation(out=heT[o][:].rearrange("p k t -> p (k t)"), in_=cx.banks[bt][:], func=AF.Copy),
                 reads=[cx.bank_r[bt]], writes=[r_heT[o]])
            for nn in range(4):
                for kk in range(4):
                    p.op("pe", lambda e, nn=nn, kk=kk, o=o, w=w: e.matmul(cx.banks[4 + nn][:], heT[o][:, kk, :], wd[w][:, kk, nn * 512:(nn + 1) * 512],
                                                                        start=(kk == 0), stop=(kk == 3)),
                         reads=[r_heT[o], r_wd[w]], writes=[cx.bank_r[4 + nn]], pe_acc=(kk > 0))
                if nn % 2 == 0:
                    p.op("act", lambda e, nn=nn, o=o: e.activation(out=yt[o][:, nn * 512:(nn + 1) * 512], in_=cx.banks[4 + nn][:], func=AF.Copy),
                         reads=[cx.bank_r[4 + nn]], writes=[r_yt[o]])
                else:
                    p.op("dve", lambda e, nn=nn, o=o: e.tensor_copy(out=yt[o][:, nn * 512:(nn + 1) * 512], in_=cx.banks[4 + nn][:]),
                         reads=[cx.bank_r[4 + nn]], writes=[r_yt[o]])
            if ex == 0:
                p.dma("pool", yacc[tl * 128:(tl + 1) * 128, :], yt[o][:], reads=[r_yt[o]], writes=[r_y], store=True)
            else:
                p.dma("pool", yacc[tl * 128:(tl + 1) * 128, :], yt[o][:], reads=[r_yt[o]], writes=[r_y], accum_op=ALU.add, store=True)
        p.barrier()


def stage_resid(p, cx, xs, yacc, modD, l, goff, NB, S, r_x, r_y, final_g=None, out=None, r_out=None):
    with contextlib.ExitStack() as st:
        def sb(name, shape, dt):
            return st.enter_context(p.sbt("R" + name, list(shape), dt))
        gb = sb("gb", [128, D], F32)
        r_gb = p.region()
        fg = sb("fg", [128, D], F32)
        r_fg = p.region()
        if final_g is not None:
            p.dma("sp", fg[:], bcast_rows(final_g, 128), writes=[r_fg])
        xt = [sb(f"xt{i}", [128, D], F32) for i in range(2)]
        yt = [sb(f"yt{i}", [128, D], F32) for i in range(2)]
        r_xt = [p.region() for _ in range(2)]
        r_yt = [p.region() for _ in range(2)]
        junk = sb("junk", [128, D], BF16)
        r_junk = p.region()
        ss = [sb(f"ss{i}", [128, 1], F32) for i in range(2)]
        r_ss = [p.region() for _ in range(2)]
        it = 0
        for b in range(NB):
            p.dma("sp", gb[:], bcast_rows(modD[l, b, goff:goff + D], 128), reads=[cx.r_modD], writes=[r_gb])
            for tt in range(S // 128):
                i = it % 2
                it += 1
                t0 = b * S + tt * 128
                p.dma("sp", xt[i][:], xs[t0:t0 + 128, :], reads=[r_x], writes=[r_xt[i]])
                p.dma("sp", yt[i][:], yacc[t0:t0 + 128, :], reads=[r_y], writes=[r_yt[i]])
                p.op("dve", lambda e, i=i: e.tensor_tensor(out=yt[i][:], in0=yt[i][:], in1=gb[:], op=ALU.mult), reads=[r_yt[i], r_gb], writes=[r_yt[i]])
                p.op("dve", lambda e, i=i: e.tensor_tensor(out=xt[i][:], in0=xt[i][:], in1=yt[i][:], op=ALU.add), reads=[r_yt[i], r_xt[i]], writes=[r_xt[i]])
                if final_g is None:
                    p.dma("pool", xs[t0:t0 + 128, :], xt[i][:], reads=[r_xt[i]], writes=[r_x], store=True)
                else:
                    p.op("act", lambda e, i=i: e.activation(out=junk[:], in_=xt[i][:], func=AF.Square, accum_out=ss[i][:]),
                         reads=[r_xt[i]], writes=[r_junk, r_ss[i]])
                    p.op("dve", lambda e, i=i: e.tensor_scalar(out=ss[i][:], in0=ss[i][:], scalar1=1.0 / D, scalar2=1e-6, op0=ALU.mult, op1=ALU.add),
                         reads=[r_ss[i]], writes=[r_ss[i]])
                    p.op("act", lambda e, i=i: e.activation(out=ss[i][:], in_=ss[i][:], func=AF.Sqrt), reads=[r_ss[i]], writes=[r_ss[i]])
                    p.op("dve", lambda e, i=i: e.reciprocal(out=ss[i][:], in_=ss[i][:]), reads=[r_ss[i]], writes=[r_ss[i]])
                    p.op("dve", lambda e, i=i: e.scalar_tensor_tensor(out=yt[i][:], in0=xt[i][:], scalar=ss[i][:, 0:1], in1=fg[:], op0=ALU.mult, op1=ALU.mult),
                         reads=[r_xt[i], r_ss[i], r_fg], writes=[r_yt[i]])
                    p.dma("pool", out[t0:t0 + 128, :], yt[i][:], reads=[r_yt[i]], writes=[r_out], store=True)
        p.barrier()


CONST_SPECS = None


def build_program(NB, S, NL):
    NT = NB * S
    nc = bass.Bass("TRN2", target_bir_lowering=False)
    p = Prog(nc)

    def ext(name, shape, dt=F32):
        return nc.dram_tensor(name, list(shape), dt, kind="ExternalInput").ap()

    def internal(name, shape, dt=F32):
        return nc.dram_tensor(name, list(shape), dt, kind="Internal").ap()
    consts = ext("consts", [128, 128])
    x = ext("x", [NT, D])
    c = ext("c", [NB, D])
    strips = ext("strips", [32, 128, W_STRIP])
    cst = {"mults": ext("mults", [4, 128, W_STRIP]), "moba_valid": ext("moba_valid", [256]), "moba_own": ext("moba_own", [256]),
           "moba_koh": ext("moba_koh", [16, S], BF16), "nsa_koh": ext("nsa_koh", [64, S], BF16), "cmp_mask": ext("cmp_mask", [256, S], BF16),
           "overlap": ext("overlap", [256, 64], BF16), "sel_mul": ext("sel_mul", [S, 64]), "sel_add": ext("sel_add", [S, 64])}
    router_w = ext("router_w", [D, 16])
    router_bias = ext("router_bias", [16])
    norm1_g = ext("norm1_g", [NL, D])
    norm2_g = ext("norm2_g", [NL, D])
    ada_w = ext("ada_w", [NL, D, 6 * D])
    ada_b = ext("ada_b", [NL, 6 * D])
    w_in = ext("w_in", [NL, D, 5144])
    pek = ext("nsa_pe_k", [NL, 32, 64])
    pev = ext("nsa_pe_v", [NL, 32, 64])
    w1k = ext("nsa_cmp_w1_k", [NL, 2048, 128])
    w2k = ext("nsa_cmp_w2_k", [NL, 128, 64])
    w1v = ext("nsa_cmp_w1_v", [NL, 2048, 128])
    w2v = ext("nsa_cmp_w2_v", [NL, 128, 64])
    sinks = ext("sinks", [NL, 8])
    mixg = ext("mix_norm_g", [NL, D])
    w_out = ext("w_out", [NL, D, D])
    wg = ext("exp_w_gate", [NL, 16, D, 512])
    wu = ext("exp_w_up", [NL, 16, D, 512])
    wd = ext("exp_w_down", [NL, 16, 512, D])
    final_g = ext("final_g", [D])
    y = nc.dram_tensor("y", [NT, D], F32, kind="ExternalOutput").ap()
    modD = internal("modD", [NL, NB, 6 * D])
    xs = internal("xs", [NT, D])
    hT = internal("hT", [D, NT], BF16)
    FT = internal("FT", [FT_COLS, NT], BF16)
    TM = internal("TM", [NT, TM_COLS], BF16)
    OD = internal("OD", [NT, D])
    yacc = internal("yacc", [NT, D])
    EBD = internal("EBD", [40, 128, W_STRIP], BF16)
    cx = setup_ctx(p, consts)
    cx.r_modD = p.region("modD")
    r_x, r_hT, r_FT, r_TM, r_OD, r_y, r_out = (p.region() for _ in range(7))
    stage_mod(p, cx, c, ada_w, ada_b, modD, NL, NB)
    stage_eb(p, cx, strips, cst, EBD, p.region())
    for l in range(NL):
        x_src = x if l == 0 else xs
        for b in range(NB):
            stage_norm_T(p, cx, x_src, b * S, S, norm1_g[l:l + 1, :], modD[l, b:b + 1, D:2 * D], modD[l, b:b + 1, 0:D], hT, b * S, "A", r_x, r_hT)
        if 'proj' not in SKIP:
            stage_proj(p, cx, w_in[l], hT, FT, TM, NT, r_hT, r_FT, r_TM)
        for b in range(NB if 'attn' not in SKIP else 0):
            stage_attn(p, cx, cst, FT, TM, OD, S, b, EBD, sinks[l], (w1k[l], w2k[l], w1v[l], w2v[l], pek[l], pev[l]), r_FT, r_TM, r_OD)
        if 'mix' not in SKIP:
          stage_mix_out(p, cx, OD, x_src, xs, mixg[l:l + 1, :], w_out[l], modD, l, NB, S, r_OD, r_x)
        for b in range(NB):
            stage_norm_T(p, cx, xs, b * S, S, norm2_g[l:l + 1, :], modD[l, b:b + 1, 4 * D:5 * D], modD[l, b:b + 1, 3 * D:4 * D], hT, b * S, "N", r_x, r_hT)
        if 'moe' not in SKIP:
          stage_moe(p, cx, hT, NT, router_w, router_bias, wg[l], wu[l], wd[l], yacc, r_hT, r_y)
        last = (l == NL - 1)
        stage_resid(p, cx, xs, yacc, modD, l, 5 * D, NB, S, r_x, r_y, final_g=(final_g if last else None), out=y, r_out=r_out)
    p.finish()
    return nc, p


N_CORES = 4
SKIP = set()


def kernel(**inputs):
    import ml_dtypes
    B, S, _ = inputs["x"].shape
    NL = inputs["ada_w"].shape[0]
    NB = B // N_CORES
    nc, prog = build_program(NB, S, NL)
    f32 = lambda a: np.ascontiguousarray(np.asarray(a), dtype=np.float32)
    cst, strips = make_consts(S, f32(inputs["rel_bias"]))
    shared = {"consts": np.eye(128, dtype=np.float32), "strips": strips, **cst}
    for k in ("router_w", "router_bias", "norm1_g", "norm2_g", "ada_w", "ada_b", "w_in", "nsa_pe_k", "nsa_pe_v", "nsa_cmp_w1_k", "nsa_cmp_w2_k",
              "nsa_cmp_w1_v", "nsa_cmp_w2_v", "sinks", "w_out", "exp_w_gate", "exp_w_up", "exp_w_down", "final_g"):
        shared[k] = f32(inputs[k])
    shared["mix_norm_g"] = f32(inputs["mix_norm_g"]).reshape(NL, D)
    xin = f32(inputs["x"])
    cin = f32(inputs["c"])
    in_maps = []
    for ci in range(N_CORES):
        m = dict(shared)
        m["x"] = xin[ci * NB:(ci + 1) * NB].reshape(NB * S, D)
        m["c"] = cin[ci * NB:(ci + 1) * NB]
        in_maps.append(m)
    res = run_bass_kernel_spmd(nc, in_maps, core_ids=list(range(N_CORES)))
    out = np.concatenate([res.results[ci]["y"].reshape(NB, S, D) for ci in range(N_CORES)], axis=0)
    return out.astype(np.float32)
```

```python
import contextlib
import math
import numpy as np
import concourse.bass as bass
import concourse.mybir as mybir
from concourse.bass_utils import run_bass_kernel_spmd

F32 = mybir.dt.float32
BF16 = mybir.dt.bfloat16
AF = mybir.ActivationFunctionType
ALU = mybir.AluOpType
AX = mybir.AxisListType

D = 2048
KC = D // 128
HD = 64
NEG = -30000.0
IN_SIZES = (512, 512, 512, 512, 128, 128, 512, 512, 512, 512, 128, 128, 128, 128, 128, 128, 24)
IN_OFFS = [0] + list(np.cumsum(IN_SIZES))
(QA, KA, VA, QB, KB_, VB, QC, KC_, VC, QD, KDC, VDC, KDS, VDS, KDW, VDW, GD) = range(17)
FT_ORDER = [QA, KA, QB, KB_, QC, KC_, QD, KDC, VDC, KDS, KDW]
TM_ORDER = [VA, VB, VC, VDS, VDW, GD]
FT_COLS = sum(IN_SIZES[i] for i in FT_ORDER)
TM_COLS = sum(IN_SIZES[i] for i in TM_ORDER)
FT_OFF = {}
_o = 0
for _i in FT_ORDER:
    FT_OFF[_i] = _o
    _o += IN_SIZES[_i]
TM_OFF = {}
_o = 0
for _i in TM_ORDER:
    TM_OFF[_i] = _o
    _o += IN_SIZES[_i]


class Region:
    __slots__ = ("name", "last_w", "readers")

    def __init__(self, name):
        self.name = name
        self.last_w = None
        self.readers = {}


class Prog:
    ENG = ("pe", "act", "dve", "pool", "sp")
    NDMA = 88

    def __init__(self, nc):
        self.nc = nc
        self.ops = {e: [] for e in self.ENG}
        self.cnt = {e: 0 for e in self.ENG}
        self.seen = {e: {} for e in self.ENG}
        self.dma_cnt = [0] * self.NDMA
        self.dma_rr = 0
        self.stack = contextlib.ExitStack()
        self.nreg = 0
        self.out_events = []

    def sbt(self, name, shape, dtype):
        self.nuniq = getattr(self, "nuniq", 0) + 1
        return self.nc.sbuf_tensor(f"{name}_{self.nuniq}", list(shape), dtype)

    def region(self, name=None):
        self.nreg += 1
        return Region(name or f"r{self.nreg}")

    def sb(self, name, shape, dtype):
        t = self.stack.enter_context(self.nc.sbuf_tensor(name, list(shape), dtype))
        return t

    def ps(self, name, shape, dtype=F32):
        return self.stack.enter_context(self.nc.psum_tensor(name, list(shape), dtype))

    def dram(self, name, shape, dtype, kind="Internal"):
        return self.nc.dram_tensor(name, list(shape), dtype, kind=kind).ap()

    def _deps(self, reads, writes, pe_acc=False, eng=None):
        deps = {}

        def add(ev):
            if ev is None:
                return
            k, v = ev
            if deps.get(k, 0) < v:
                deps[k] = v
        for r in reads:
            add(r.last_w)
        for w in writes:
            if not (pe_acc and w.last_w is not None and w.last_w[0] == eng):
                add(w.last_w)
            for k, v in w.readers.items():
                add((k, v))
        return deps

    def _commit(self, ev, reads, writes):
        for r in reads:
            k, v = ev
            if r.readers.get(k, 0) < v:
                r.readers[k] = v
        for w in writes:
            w.last_w = ev
            w.readers = {}

    def _waits(self, eng, deps):
        waits = []
        for k, v in deps.items():
            if self.seen[eng].get(k, 0) < v:
                self.seen[eng][k] = v
                waits.append((k, v))
        return waits

    def op(self, eng, fn, reads=(), writes=(), pe_acc=False):
        deps = self._deps(reads, writes, pe_acc, eng)
        waits = self._waits(eng, deps)
        self.cnt[eng] += 1
        ev = (eng, self.cnt[eng])
        self.ops[eng].append((waits, fn, (eng, 1)))
        self._commit(ev, reads, writes)
        return ev

    def dma(self, q, out, in_, reads=(), writes=(), store=False, **kw):
        if store:
            deps = self._deps(reads, ())
            for w in writes:
                for k, v in w.readers.items():
                    if deps.get(k, 0) < v:
                        deps[k] = v
            reg = reads[0]
        else:
            deps = self._deps(reads, writes)
            reg = writes[0]
        waits = self._waits(q, deps)
        if not hasattr(self, "reg_sem"):
            self.reg_sem = {}
        if id(reg) not in self.reg_sem:
            self.reg_sem[id(reg)] = len(self.reg_sem) % self.NDMA
        j = self.reg_sem[id(reg)]
        self.dma_cnt[j] += 16
        ev = (("dma", j), self.dma_cnt[j])
        self.ops[q].append((waits, lambda e: e.dma_start(out=out, in_=in_, **kw), (("dma", j), 16)))
        self._commit(ev, reads, writes)
        return ev

    def barrier(self):
        evs = {}
        for e in self.ENG:
            if self.cnt[e]:
                evs[e] = self.cnt[e]
        for j in range(self.NDMA):
            if self.dma_cnt[j]:
                evs[("dma", j)] = self.dma_cnt[j]
        for e in self.ENG:
            waits = self._waits(e, {k: v for k, v in evs.items() if k != e})
            if waits:
                self.ops[e].append((waits, None, None))

    def finish(self):
        nc = self.nc
        sems = {}
        for e in self.ENG:
            sems[e] = self.stack.enter_context(nc.semaphore("sem_" + e))
        for j in range(self.NDMA):
            sems[("dma", j)] = self.stack.enter_context(nc.semaphore(f"sem_dma{j}"))
        final = []
        for e in self.ENG:
            if self.cnt[e] and e != "sp":
                final.append((e, self.cnt[e]))
        for j in range(self.NDMA):
            if self.dma_cnt[j]:
                final.append((("dma", j), self.dma_cnt[j]))
        ops = self.ops
        with nc.Block() as block:
            def run(engobj, name):
                for waits, fn, inc in ops[name]:
                    for k, v in waits:
                        engobj.wait_ge(sems[k], v)
                    if fn is None:
                        continue
                    ins = fn(engobj)
                    ins.then_inc(sems[inc[0]], inc[1])

            @block.tensor
            def _(t):
                run(t, "pe")

            @block.scalar
            def _(s):
                run(s, "act")

            @block.vector
            def _(v):
                run(v, "dve")

            @block.gpsimd
            def _(g):
                run(g, "pool")

            @block.sync
            def _(sp):
                run(sp, "sp")
                for k, v in final:
                    sp.wait_ge(sems[k], v)
        self.stack.close()


class Ctx:
    pass


def t5_bucket_np(dist):
    dist = np.maximum(dist, 0)
    max_exact = 16
    with np.errstate(divide="ignore"):
        large = max_exact + (np.log(np.maximum(dist, 1).astype(np.float32) / np.float32(max_exact))
                             / np.float32(math.log(2048 / max_exact)) * np.float32(32 - max_exact)).astype(np.int32)
    large = np.minimum(large, 31)
    return np.where(dist < max_exact, dist, large)


def setup_ctx(p, consts_ap):
    cx = Ctx()
    cx.banks = [p.ps(f"bank{i}", [128, 512], F32) for i in range(8)]
    cx.bank_r = [p.region(f"bank{i}") for i in range(8)]
    cx.ident_f = p.sb("ident_f", [128, 128], F32)
    cx.ident_b = p.sb("ident_b", [128, 128], BF16)
    cx.r_ident = p.region("ident")
    p.dma("sp", cx.ident_f[:], consts_ap[0:128, 0:128], writes=[cx.r_ident])
    p.op("dve", lambda e: e.tensor_copy(out=cx.ident_b[:], in_=cx.ident_f[:]), reads=[cx.r_ident], writes=[cx.r_ident])
    return cx


def stage_mod(p, cx, c_ap, ada_w, ada_b, modD, NL, NB):
    with contextlib.ExitStack() as st:
        def sb(name, shape, dt):
            return st.enter_context(p.sbt(name, list(shape), dt))
        cT = sb("m_cT", [128, KC, NB], F32)
        r_cT = p.region()
        for b in range(NB):
            p.dma("sp", cT[:, :, b], c_ap[b].rearrange("(k p) -> p k", p=128), writes=[r_cT], allow_slow_non_contiguous=True) \
                if False else p.dma("sp", cT[:, :, b:b + 1], c_ap[b:b + 1, :].rearrange("b (k p) -> p k b", p=128), writes=[r_cT],
                                    allow_slow_non_contiguous=True)
        p.op("act", lambda e: e.activation(out=cT[:], in_=cT[:], func=AF.Silu), reads=[r_cT], writes=[r_cT])
        wt = [sb(f"m_w{i}", [128, KC, 512], F32) for i in range(2)]
        r_w = [p.region() for _ in range(2)]
        bt = [sb(f"m_b{i}", [NB, 512], F32) for i in range(2)]
        r_b = [p.region() for _ in range(2)]
        ot = [sb(f"m_o{i}", [NB, 512], F32) for i in range(2)]
        r_o = [p.region() for _ in range(2)]
        it = 0
        for l in range(NL):
            for cc in range(6 * D // 512):
                i = it % 2
                it += 1
                p.dma("sp", wt[i][:], ada_w[l, :, cc * 512:(cc + 1) * 512].rearrange("(k p) c -> p k c", p=128), writes=[r_w[i]])
                for b in range(NB):
                    p.dma("sp", bt[i][b:b + 1, :], ada_b[l:l + 1, cc * 512:(cc + 1) * 512], writes=[r_b[i]])
                bk = cx.banks[i]
                for k in range(KC):
                    p.op("pe", lambda e, k=k, i=i, bk=bk: e.matmul(bk[0:NB, :], cT[:, k, :], wt[i][:, k, :], start=(k == 0), stop=(k == KC - 1)),
                         reads=[r_cT, r_w[i]], writes=[cx.bank_r[i]], pe_acc=(k > 0))
                p.op("dve", lambda e, i=i, bk=bk: e.tensor_tensor(out=ot[i][:], in0=bk[0:NB, :], in1=bt[i][:], op=ALU.add),
                     reads=[cx.bank_r[i], r_b[i]], writes=[r_o[i]])
                p.dma("sp", modD[l, :, cc * 512:(cc + 1) * 512], ot[i][:], reads=[r_o[i]], writes=[cx.r_modD], store=True)
        p.barrier()


def load_cols(p, sbt, r, src_row):
    p.dma("sp", sbt[:].rearrange("p (k o) -> p k o", o=1), src_row.rearrange("o (k p) -> p k o", p=128), writes=[r],
          allow_slow_non_contiguous=True)


def stage_norm_T(p, cx, x_src, tok0, ntok, g_row, sc_row, sh_row, hT, hcol0, tag, r_x, r_hT):
    with contextlib.ExitStack() as st:
        def sb(name, shape, dt):
            return st.enter_context(p.sbt(tag + name, list(shape), dt))
        gc = sb("gc", [128, KC], F32)
        sc = sb("sc", [128, KC], F32)
        sh = sb("sh", [128, KC], F32)
        r_c = p.region()
        load_cols(p, gc, r_c, g_row)
        load_cols(p, sc, r_c, sc_row)
        load_cols(p, sh, r_c, sh_row)
        p.op("dve", lambda e: e.scalar_tensor_tensor(out=sc[:], in0=sc[:], scalar=1.0, in1=gc[:], op0=ALU.add, op1=ALU.mult),
             reads=[r_c], writes=[r_c])
        xt = [sb(f"xt{i}", [128, D], F32) for i in range(2)]
        r_xt = [p.region() for _ in range(2)]
        junk = sb("junk", [128, D], BF16)
        r_junk = p.region()
        ss = [sb(f"ss{i}", [128, 1], F32) for i in range(2)]
        r_ss = [p.region() for _ in range(2)]
        xn = [sb(f"xn{i}", [128, D], BF16) for i in range(4)]
        r_xn = [p.region() for _ in range(4)]
        hs = [sb(f"hs{i}", [128, KC, 512], BF16) for i in range(2)]
        r_hs = [p.region() for _ in range(2)]
        nblk = ntok // 512
        it = 0
        for blk in range(nblk):
            hb = blk % 2
            for tt in range(4):
                i = it % 2
                it += 1
                t0 = tok0 + blk * 512 + tt * 128
                p.dma("sp", xt[i][:], x_src[t0:t0 + 128, :], reads=[r_x], writes=[r_xt[i]])
                p.op("act", lambda e, i=i: e.activation(out=junk[:], in_=xt[i][:], func=AF.Square, accum_out=ss[i][:]),
                     reads=[r_xt[i]], writes=[r_junk, r_ss[i]])
                p.op("dve", lambda e, i=i: e.tensor_scalar(out=ss[i][:], in0=ss[i][:], scalar1=1.0 / D, scalar2=1e-6, op0=ALU.mult, op1=ALU.add),
                     reads=[r_ss[i]], writes=[r_ss[i]])
                p.op("act", lambda e, i=i: e.activation(out=ss[i][:], in_=ss[i][:], func=AF.Sqrt), reads=[r_ss[i]], writes=[r_ss[i]])
                p.op("dve", lambda e, i=i: e.reciprocal(out=ss[i][:], in_=ss[i][:]), reads=[r_ss[i]], writes=[r_ss[i]])
                p.op("dve", lambda e, i=i, tt=tt: e.tensor_scalar(out=xn[tt][:], in0=xt[i][:], scalar1=ss[i][:, 0:1], scalar2=None, op0=ALU.mult),
                     reads=[r_ss[i], r_xt[i]], writes=[r_xn[tt]])
            for j in range(KC):
                bk = 4 + j % 4
                for tt in range(4):
                    p.op("pe", lambda e, j=j, tt=tt, bk=bk: e.transpose(
                        out=cx.banks[bk][:].bitcast(BF16)[:, tt * 128:(tt + 1) * 128], in_=xn[tt][:, j * 128:(j + 1) * 128], identity=cx.ident_b[:]),
                        reads=[r_xn[tt], cx.r_ident], writes=[cx.bank_r[bk]], pe_acc=(tt > 0))
                p.op("act", lambda e, j=j, bk=bk, hb=hb: e.activation(
                    out=hs[hb][:, j, :], in_=cx.banks[bk][:].bitcast(BF16)[:, 0:512], func=AF.Identity, scale=sc[:, j:j + 1], bias=sh[:, j:j + 1]),
                    reads=[cx.bank_r[bk], r_c], writes=[r_hs[hb]])
            c0 = hcol0 + blk * 512
            p.dma("pool", hT[:, c0:c0 + 512].rearrange("(k p) t -> p k t", p=128), hs[hb][:], reads=[r_hs[hb]], writes=[r_hT], store=True)
        p.barrier()


def stage_proj(p, cx, w_in_l, hT, FT, TM, ntok, r_hT, r_FT, r_TM):
    with contextlib.ExitStack() as st:
        def sb(name, shape, dt):
            return st.enter_context(p.sbt("B" + name, list(shape), dt))
        wg = sb("wg", [128, KC, 1024], BF16)
        r_wg = p.region()
        hb = [sb(f"hb{i}", [128, KC, 512], BF16) for i in range(2)]
        r_hb = [p.region() for _ in range(2)]
        og = [sb(f"og{i}", [128, 512], BF16) for i in range(2)]
        r_og = [p.region() for _ in range(2)]
        nblk = ntok // 512
        chunks = []
        for seg in FT_ORDER:
            for c in range(IN_SIZES[seg] // 128):
                chunks.append((IN_OFFS[seg] + c * 128, seg in (QA, QB, QC, QD)))
        ngrp = (len(chunks) + 7) // 8
        it = 0
        ib = 0
        for g in range(ngrp):
            gch = chunks[g * 8:(g + 1) * 8]
            for j, (c0, isq) in enumerate(gch):
                p.dma("pool", wg[:, :, j * 128:(j + 1) * 128], w_in_l[:, c0:c0 + 128].rearrange("(k p) c -> p k c", p=128), writes=[r_wg])
            for blk in range(nblk):
                i = ib % 2
                ib += 1
                p.dma("sp", hb[i][:], hT[:, blk * 512:(blk + 1) * 512].rearrange("(k p) t -> p k t", p=128), reads=[r_hT], writes=[r_hb[i]])
                for j, (c0, isq) in enumerate(gch):
                    bk = it % 4
                    o = it % 2
                    it += 1
                    for k in range(KC):
                        p.op("pe", lambda e, j=j, k=k, i=i, bk=bk: e.matmul(cx.banks[bk][:], wg[:, k, j * 128:(j + 1) * 128], hb[i][:, k, :],
                                                                        start=(k == 0), stop=(k == KC - 1)),
                             reads=[r_wg, r_hb[i]], writes=[cx.bank_r[bk]], pe_acc=(k > 0))
                    p.op("act", lambda e, bk=bk, o=o, isq=isq: e.activation(out=og[o][:], in_=cx.banks[bk][:], func=AF.Copy, scale=(0.125 if isq else 1.0)),
                         reads=[cx.bank_r[bk]], writes=[r_og[o]])
                    row = (g * 8 + j) * 128
                    p.dma("sp", FT[row:row + 128, blk * 512:(blk + 1) * 512], og[o][:], reads=[r_og[o]], writes=[r_FT], store=True)
        wt = sb("wt", [128, KC, TM_COLS], BF16)
        r_wt = p.region()
        for seg in TM_ORDER:
            p.dma("pool", wt[:, :, TM_OFF[seg]:TM_OFF[seg] + IN_SIZES[seg]],
                  w_in_l[:, IN_OFFS[seg]:IN_OFFS[seg] + IN_SIZES[seg]].rearrange("(k p) c -> p k c", p=128), writes=[r_wt])
        ot = [sb(f"ot{i}", [128, TM_COLS], BF16) for i in range(2)]
        r_ot = [p.region() for _ in range(2)]
        cgs = [(0, 512), (512, 512), (1024, TM_COLS - 1024)]
        itt = 0
        for blk in range(nblk):
            i = ib % 2
            ib += 1
            p.dma("sp", hb[i][:], hT[:, blk * 512:(blk + 1) * 512].rearrange("(k p) t -> p k t", p=128), reads=[r_hT], writes=[r_hb[i]])
            for tt in range(4):
                o = itt % 2
                itt += 1
                for (c0, cn) in cgs:
                    bk = it % 4
                    it += 1
                    for k in range(KC):
                        p.op("pe", lambda e, k=k, i=i, bk=bk, tt=tt, c0=c0, cn=cn: e.matmul(
                            cx.banks[bk][:, 0:cn], hb[i][:, k, tt * 128:(tt + 1) * 128], wt[:, k, c0:c0 + cn], start=(k == 0), stop=(k == KC - 1)),
                            reads=[r_wt, r_hb[i]], writes=[cx.bank_r[bk]], pe_acc=(k > 0))
                    p.op("dve", lambda e, bk=bk, o=o, c0=c0, cn=cn: e.tensor_copy(out=ot[o][:, c0:c0 + cn], in_=cx.banks[bk][:, 0:cn]),
                         reads=[cx.bank_r[bk]], writes=[r_ot[o]])
                t0 = blk * 512 + tt * 128
                p.dma("sp", TM[t0:t0 + 128, :], ot[o][:], reads=[r_ot[o]], writes=[r_TM], store=True)
        p.barrier()


W_STRIP = 3072
NSA_ONLY = None
DBG = None
OFF_STRIP = 384


def attn_core(p, cx, pT, r_pT, S, QT, r_q, KT, r_k, Kc, V, r_v, nv, eb, r_eb, clamp, ktiles_fn, skip_fn, out_cb, cmask=None, r_cm=None):
    steps = []
    nqc = S // 512
    for qc in range(nqc):
        kts = ktiles_fn(qc)
        for kt in kts:
            steps.append((qc, kt))
    used = {}
    for qc in range(nqc):
        for tq in range(4):
            used[(qc, tq)] = [kt for kt in ktiles_fn(qc) if not skip_fn(kt, qc * 4 + tq)]

    SB = (0, 1, 6, 7)
    NP = len(pT)

    def qk(s):
        qc, kt = steps[s]
        bk = SB[s % 4]
        extra = BIAS_ON_PE and ((eb is not None) or (cmask is not None))
        p.op("pe", lambda e, qc=qc, kt=kt, bk=bk, extra=extra: e.matmul(cx.banks[bk][:], KT[0:Kc, kt * 128:(kt + 1) * 128], QT[0:Kc, qc * 512:(qc + 1) * 512],
                                                                      start=True, stop=(not extra)),
             reads=[r_q, r_k], writes=[cx.bank_r[bk]])
        if eb is not None and BIAS_ON_PE:
            b = qc * 512 - kt * 128 + OFF_STRIP
            if clamp and b > 2048:
                b = 2048
            p.op("pe", lambda e, bk=bk, b=b: e.matmul(cx.banks[bk][:], cx.ident_b[:], eb[:, b:b + 512], start=False, stop=True),
                 reads=[r_eb, cx.r_ident], writes=[cx.bank_r[bk]], pe_acc=True)
        if cmask is not None and BIAS_ON_PE:
            p.op("pe", lambda e, bk=bk, kt=kt, qc=qc: e.matmul(cx.banks[bk][:], cx.ident_b[:], cmask[:, kt, qc * 512:(qc + 1) * 512], start=False, stop=True),
                 reads=[r_cm, cx.r_ident], writes=[cx.bank_r[bk]], pe_acc=True)
    def front(s):
        qc, kt = steps[s]
        bk = SB[s % 4]
        pb = s % NP
        W_ = 128 if ATT_DIAG == 4 else 512
        p.op("act", lambda e, bk=bk, pb=pb, W_=W_: e.activation(out=pT[pb][:, 0:W_], in_=cx.banks[bk][:, 0:W_], func=AF.Exp), reads=[cx.bank_r[bk]], writes=[r_pT[pb]])
        if not BIAS_ON_PE:
            if eb is not None:
                b = qc * 512 - kt * 128 + OFF_STRIP
                if clamp and b > 2048:
                    b = 2048
                p.op("dve", lambda e, pb=pb, b=b: e.tensor_tensor(out=pT[pb][:], in0=pT[pb][:], in1=eb[:, b:b + 512], op=ALU.mult),
                     reads=[r_eb, r_pT[pb]], writes=[r_pT[pb]])
            if cmask is not None:
                p.op("dve", lambda e, pb=pb, kt=kt, qc=qc: e.tensor_tensor(out=pT[pb][:], in0=pT[pb][:], in1=cmask[:, kt, qc * 512:(qc + 1) * 512], op=ALU.mult),
                     reads=[r_cm, r_pT[pb]], writes=[r_pT[pb]])

    def back(s):
        qc, kt = steps[s]
        pb = s % NP
        for tq in range(4):
            ul = used[(qc, tq)]
            if kt not in ul:
                continue
            ob = 2 + tq
            first = (kt == ul[0])
            last = (kt == ul[-1])
            p.op("pe", lambda e, pb=pb, tq=tq, kt=kt, ob=ob, first=first, last=last: e.matmul(
                cx.banks[ob][:, 0:nv], pT[pb][:, tq * 128:(tq + 1) * 128], V[:, kt, 0:nv], start=first, stop=last),
                reads=[r_pT[pb], r_v], writes=[cx.bank_r[ob]], pe_acc=True)
            if last:
                out_cb(qc * 4 + tq, cx.banks[ob][:, 0:nv], cx.bank_r[ob])

    n = len(steps)
    for s0 in range(min(3, n)):
        qk(s0)
    for s in range(n + PV_DELAY):
        if s + 3 < n:
            qk(s + 3)
        if s < n:
            front(s)
        if s - PV_DELAY >= 0:
            back(s - PV_DELAY)


def stage_eb(p, cx, rel_strips, cst, EBD, r_EBD):
    with contextlib.ExitStack() as st:
        def sb(name, shape, dt):
            return st.enter_context(p.sbt("P" + name, list(shape), dt))
        sbias = [sb(f"sbias{i}", [128, W_STRIP], F32) for i in range(2)]
        smult = sb("smult", [128, W_STRIP], F32)
        eb = [sb(f"eb{i}", [128, W_STRIP], BF16) for i in range(2)]
        r_sb = [p.region() for _ in range(2)]
        r_eb = [p.region() for _ in range(2)]
        r_sm = p.region()
        combos = [(h, 0) for h in range(8)] + [(8 + h, 1) for h in range(8)] + [(16 + h, 2) for h in range(8)] + \
                 [(24 + h, 2) for h in range(8)] + [(24 + h, 3) for h in range(8)]
        last_m = None
        for idx, (si, mi) in enumerate(combos):
            i = idx % 2
            if mi != last_m:
                p.dma("sp", smult[:], cst["mults"][mi], writes=[r_sm])
                last_m = mi
            p.dma("sp", sbias[i][:], rel_strips[si], writes=[r_sb[i]])
            p.op("dve", lambda e, i=i: e.tensor_tensor(out=eb[i][:], in0=sbias[i][:], in1=smult[:], op=ALU.add), reads=[r_sb[i], r_sm], writes=[r_eb[i]])
            p.dma("pool", EBD[idx], eb[i][:], reads=[r_eb[i]], writes=[r_EBD], store=True)
        p.barrier()


def stage_attn(p, cx, cst, FT, TM, OD, S, b, EBD, sinks_l, nsa_w, r_FT, r_TM, r_OD):
    col0 = b * S
    NKT = S // 128
    with contextlib.ExitStack() as st:
        def sb(name, shape, dt):
            return st.enter_context(p.sbt("C" + name, list(shape), dt))
        QT = [sb(f"QT{i}", [128, S], BF16) for i in range(2)]
        KT = [sb(f"KT{i}", [128, S], BF16) for i in range(2)]
        V = [sb(f"V{i}", [128, NKT, 65], BF16) for i in range(2)]
        eb = [sb(f"eb{i}", [128, W_STRIP], BF16) for i in range(2)]
        r_q = [p.region() for _ in range(2)]
        r_k = [p.region() for _ in range(2)]
        r_v = [p.region() for _ in range(2)]
        r_eb = [p.region() for _ in range(2)]
        pT = [sb(f"pT{i}", [128, 512], BF16) for i in range(8)]
        r_pT = [p.region() for _ in range(8)]
        osb = [sb(f"osb{i}", [128, 64], F32) for i in range(8)]
        r_osb = [p.region() for _ in range(8)]
        rden = [sb(f"rden{i}", [128, 1], F32) for i in range(8)]
        r_rd = [p.region() for _ in range(8)]
        esink = sb("esink", [128, 8], F32)
        r_es = p.region()
        p.dma("sp", esink[:], bcast_rows(sinks_l, 128), writes=[r_es])
        p.op("act", lambda e: e.activation(out=esink[:], in_=esink[:], func=AF.Exp), reads=[r_es], writes=[r_es])
        for i in range(2):
            p.op("pool", lambda e, i=i: e.memset(V[i][:, :, 64:65], 1.0), writes=[r_v[i]])
        cnt = [0]
        slot = [-1]

        def nxt():
            slot[0] += 1
            return slot[0] % 2

        def load_eb(ebidx, dst, r_dst):
            p.dma("sp", dst[:], EBD[ebidx], writes=[r_dst])

        def load_ft(dst, rows, seg, idx, width, r_dst):
            r0 = FT_OFF[seg] + idx * width
            p.dma("sp", dst[rows[0]:rows[0] + width, :], FT[r0:r0 + width, col0:col0 + S], reads=[r_FT], writes=[r_dst])

        def load_v(Vt, seg, idx, r_dst):
            if ATT_DIAG == 3:
                return
            c0 = TM_OFF[seg] + idx * 64
            p.dma("sp", Vt[:, :, 0:64], TM[col0:col0 + S, c0:c0 + 64].rearrange("(t p) c -> p t c", p=128), reads=[r_TM], writes=[r_dst])

        def simple_out(mixer, h, extra_den=None):
            def cb(qt, ps, r_bank):
                i = cnt[0] % 8
                cnt[0] += 1
                if extra_den is not None:
                    p.op("dve", lambda e, i=i: e.tensor_tensor(out=rden[i][:], in0=ps[:, 64:65], in1=extra_den, op=ALU.add),
                         reads=[r_bank, r_es], writes=[r_rd[i]])
                    p.op("dve", lambda e, i=i: e.reciprocal(out=rden[i][:], in_=rden[i][:]), reads=[r_rd[i]], writes=[r_rd[i]])
                else:
                    p.op("dve", lambda e, i=i: e.reciprocal(out=rden[i][:], in_=ps[:, 64:65]), reads=[r_bank], writes=[r_rd[i]])
                p.op("dve", lambda e, i=i: e.tensor_scalar(out=osb[i][:], in0=ps[:, 0:64], scalar1=rden[i][:, 0:1], scalar2=None, op0=ALU.mult),
                     reads=[r_bank, r_rd[i]], writes=[r_osb[i]])
                t0 = col0 + qt * 128
                c0 = mixer * 512 + h * 64
                p.dma("pool", OD[t0:t0 + 128, c0:c0 + 64], osb[i][:], reads=[r_osb[i]], writes=[r_OD], store=True)
            return cb

        causal_skip = lambda kt, qt: kt > qt
        for h in range(8):
            i = nxt()
            load_ft(QT[i], (0,), QA, h, 64, r_q[i])
            load_ft(KT[i], (0,), KA, h, 64, r_k[i])
            load_v(V[i], VA, h, r_v[i])
            load_eb(h, eb[i], r_eb[i])
            attn_core(p, cx, pT, r_pT, S, QT[i], r_q[i], KT[i], r_k[i], 64, V[i], r_v[i], 65, eb[i], r_eb[i], False,
                      lambda qc: list(range(max(0, (qc * 512 - 2048) // 128), min(NKT, qc * 4 + 4))), causal_skip, simple_out(0, h))
        for h in range(8):
            i = nxt()
            load_ft(QT[i], (0,), QB, h, 64, r_q[i])
            load_ft(KT[i], (0,), KB_, h // 4, 64, r_k[i])
            load_v(V[i], VB, h // 4, r_v[i])
            load_eb(8 + h, eb[i], r_eb[i])
            attn_core(p, cx, pT, r_pT, S, QT[i], r_q[i], KT[i], r_k[i], 64, V[i], r_v[i], 65, eb[i], r_eb[i], False,
                      lambda qc: list(range(max(0, qc * 4 - 1), min(NKT, qc * 4 + 4))), causal_skip, simple_out(1, h, extra_den=esink[:, h:h + 1]))
        stage_moba(p, cx, cst, sb, S, col0, FT, QT, KT, V, r_q, r_k, r_v, pT, r_pT, eb, r_eb, nxt, load_ft, load_v, load_eb, simple_out, causal_skip, r_FT)
        stage_nsa(p, cx, cst, sb, S, col0, FT, TM, OD, QT, KT, V, r_q, r_k, r_v, pT, r_pT, eb, r_eb, nxt, load_ft, load_v, load_eb,
                  causal_skip, nsa_w, r_FT, r_TM, r_OD, rden, r_rd, osb, r_osb, cnt)
        p.barrier()


def bcast_rows(ap1d, n):
    return bass.AP(tensor=ap1d.tensor, offset=ap1d.offset, ap=[[0, n]] + [list(x) for x in ap1d.ap])


def stage_moba(p, cx, cst, sb, S, col0, FT, QTs, KTs, Vs, r_qs, r_ks, r_vs, pT, r_pT, ebs, r_ebs, nxt, load_ft, load_v, load_eb, simple_out, causal_skip, r_FT):
    NKT = S // 128
    nblk = S // 256
    kmf = sb("kmf", [64, 16], F32)
    kmb = sb("kmb", [64, 16], BF16)
    r_km = p.region()
    mv = sb("mv", [128, 16, 16], F32)
    own = sb("own", [128, 16, 16], F32)
    r_mc = p.region()
    p.dma("sp", mv[:].rearrange("p a b -> p (a b)"), bcast_rows(cst["moba_valid"], 128), writes=[r_mc])
    p.dma("sp", own[:].rearrange("p a b -> p (a b)"), bcast_rows(cst["moba_own"], 128), writes=[r_mc])
    gs = sb("gs", [128, 16], F32)
    m8 = sb("m8", [128, 8], F32)
    sel = sb("sel", [128, 16], F32)
    wide = sb("wide", [128, 128], F32)
    r_gs, r_m8, r_sel, r_wide = p.region(), p.region(), p.region(), p.region()
    p.op("pool", lambda e: e.memset(wide[:], 0.0), writes=[r_wide])
    p.op("pool", lambda e: e.memset(kmf[:], 0.0), writes=[r_km])
    for i in range(2):
        p.dma("sp", KTs[i][64:80, :], cst["moba_koh"], writes=[r_ks[i]])
    for h in range(8):
        i = nxt()
        QT, KT, V, eb, r_q, r_k, r_v, r_eb = QTs[i], KTs[i], Vs[i], ebs[i], r_qs[i], r_ks[i], r_vs[i], r_ebs[i]
        load_ft(QT, (0,), QC, h, 64, r_q)
        load_ft(KT, (0,), KC_, h, 64, r_k)
        load_v(V, VC, h, r_v)
        load_eb(16 + h, eb, r_eb)
        p.op("dve", lambda e, KT=KT: e.tensor_reduce(out=kmf[:, 0:nblk], in_=KT[0:64, :].rearrange("p (n k) -> p n k", k=256), axis=AX.X, op=ALU.add),
             reads=[r_k], writes=[r_km])
        p.op("dve", lambda e: e.tensor_scalar(out=kmb[:], in0=kmf[:], scalar1=1.0 / 256, scalar2=None, op0=ALU.mult), reads=[r_km], writes=[r_km])
        for qt in range(NKT):
            cur = qt // 2
            p.op("pe", lambda e, qt=qt, QT=QT: e.matmul(cx.banks[6][:, 0:16], QT[0:64, qt * 128:(qt + 1) * 128], kmb[:], start=True, stop=True),
                 reads=[r_q, r_km], writes=[cx.bank_r[6]])
            p.op("dve", lambda e, cur=cur: e.tensor_tensor(out=gs[:], in0=cx.banks[6][:, 0:16], in1=mv[:, cur, :], op=ALU.add),
                 reads=[cx.bank_r[6], r_mc], writes=[r_gs])
            p.op("dve", lambda e: e.max(out=m8[:], in_=gs[:]), reads=[r_gs], writes=[r_m8])
            p.op("dve", lambda e: e.tensor_scalar(out=m8[:, 2:3], in0=m8[:, 2:3], scalar1=-1e29, scalar2=None, op0=ALU.max), reads=[r_m8], writes=[r_m8])
            p.op("dve", lambda e: e.tensor_scalar(out=sel[:], in0=gs[:], scalar1=m8[:, 2:3], scalar2=None, op0=ALU.is_ge), reads=[r_gs, r_m8], writes=[r_sel])
            p.op("dve", lambda e, cur=cur: e.tensor_tensor(out=sel[:], in0=sel[:], in1=own[:, cur, :], op=ALU.max), reads=[r_sel, r_mc], writes=[r_sel])
            p.op("dve", lambda e: e.tensor_scalar(out=wide[:, 64:80], in0=sel[:], scalar1=-1.0, scalar2=-NEG, op0=ALU.add, op1=ALU.mult),
                 reads=[r_sel], writes=[r_wide])
            p.op("pe", lambda e: e.transpose(out=cx.banks[7][:, 0:128], in_=wide[:], identity=cx.ident_f[:]), reads=[r_wide, cx.r_ident], writes=[cx.bank_r[7]])
            p.op("act", lambda e, qt=qt, QT=QT: e.activation(out=QT[64:80, qt * 128:(qt + 1) * 128], in_=cx.banks[7][64:80, 0:128], func=AF.Copy),
                 reads=[cx.bank_r[7]], writes=[r_q])
        attn_core(p, cx, pT, r_pT, S, QT, r_q, KT, r_k, 80, V, r_v, 65, eb, r_eb, True,
                  lambda qc: list(range(0, min(NKT, qc * 4 + 4))), causal_skip, simple_out(2, h))


def stage_nsa(p, cx, cst, sb, S, col0, FT, TM, OD, QTs, KTs, Vs, r_qs, r_ks, r_vs, pT, r_pT, ebs, r_ebs, nxt, load_ft, load_v, load_eb,
              causal_skip, nsa_w, r_FT, r_TM, r_OD, rden, r_rd, osb, r_osb, cnt):
    NKT = S // 128
    ncmp = (S - 32) // 16 + 1
    w1k, w2k, w1v, w2v, pek, pev = nsa_w
    w1 = [sb(f"w1_{i}", [64, 32, 128], BF16) for i in range(2)]
    w2 = [sb(f"w2_{i}", [128, 64], BF16) for i in range(2)]
    peT = [sb(f"peT{i}", [64, 32], BF16) for i in range(2)]
    r_w = p.region()
    for i, (a, b2, c) in enumerate(((w1k, w2k, pek), (w1v, w2v, pev))):
        p.dma("pool", w1[i][:], a.rearrange("(t d) m -> d t m", d=64), writes=[r_w])
        p.dma("pool", w2[i][:], b2, writes=[r_w])
        p.dma("pool", peT[i][:], c.rearrange("t d -> d t"), writes=[r_w], allow_slow_non_contiguous=True)
    src = sb("csrc", [64, S], BF16)
    r_src = p.region()
    hbias = sb("hbias", [128, 1], F32)
    r_hb = p.region()
    xs = sb("xs", [128, 256], F32)
    u = sb("u", [128, 256], F32)
    hid = sb("hid", [128, 256], BF16)
    r_xs, r_u, r_hid = p.region(), p.region(), p.region()
    KTc = sb("KTc", [64, 256], BF16)
    Vc = sb("Vc", [128, 2, 129], BF16)
    r_ktc, r_vc = p.region(), p.region()
    cmask = sb("cmask", [128, 2, S], BF16)
    r_cm = p.region()
    p.dma("sp", cmask[:], cst["cmp_mask"].rearrange("(t p) s -> p t s", p=128), writes=[r_cm])
    gsig = sb("gsig", [128, NKT, 24], F32)
    r_g = p.region()
    p.dma("sp", gsig[:], TM[col0:col0 + S, TM_OFF[GD]:TM_OFF[GD] + 24].rearrange("(t p) c -> p t c", p=128), reads=[r_TM], writes=[r_g]) \
        if False else None
    gtmp = sb("gtmp", [128, NKT, 24], BF16)
    p.dma("sp", gtmp[:], TM[col0:col0 + S, TM_OFF[GD]:TM_OFF[GD] + 24].rearrange("(t p) c -> p t c", p=128), reads=[r_TM], writes=[r_g])
    p.op("act", lambda e: e.activation(out=gsig[:], in_=gtmp[:], func=AF.Sigmoid), reads=[r_g], writes=[r_g])
    imp = sb("imp", [128, NKT, 64], F32)
    r_imp = p.region()
    oacc = [sb(f"oacc{g}", [128, NKT, 64], F32) for g in range(4)]
    r_oacc = [p.region() for _ in range(4)]
    selmul = sb("selmul", [128, 64], F32)
    seladd = sb("seladd", [128, 64], F32)
    r_sc = p.region()
    selT = sb("selT", [128, S], BF16)
    r_selT = p.region()
    v1 = sb("v1", [128, 64], F32)
    v2 = sb("v2", [128, 64], F32)
    m8a = sb("m8a", [128, 8], F32)
    m8b = sb("m8b", [128, 8], F32)
    wide = sb("nwide", [128, 128], F32)
    r_v1, r_v2, r_m8a, r_m8b, r_wide = p.region(), p.region(), p.region(), p.region(), p.region()
    p.op("pool", lambda e: e.memset(wide[:], 0.0), writes=[r_wide])
    p.op("pool", lambda e: e.memset(hid[:], 0.0), writes=[r_hid])
    p.op("pool", lambda e: e.memset(KTc[:], 0.0), writes=[r_ktc])
    p.op("pool", lambda e: e.memset(Vc[:, :, 64:65], 1.0), writes=[r_vc])
    p.dma("sp", Vc[:, :, 65:129], cst["overlap"].rearrange("(t p) c -> p t c", p=128), writes=[r_vc])
    tmp = [sb(f"ntmp{i}", [128, 64], F32) for i in range(2)]
    r_tmp = [p.region() for _ in range(2)]

    def compress(i, seg, kvh):
        r0 = FT_OFF[seg] + kvh * 64
        p.dma("sp", src[:], FT[r0:r0 + 64, col0:col0 + S], reads=[r_FT], writes=[r_src])
        sv = src[:].rearrange("p (n s) -> p n s", s=16)
        for t in range(32):
            p.op("pe", lambda e, t=t: e.matmul(cx.banks[6][:, 0:ncmp], w1[i][:, t, :], sv[:, t // 16:t // 16 + ncmp, t % 16], start=(t == 0), stop=(t == 31)),
                 reads=[r_w, r_src], writes=[cx.bank_r[6]], pe_acc=(t > 0))
        for t in range(32):
            p.op("pe", lambda e, t=t: e.matmul(cx.banks[7][:, 0:1], w1[i][:, t, :], peT[i][:, t:t + 1], start=(t == 0), stop=(t == 31)),
                 reads=[r_w], writes=[cx.bank_r[7]], pe_acc=(t > 0))
        p.op("dve", lambda e: e.tensor_copy(out=hbias[:], in_=cx.banks[7][:, 0:1]), reads=[cx.bank_r[7]], writes=[r_hb])
        p.op("act", lambda e: e.activation(out=xs[:, 0:ncmp], in_=cx.banks[6][:, 0:ncmp], func=AF.Identity, bias=hbias[:, 0:1]),
             reads=[cx.bank_r[6], r_hb], writes=[r_xs])
        p.op("dve", lambda e: e.tensor_tensor(out=u[:, 0:ncmp], in0=xs[:, 0:ncmp], in1=xs[:, 0:ncmp], op=ALU.mult), reads=[r_xs], writes=[r_u])
        p.op("dve", lambda e: e.tensor_scalar(out=u[:, 0:ncmp], in0=u[:, 0:ncmp], scalar1=0.044715, scalar2=1.0, op0=ALU.mult, op1=ALU.add), reads=[r_u], writes=[r_u])
        p.op("dve", lambda e: e.tensor_tensor(out=u[:, 0:ncmp], in0=u[:, 0:ncmp], in1=xs[:, 0:ncmp], op=ALU.mult), reads=[r_u, r_xs], writes=[r_u])
        p.op("act", lambda e: e.activation(out=u[:, 0:ncmp], in_=u[:, 0:ncmp], func=AF.Sigmoid, scale=2.0 * 0.7978845608028654), reads=[r_u], writes=[r_u])
        p.op("dve", lambda e: e.tensor_tensor(out=hid[:, 0:ncmp], in0=u[:, 0:ncmp], in1=xs[:, 0:ncmp], op=ALU.mult), reads=[r_u, r_xs], writes=[r_hid])

    for i in range(2):
        p.dma("sp", KTs[i][64:128, :], cst["nsa_koh"], writes=[r_ks[i]])
    for kvh in range(2):
        compress(0, KDC, kvh)
        p.op("pe", lambda e: e.matmul(cx.banks[6][0:64, 0:256], w2[0][:], hid[:], start=True, stop=True), reads=[r_w, r_hid], writes=[cx.bank_r[6]])
        p.op("act", lambda e: e.activation(out=KTc[:, 0:ncmp], in_=cx.banks[6][0:64, 0:ncmp], func=AF.Copy), reads=[cx.bank_r[6]], writes=[r_ktc])
        compress(1, VDC, kvh)
        for nt in range((ncmp + 127) // 128):
            p.op("pe", lambda e, nt=nt: e.matmul(cx.banks[7][:, 0:64], hid[:, nt * 128:(nt + 1) * 128], w2[1][:], start=True, stop=True),
                 reads=[r_w, r_hid], writes=[cx.bank_r[7]])
            p.op("act", lambda e, nt=nt: e.activation(out=Vc[:, nt, 0:64], in_=cx.banks[7][:, 0:64], func=AF.Copy), reads=[cx.bank_r[7]], writes=[r_vc])
        p.op("pool", lambda e: e.memset(imp[:], 0.0), writes=[r_imp])
        nct = (ncmp + 127) // 128
        for g in range(4):
            h = kvh * 4 + g
            i = nxt()
            QT, r_q = QTs[i], r_qs[i]
            load_ft(QT, (0,), QD, h, 64, r_q)

            def cmp_cb(qt, ps, r_bank, g=g, h=h):
                i = cnt[0] % 2
                cnt[0] += 1
                p.op("dve", lambda e, i=i: e.tensor_scalar(out=rden[i][:], in0=ps[:, 64:65], scalar1=1e-30, scalar2=None, op0=ALU.max), reads=[r_bank], writes=[r_rd[i]])
                p.op("dve", lambda e, i=i: e.reciprocal(out=rden[i][:], in_=rden[i][:]), reads=[r_rd[i]], writes=[r_rd[i]])
                p.op("dve", lambda e, i=i, qt=qt: e.scalar_tensor_tensor(out=imp[:, qt, :], in0=ps[:, 65:129], scalar=rden[i][:, 0:1], in1=imp[:, qt, :],
                                                                       op0=ALU.mult, op1=ALU.add), reads=[r_bank, r_rd[i], r_imp], writes=[r_imp])
                p.op("dve", lambda e, i=i: e.tensor_scalar(out=tmp[i][:], in0=ps[:, 0:64], scalar1=rden[i][:, 0:1], scalar2=None, op0=ALU.mult),
                     reads=[r_bank, r_rd[i]], writes=[r_tmp[i]])
                p.op("dve", lambda e, i=i, qt=qt: e.tensor_scalar(out=oacc[g][:, qt, :], in0=tmp[i][:], scalar1=(gsig[:, qt, h * 3:h * 3 + 1] if NSA_ONLY in (None, 0) else 0.0), scalar2=None, op0=ALU.mult),
                     reads=[r_tmp[i], r_g], writes=[r_oacc[g]])
            attn_core(p, cx, pT, r_pT, S, QT, r_q, KTc, r_ktc, 64, Vc, r_vc, 129, None, None, False,
                      lambda qc: list(range(nct)), lambda kt, qt: False, cmp_cb, cmask=cmask, r_cm=r_cm)
        for qt in range(NKT):
            p.dma("sp", selmul[:], cst["sel_mul"][qt * 128:(qt + 1) * 128, :], writes=[r_sc])
            p.dma("sp", seladd[:], cst["sel_add"][qt * 128:(qt + 1) * 128, :], writes=[r_sc])
            p.op("dve", lambda e, qt=qt: e.tensor_tensor(out=v1[:], in0=imp[:, qt, :], in1=selmul[:], op=ALU.mult), reads=[r_imp, r_sc], writes=[r_v1])
            p.op("dve", lambda e, qt=qt: e.tensor_tensor(out=v1[:], in0=v1[:], in1=seladd[:], op=ALU.add), reads=[r_v1, r_sc], writes=[r_v1])
            p.op("dve", lambda e: e.max(out=m8a[:], in_=v1[:]), reads=[r_v1], writes=[r_m8a])
            p.op("dve", lambda e: e.match_replace(out=v2[:], in_to_replace=m8a[:], in_values=v1[:], imm_value=-1e30), reads=[r_v1, r_m8a], writes=[r_v2])
            p.op("dve", lambda e: e.max(out=m8b[:], in_=v2[:]), reads=[r_v2], writes=[r_m8b])
            p.op("dve", lambda e: e.tensor_scalar(out=m8b[:, 7:8], in0=m8b[:, 7:8], scalar1=-1e29, scalar2=None, op0=ALU.max), reads=[r_m8b], writes=[r_m8b])
            p.op("dve", lambda e: e.tensor_scalar(out=v2[:], in0=v1[:], scalar1=m8b[:, 7:8], scalar2=None, op0=ALU.is_ge), reads=[r_v1, r_m8b], writes=[r_v2])
            p.op("dve", lambda e: e.tensor_scalar(out=wide[:, 64:128], in0=v2[:], scalar1=-1.0, scalar2=-NEG, op0=ALU.add, op1=ALU.mult), reads=[r_v2], writes=[r_wide])
            p.op("pe", lambda e: e.transpose(out=cx.banks[7][:, 0:128], in_=wide[:], identity=cx.ident_f[:]), reads=[r_wide, cx.r_ident], writes=[cx.bank_r[7]])
            p.op("act", lambda e, qt=qt: e.activation(out=selT[64:128, qt * 128:(qt + 1) * 128], in_=cx.banks[7][64:128, 0:128], func=AF.Copy),
                 reads=[cx.bank_r[7]], writes=[r_selT])
        for g in range(4):
            h = kvh * 4 + g
            for br in (1, 2):
                i = nxt()
                QT, KT, V, eb, r_q, r_k, r_v, r_eb = QTs[i], KTs[i], Vs[i], ebs[i], r_qs[i], r_ks[i], r_vs[i], r_ebs[i]
                load_ft(QT, (0,), QD, h, 64, r_q)
                if br == 1:
                    p.op("act", lambda e, QT=QT: e.activation(out=QT[64:128, :], in_=selT[64:128, :], func=AF.Copy), reads=[r_selT], writes=[r_q])
                    load_ft(KT, (0,), KDS, kvh, 64, r_k)
                    load_v(V, VDS, kvh, r_v)
                    load_eb(24 + h, eb, r_eb)
                    kfn = lambda qc: list(range(0, min(NKT, qc * 4 + 4)))
                    Kc, clamp = 128, True
                else:
                    load_ft(KT, (0,), KDW, kvh, 64, r_k)
                    load_v(V, VDW, kvh, r_v)
                    load_eb(32 + h, eb, r_eb)
                    kfn = lambda qc: list(range(max(0, qc * 4 - 4), min(NKT, qc * 4 + 4)))
                    Kc, clamp = 64, False

                def br_cb(qt, ps, r_bank, g=g, h=h, br=br):
                    if NSA_ONLY is not None and NSA_ONLY != br:
                        return
                    i = cnt[0] % 2
                    cnt[0] += 1
                    p.op("dve", lambda e, i=i: e.reciprocal(out=rden[i][:], in_=ps[:, 64:65]), reads=[r_bank], writes=[r_rd[i]])
                    p.op("dve", lambda e, i=i: e.tensor_scalar(out=tmp[i][:], in0=ps[:, 0:64], scalar1=rden[i][:, 0:1], scalar2=None, op0=ALU.mult),
                         reads=[r_bank, r_rd[i]], writes=[r_tmp[i]])
                    p.op("dve", lambda e, i=i, qt=qt: e.scalar_tensor_tensor(out=oacc[g][:, qt, :], in0=tmp[i][:], scalar=gsig[:, qt, h * 3 + br:h * 3 + br + 1],
                                                                           in1=oacc[g][:, qt, :], op0=ALU.mult, op1=ALU.add),
                         reads=[r_tmp[i], r_g, r_oacc[g]], writes=[r_oacc[g]])
                attn_core(p, cx, pT, r_pT, S, QT, r_q, KT, r_k, Kc, V, r_v, 65, eb, r_eb, clamp, kfn, causal_skip, br_cb)
            c0 = 3 * 512 + h * 64
            p.dma("pool", OD[col0:col0 + S, c0:c0 + 64].rearrange("(t p) c -> p t c", p=128), oacc[g][:], reads=[r_oacc[g]], writes=[r_OD], store=True)


def make_consts(S, rel_bias):
    cst = {}
    j = np.arange(128)[:, None]
    m = np.arange(W_STRIP)[None, :]
    d = m - OFF_STRIP - j
    dd = np.maximum(d, 0)
    bucket = t5_bucket_np(dd)
    strips = np.ascontiguousarray(np.transpose(rel_bias[bucket], (2, 0, 1))).astype(np.float32)
    causal = (d >= 0)
    multA = ((d >= 0) & (d <= 128)).astype(np.float32) + ((d >= 0) & (d % 4 == 0) & (d <= 512)) + ((d >= 0) & (d % 16 == 0) & (d <= 2048))
    mults = np.stack([multA, (causal & (d <= 127)), causal, (causal & (d <= 511))]).astype(np.float32)
    with np.errstate(divide="ignore"):
        cst["mults"] = np.where(mults > 0, np.log(np.maximum(mults, 1e-30)), NEG).astype(np.float32)
    nblk = 16
    cur = np.arange(16)[:, None]
    n = np.arange(16)[None, :]
    cst["moba_valid"] = np.where(n < cur, 0.0, -1e30).astype(np.float32).reshape(-1)
    cst["moba_own"] = (n == cur).astype(np.float32).reshape(-1)
    import ml_dtypes
    bf = ml_dtypes.bfloat16
    key = np.arange(S)[None, :]
    cst["moba_koh"] = (key // 256 == np.arange(16)[:, None]).astype(bf)
    cst["nsa_koh"] = (key // 64 == np.arange(64)[:, None]).astype(bf)
    ncmp = (S - 32) // 16 + 1
    nn = np.arange(256)[:, None]
    cst["cmp_mask"] = np.where((nn * 16 + 31 <= key) & (nn < ncmp), 0.0, NEG).astype(bf)
    c_start = nn * 16
    j_start = np.arange(64)[None, :] * 64
    cst["overlap"] = ((c_start < j_start + 64) & (c_start + 32 > j_start) & (nn < ncmp)).astype(bf)
    pos = np.arange(S)[:, None]
    curq = pos // 64
    jj = np.arange(64)[None, :]
    forced = (jj == 0) | (jj == curq) | (jj == curq - 1)
    valid = jj <= curq
    cst["sel_mul"] = (valid & ~forced).astype(np.float32)
    cst["sel_add"] = np.where(forced, 1e30, np.where(valid, 0.0, -1e30)).astype(np.float32)
    return cst, strips


def stage_mix_out(p, cx, OD, x_src, x_dst, mixg_row, w_out_l, modD, l, NB, S, r_OD, r_x):
    with contextlib.ExitStack() as st:
        def sb(name, shape, dt):
            return st.enter_context(p.sbt("E" + name, list(shape), dt))
        wo = sb("wo", [128, KC, D], BF16)
        r_wo = p.region()
        for k4 in range(4):
            p.dma("pool", wo[:, k4 * 4:(k4 + 1) * 4, :], w_out_l[k4 * 512:(k4 + 1) * 512, :].rearrange("(k p) c -> p k c", p=128), writes=[r_wo])
        mg = sb("mg", [128, KC], F32)
        r_mg = p.region()
        load_cols(p, mg, r_mg, mixg_row)
        g1b = sb("g1b", [128, D], F32)
        r_g1 = p.region()
        ot = [sb(f"ot{i}", [128, D], F32) for i in range(2)]
        r_ot = [p.region() for _ in range(2)]
        junk = sb("junk", [128, 512], BF16)
        r_junk = p.region()
        ss = [sb(f"ss{i}", [128, 4], F32) for i in range(2)]
        r_ss = [p.region() for _ in range(2)]
        on = sb("on", [128, D], BF16)
        r_on = p.region()
        cT = [sb(f"cT{i}", [128, KC, 128], BF16) for i in range(2)]
        r_cT = [p.region() for _ in range(2)]
        xt = [sb(f"xt{i}", [128, D], F32) for i in range(2)]
        r_xt = [p.region() for _ in range(2)]
        x1 = [sb(f"x1{i}", [128, D], F32) for i in range(2)]
        r_x1 = [p.region() for _ in range(2)]
        it = 0
        for b in range(NB):
            p.dma("sp", g1b[:], bcast_rows(modD[l, b, 2 * D:3 * D], 128), reads=[cx.r_modD], writes=[r_g1])
            for tt in range(S // 128):
                i = it % 2
                it += 1
                t0 = b * S + tt * 128
                p.dma("sp", ot[i][:], OD[t0:t0 + 128, :], reads=[r_OD], writes=[r_ot[i]])
                p.dma("sp", xt[i][:], x_src[t0:t0 + 128, :], reads=[r_x], writes=[r_xt[i]])
                for m in range(4):
                    p.op("act", lambda e, i=i, m=m: e.activation(out=junk[:], in_=ot[i][:, m * 512:(m + 1) * 512], func=AF.Square, accum_out=ss[i][:, m:m + 1]),
                         reads=[r_ot[i]], writes=[r_junk, r_ss[i]])
                p.op("dve", lambda e, i=i: e.tensor_scalar(out=ss[i][:], in0=ss[i][:], scalar1=1.0 / 512, scalar2=1e-6, op0=ALU.mult, op1=ALU.add),
                     reads=[r_ss[i]], writes=[r_ss[i]])
                p.op("act", lambda e, i=i: e.activation(out=ss[i][:], in_=ss[i][:], func=AF.Sqrt), reads=[r_ss[i]], writes=[r_ss[i]])
                p.op("dve", lambda e, i=i: e.reciprocal(out=ss[i][:], in_=ss[i][:]), reads=[r_ss[i]], writes=[r_ss[i]])
                for m in range(4):
                    p.op("dve", lambda e, i=i, m=m: e.tensor_scalar(out=on[:, m * 512:(m + 1) * 512], in0=ot[i][:, m * 512:(m + 1) * 512],
                                                                  scalar1=ss[i][:, m:m + 1], scalar2=None, op0=ALU.mult),
                         reads=[r_ot[i], r_ss[i]], writes=[r_on])
                for j4 in range(4):
                    bk = 4 + j4
                    for jj in range(4):
                        j = j4 * 4 + jj
                        p.op("pe", lambda e, j=j, jj=jj, bk=bk: e.transpose(out=cx.banks[bk][:].bitcast(BF16)[:, jj * 128:(jj + 1) * 128], in_=on[:, j * 128:(j + 1) * 128],
                                                                          identity=cx.ident_b[:]),
                             reads=[r_on, cx.r_ident], writes=[cx.bank_r[bk]], pe_acc=(jj > 0))
                    for jj in range(4):
                        j = j4 * 4 + jj
                        p.op("act", lambda e, j=j, jj=jj, bk=bk, i=i: e.activation(out=cT[i][:, j, :], in_=cx.banks[bk][:].bitcast(BF16)[:, jj * 128:(jj + 1) * 128],
                                                                             func=AF.Copy, scale=mg[:, j:j + 1]),
                             reads=[cx.bank_r[bk], r_mg], writes=[r_cT[i]])
                for n in range(4):
                    for k in range(KC):
                        p.op("pe", lambda e, n=n, k=k, i=i: e.matmul(cx.banks[n][:], cT[i][:, k, :], wo[:, k, n * 512:(n + 1) * 512], start=(k == 0), stop=(k == KC - 1)),
                             reads=[r_cT[i], r_wo], writes=[cx.bank_r[n]], pe_acc=(k > 0))
                    p.op("dve", lambda e, n=n, i=i: e.tensor_tensor(out=x1[i][:, n * 512:(n + 1) * 512], in0=cx.banks[n][:], in1=g1b[:, n * 512:(n + 1) * 512], op=ALU.mult),
                         reads=[cx.bank_r[n], r_g1], writes=[r_x1[i]])
                    p.op("dve", lambda e, n=n, i=i: e.tensor_tensor(out=x1[i][:, n * 512:(n + 1) * 512], in0=x1[i][:, n * 512:(n + 1) * 512],
                                                                  in1=xt[i][:, n * 512:(n + 1) * 512], op=ALU.add),
                         reads=[r_x1[i], r_xt[i]], writes=[r_x1[i]])
                p.dma("pool", x_dst[t0:t0 + 128, :], x1[i][:], reads=[r_x1[i]], writes=[r_x], store=True)
        p.barrier()


def stage_moe(p, cx, h2T, NT, router_w, router_bias, wg_l, wu_l, wd_l, yacc, r_h2T, r_y):
    NTL = NT // 128
    with contextlib.ExitStack() as st:
        def sb(name, shape, dt):
            return st.enter_context(p.sbt("M" + name, list(shape), dt))
        rwb = sb("rwb", [128, KC, 16], BF16)
        r_rw = p.region()
        p.dma("pool", rwb[:], router_w.rearrange("(k p) e -> p k e", p=128), writes=[r_rw])
        rb = sb("rb", [128, 16], F32)
        p.dma("sp", rb[:], bcast_rows(router_bias, 128), writes=[r_rw])
        comb = sb("comb", [128, NTL, 16], F32)
        r_comb = p.region()
        hb = [sb(f"hb{i}", [128, KC, 512], BF16) for i in range(2)]
        r_hb = [p.region() for _ in range(2)]
        aff = sb("aff", [128, 16], F32)
        sc = sb("sc", [128, 16], F32)
        t16 = sb("t16", [128, 16], F32)
        m1 = sb("m1", [128, 4], F32)
        m2 = sb("m2", [128, 4], F32)
        gm = sb("gm", [128, 1], F32)
        e1 = sb("e1", [128, 1], F32)
        r_aff, r_sc, r_t16, r_m1, r_m2, r_gm, r_e1 = (p.region() for _ in range(7))
        ib = 0

        def v3(t):
            return t[:].rearrange("p (g k) -> p g k", k=4)
        for blk in range(NT // 512):
            i = ib % 2
            ib += 1
            p.dma("sp", hb[i][:], h2T[:, blk * 512:(blk + 1) * 512].rearrange("(k p) t -> p k t", p=128), reads=[r_h2T], writes=[r_hb[i]])
            for tt in range(4):
                tl = blk * 4 + tt
                for k in range(KC):
                    p.op("pe", lambda e, k=k, i=i, tt=tt: e.matmul(cx.banks[6][:, 0:16], hb[i][:, k, tt * 128:(tt + 1) * 128], rwb[:, k, :], start=(k == 0), stop=(k == KC - 1)),
                         reads=[r_hb[i], r_rw], writes=[cx.bank_r[6]], pe_acc=(k > 0))
                p.op("act", lambda e: e.activation(out=aff[:], in_=cx.banks[6][:, 0:16], func=AF.Sigmoid), reads=[cx.bank_r[6]], writes=[r_aff])
                p.op("dve", lambda e: e.tensor_tensor(out=sc[:], in0=aff[:], in1=rb[:], op=ALU.add), reads=[r_aff, r_rw], writes=[r_sc])
                p.op("dve", lambda e: e.tensor_reduce(out=m1[:], in_=v3(sc), axis=AX.X, op=ALU.max), reads=[r_sc], writes=[r_m1])
                p.op("dve", lambda e: e.tensor_tensor(out=v3(t16), in0=v3(sc), in1=m1[:].unsqueeze(2).to_broadcast([128, 4, 4]), op=ALU.is_equal),
                     reads=[r_sc, r_m1], writes=[r_t16])
                p.op("dve", lambda e: e.scalar_tensor_tensor(out=t16[:], in0=t16[:], scalar=-1e30, in1=sc[:], op0=ALU.mult, op1=ALU.add),
                     reads=[r_t16, r_sc], writes=[r_t16])
                p.op("dve", lambda e: e.tensor_reduce(out=m2[:], in_=v3(t16), axis=AX.X, op=ALU.max), reads=[r_t16], writes=[r_m2])
                p.op("dve", lambda e: e.tensor_tensor(out=m1[:], in0=m1[:], in1=m2[:], op=ALU.add), reads=[r_m1, r_m2], writes=[r_m1])
                p.op("dve", lambda e: e.tensor_reduce(out=gm[:], in_=m1[:], axis=AX.X, op=ALU.max), reads=[r_m1], writes=[r_gm])
                p.op("dve", lambda e: e.tensor_scalar(out=m2[:], in0=m1[:], scalar1=gm[:, 0:1], scalar2=None, op0=ALU.is_ge), reads=[r_m1, r_gm], writes=[r_m2])
                p.op("dve", lambda e: e.tensor_scalar(out=m2[:], in0=m2[:], scalar1=-1.0, scalar2=1e30, op0=ALU.add, op1=ALU.mult), reads=[r_m2], writes=[r_m2])
                p.op("dve", lambda e: e.tensor_tensor(out=v3(t16), in0=v3(sc), in1=m2[:].unsqueeze(2).to_broadcast([128, 4, 4]), op=ALU.add),
                     reads=[r_sc, r_m2], writes=[r_t16])
                p.op("dve", lambda e: e.tensor_reduce(out=e1[:], in_=t16[:], axis=AX.X, op=ALU.max), reads=[r_t16], writes=[r_e1])
                p.op("dve", lambda e: e.tensor_scalar(out=sc[:], in0=t16[:], scalar1=e1[:, 0:1], scalar2=None, op0=ALU.is_equal), reads=[r_t16, r_e1], writes=[r_sc])
                p.op("dve", lambda e: e.scalar_tensor_tensor(out=t16[:], in0=sc[:], scalar=-1e30, in1=t16[:], op0=ALU.mult, op1=ALU.add),
                     reads=[r_sc, r_t16], writes=[r_t16])
                p.op("dve", lambda e: e.tensor_reduce(out=e1[:], in_=t16[:], axis=AX.X, op=ALU.max), reads=[r_t16], writes=[r_e1])
                p.op("dve", lambda e: e.tensor_scalar(out=t16[:], in0=t16[:], scalar1=e1[:, 0:1], scalar2=None, op0=ALU.is_equal), reads=[r_t16, r_e1], writes=[r_t16])
                p.op("dve", lambda e: e.tensor_tensor(out=sc[:], in0=sc[:], in1=t16[:], op=ALU.add), reads=[r_sc, r_t16], writes=[r_sc])
                p.op("dve", lambda e: e.tensor_tensor(out=sc[:], in0=sc[:], in1=aff[:], op=ALU.mult), reads=[r_sc, r_aff], writes=[r_sc])
                p.op("dve", lambda e: e.tensor_reduce(out=e1[:], in_=sc[:], axis=AX.X, op=ALU.add), reads=[r_sc], writes=[r_e1])
                p.op("dve", lambda e: e.reciprocal(out=e1[:], in_=e1[:]), reads=[r_e1], writes=[r_e1])
                p.op("dve", lambda e, tl=tl: e.tensor_scalar(out=comb[:, tl, :], in0=sc[:], scalar1=e1[:, 0:1], scalar2=None, op0=ALU.mult),
                     reads=[r_sc, r_e1], writes=[r_comb])
        wgu = [sb(f"wgu{i}", [128, KC, 1024], BF16) for i in range(2)]
        wd = [sb(f"wd{i}", [128, 4, D], BF16) for i in range(2)]
        r_wgu = [p.region() for _ in range(2)]
        r_wd = [p.region() for _ in range(2)]
        sg = [sb(f"sg{i}", [128, 512], F32) for i in range(2)]
        he = [sb(f"he{i}", [128, 512], BF16) for i in range(2)]
        heT = [sb(f"heT{i}", [128, 4, 128], BF16) for i in range(2)]
        yt = [sb(f"yt{i}", [128, D], F32) for i in range(2)]
        r_sg = [p.region() for _ in range(2)]
        r_he = [p.region() for _ in range(2)]
        r_heT = [p.region() for _ in range(2)]
        r_yt = [p.region() for _ in range(2)]
        def load_w(ex):
            w = ex % 2
            p.dma("pool", wgu[w][:, :, 0:512], wg_l[ex].rearrange("(k p) f -> p k f", p=128), writes=[r_wgu[w]])
            p.dma("pool", wgu[w][:, :, 512:1024], wu_l[ex].rearrange("(k p) f -> p k f", p=128), writes=[r_wgu[w]])
            p.dma("pool", wd[w][:], wd_l[ex].rearrange("(k p) c -> p k c", p=128), writes=[r_wd[w]])

        steps = [(ex, blk, tt) for ex in range(16) for blk in range(NT // 512) for tt in range(4)]
        hb_of = {}

        def gate_up(n):
            ex, blk, tt = steps[n]
            w = ex % 2
            if tt == 0:
                i = (ib0[0]) % 2
                ib0[0] += 1
                hb_of[(ex, blk)] = i
                p.dma("sp", hb[i][:], h2T[:, blk * 512:(blk + 1) * 512].rearrange("(k p) t -> p k t", p=128), reads=[r_h2T], writes=[r_hb[i]])
            i = hb_of[(ex, blk)]
            pr = 2 * (n % 2)
            for half in range(2):
                for k in range(KC):
                    p.op("pe", lambda e, k=k, i=i, tt=tt, w=w, half=half, pr=pr: e.matmul(
                        cx.banks[pr + half][:], hb[i][:, k, tt * 128:(tt + 1) * 128], wgu[w][:, k, half * 512:(half + 1) * 512], start=(k == 0), stop=(k == KC - 1)),
                        reads=[r_hb[i], r_wgu[w]], writes=[cx.bank_r[pr + half]], pe_acc=(k > 0))
        ib0 = [ib]
        load_w(0)
        gate_up(0)
        for n, (ex, blk, tt) in enumerate(steps):
            w = ex % 2
            if blk == 0 and tt == 0 and ex + 1 < 16:
                load_w(ex + 1)
            if n + 1 < len(steps):
                gate_up(n + 1)
            tl = blk * 4 + tt
            o = n % 2
            pr = 2 * (n % 2)
            p.op("act", lambda e, o=o, pr=pr: e.activation(out=sg[o][:], in_=cx.banks[pr][:], func=AF.Silu), reads=[cx.bank_r[pr]], writes=[r_sg[o]])
            p.op("dve", lambda e, o=o, tl=tl, ex=ex, pr=pr: e.scalar_tensor_tensor(out=he[o][:], in0=cx.banks[pr + 1][:], scalar=comb[:, tl, ex:ex + 1], in1=sg[o][:],
                                                                             op0=ALU.mult, op1=ALU.mult),
                 reads=[cx.bank_r[pr + 1], r_comb, r_sg[o]], writes=[r_he[o]])
            bt = pr
            for kk in range(4):
                p.op("pe", lambda e, kk=kk, o=o, bt=bt: e.transpose(out=cx.banks[bt][:].bitcast(BF16)[:, kk * 128:(kk + 1) * 128], in_=he[o][:, kk * 128:(kk + 1) * 128],
                                                                  identity=cx.ident_b[:]),
                     reads=[r_he[o], cx.r_ident], writes=[cx.bank_r[bt]], pe_acc=(kk > 0))
            p.op("act", lambda e, o=o, bt=bt: e.activation(out=heT[o][:].rearrange("p k t -> p (k t)"), in_=cx.banks[bt][:].bitcast(BF16)[:, 0:512], func=AF.Copy),
                 reads=[cx.bank_r[bt]], writes=[r_heT[o]])
            for nn in range(4):
                for kk in range(4):
                    p.op("pe", lambda e, nn=nn, kk=kk, o=o, w=w: e.matmul(cx.banks[4 + nn][:], heT[o][:, kk, :], wd[w][:, kk, nn * 512:(nn + 1) * 512],
                                                                        start=(kk == 0), stop=(kk == 3)),
                         reads=[r_heT[o], r_wd[w]], writes=[cx.bank_r[4 + nn]], pe_acc=(kk > 0))
                if nn % 2 == 0:
                    p.op("act", lambda e, nn=nn, o=o: e.activation(out=yt[o][:, nn * 512:(nn + 1) * 512], in_=cx.banks[4 + nn][:], func=AF.Copy),
                         reads=[cx.bank_r[4 + nn]], writes=[r_yt[o]])
                else:
                    p.op("dve", lambda e, nn=nn, o=o: e.tensor_copy(out=yt[o][:, nn * 512:(nn + 1) * 512], in_=cx.banks[4 + nn][:]),
                         reads=[cx.bank_r[4 + nn]], writes=[r_yt[o]])
            if ex == 0 or MOE_DIAG == 2:
                p.dma("pool", yacc[tl * 128:(tl + 1) * 128, :], yt[o][:], reads=[r_yt[o]], writes=[r_y], store=True)
            elif MOE_DIAG == 1:
                pass
            else:
                p.dma("pool", yacc[tl * 128:(tl + 1) * 128, :], yt[o][:], reads=[r_yt[o]], writes=[r_y], accum_op=ALU.add, store=True)
        p.barrier()


def stage_resid(p, cx, xs, yacc, modD, l, goff, NB, S, r_x, r_y, final_g=None, out=None, r_out=None):
    with contextlib.ExitStack() as st:
        def sb(name, shape, dt):
            return st.enter_context(p.sbt("R" + name, list(shape), dt))
        gb = sb("gb", [128, D], F32)
        r_gb = p.region()
        fg = sb("fg", [128, D], F32)
        r_fg = p.region()
        if final_g is not None:
            p.dma("sp", fg[:], bcast_rows(final_g, 128), writes=[r_fg])
        xt = [sb(f"xt{i}", [128, D], F32) for i in range(2)]
        yt = [sb(f"yt{i}", [128, D], F32) for i in range(2)]
        r_xt = [p.region() for _ in range(2)]
        r_yt = [p.region() for _ in range(2)]
        junk = sb("junk", [128, D], BF16)
        r_junk = p.region()
        ss = [sb(f"ss{i}", [128, 1], F32) for i in range(2)]
        r_ss = [p.region() for _ in range(2)]
        it = 0
        for b in range(NB):
            p.dma("sp", gb[:], bcast_rows(modD[l, b, goff:goff + D], 128), reads=[cx.r_modD], writes=[r_gb])
            for tt in range(S // 128):
                i = it % 2
                it += 1
                t0 = b * S + tt * 128
                p.dma("sp", xt[i][:], xs[t0:t0 + 128, :], reads=[r_x], writes=[r_xt[i]])
                p.dma("sp", yt[i][:], yacc[t0:t0 + 128, :], reads=[r_y], writes=[r_yt[i]])
                p.op("dve", lambda e, i=i: e.tensor_tensor(out=yt[i][:], in0=yt[i][:], in1=gb[:], op=ALU.mult), reads=[r_yt[i], r_gb], writes=[r_yt[i]])
                p.op("dve", lambda e, i=i: e.tensor_tensor(out=xt[i][:], in0=xt[i][:], in1=yt[i][:], op=ALU.add), reads=[r_yt[i], r_xt[i]], writes=[r_xt[i]])
                if final_g is None:
                    p.dma("pool", xs[t0:t0 + 128, :], xt[i][:], reads=[r_xt[i]], writes=[r_x], store=True)
                else:
                    p.op("act", lambda e, i=i: e.activation(out=junk[:], in_=xt[i][:], func=AF.Square, accum_out=ss[i][:]),
                         reads=[r_xt[i]], writes=[r_junk, r_ss[i]])
                    p.op("dve", lambda e, i=i: e.tensor_scalar(out=ss[i][:], in0=ss[i][:], scalar1=1.0 / D, scalar2=1e-6, op0=ALU.mult, op1=ALU.add),
                         reads=[r_ss[i]], writes=[r_ss[i]])
                    p.op("act", lambda e, i=i: e.activation(out=ss[i][:], in_=ss[i][:], func=AF.Sqrt), reads=[r_ss[i]], writes=[r_ss[i]])
                    p.op("dve", lambda e, i=i: e.reciprocal(out=ss[i][:], in_=ss[i][:]), reads=[r_ss[i]], writes=[r_ss[i]])
                    p.op("dve", lambda e, i=i: e.scalar_tensor_tensor(out=yt[i][:], in0=xt[i][:], scalar=ss[i][:, 0:1], in1=fg[:], op0=ALU.mult, op1=ALU.mult),
                         reads=[r_xt[i], r_ss[i], r_fg], writes=[r_yt[i]])
                    p.dma("pool", out[t0:t0 + 128, :], yt[i][:], reads=[r_yt[i]], writes=[r_out], store=True)
        p.barrier()


CONST_SPECS = None


def build_program(NB, S, NL):
    NT = NB * S
    nc = bass.Bass("TRN2", target_bir_lowering=False)
    p = Prog(nc)

    def ext(name, shape, dt=F32):
        return nc.dram_tensor(name, list(shape), dt, kind="ExternalInput").ap()

    def internal(name, shape, dt=F32):
        return nc.dram_tensor(name, list(shape), dt, kind="Internal").ap()
    consts = ext("consts", [128, 128])
    x = ext("x", [NT, D])
    c = ext("c", [NB, D])
    strips = ext("strips", [32, 128, W_STRIP])
    cst = {"mults": ext("mults", [4, 128, W_STRIP]), "moba_valid": ext("moba_valid", [256]), "moba_own": ext("moba_own", [256]),
           "moba_koh": ext("moba_koh", [16, S], BF16), "nsa_koh": ext("nsa_koh", [64, S], BF16), "cmp_mask": ext("cmp_mask", [256, S], BF16),
           "overlap": ext("overlap", [256, 64], BF16), "sel_mul": ext("sel_mul", [S, 64]), "sel_add": ext("sel_add", [S, 64])}
    router_w = ext("router_w", [D, 16])
    router_bias = ext("router_bias", [16])
    norm1_g = ext("norm1_g", [NL, D])
    norm2_g = ext("norm2_g", [NL, D])
    ada_w = ext("ada_w", [NL, D, 6 * D])
    ada_b = ext("ada_b", [NL, 6 * D])
    w_in = ext("w_in", [NL, D, 5144])
    pek = ext("nsa_pe_k", [NL, 32, 64])
    pev = ext("nsa_pe_v", [NL, 32, 64])
    w1k = ext("nsa_cmp_w1_k", [NL, 2048, 128])
    w2k = ext("nsa_cmp_w2_k", [NL, 128, 64])
    w1v = ext("nsa_cmp_w1_v", [NL, 2048, 128])
    w2v = ext("nsa_cmp_w2_v", [NL, 128, 64])
    sinks = ext("sinks", [NL, 8])
    mixg = ext("mix_norm_g", [NL, D])
    w_out = ext("w_out", [NL, D, D])
    wg = ext("exp_w_gate", [NL, 16, D, 512])
    wu = ext("exp_w_up", [NL, 16, D, 512])
    wd = ext("exp_w_down", [NL, 16, 512, D])
    final_g = ext("final_g", [D])
    y = nc.dram_tensor("y", [NT, D], F32, kind="ExternalOutput").ap()
    modD = internal("modD", [NL, NB, 6 * D])
    xs = internal("xs", [NT, D])
    hT = internal("hT", [D, NT], BF16)
    FT = internal("FT", [FT_COLS, NT], BF16)
    TM = internal("TM", [NT, TM_COLS], BF16)
    OD = internal("OD", [NT, D])
    yacc = internal("yacc", [NT, D])
    EBD = internal("EBD", [40, 128, W_STRIP], BF16)
    cx = setup_ctx(p, consts)
    cx.r_modD = p.region("modD")
    r_x, r_hT, r_FT, r_TM, r_OD, r_y, r_out = (p.region() for _ in range(7))
    stage_mod(p, cx, c, ada_w, ada_b, modD, NL, NB)
    stage_eb(p, cx, strips, cst, EBD, p.region())
    for l in range(NL):
        x_src = x if l == 0 else xs
        for b in range(NB):
            stage_norm_T(p, cx, x_src, b * S, S, norm1_g[l:l + 1, :], modD[l, b:b + 1, D:2 * D], modD[l, b:b + 1, 0:D], hT, b * S, "A", r_x, r_hT)
        if 'proj' not in SKIP:
            stage_proj(p, cx, w_in[l], hT, FT, TM, NT, r_hT, r_FT, r_TM)
        for b in range(NB if 'attn' not in SKIP else 0):
            stage_attn(p, cx, cst, FT, TM, OD, S, b, EBD, sinks[l], (w1k[l], w2k[l], w1v[l], w2v[l], pek[l], pev[l]), r_FT, r_TM, r_OD)
        if 'mix' not in SKIP:
          stage_mix_out(p, cx, OD, x_src, xs, mixg[l:l + 1, :], w_out[l], modD, l, NB, S, r_OD, r_x)
        for b in range(NB):
            stage_norm_T(p, cx, xs, b * S, S, norm2_g[l:l + 1, :], modD[l, b:b + 1, 4 * D:5 * D], modD[l, b:b + 1, 3 * D:4 * D], hT, b * S, "N", r_x, r_hT)
        if 'moe' not in SKIP:
          stage_moe(p, cx, hT, NT, router_w, router_bias, wg[l], wu[l], wd[l], yacc, r_hT, r_y)
        last = (l == NL - 1)
        stage_resid(p, cx, xs, yacc, modD, l, 5 * D, NB, S, r_x, r_y, final_g=(final_g if last else None), out=y, r_out=r_out)
    p.finish()
    return nc, p


N_CORES = 4
SKIP = set()
MOE_DIAG = 0
ATT_DIAG = 0
BIAS_ON_PE = True
PV_DELAY = 2


def kernel(**inputs):
    import ml_dtypes
    B, S, _ = inputs["x"].shape
    NL = inputs["ada_w"].shape[0]
    NB = B // N_CORES
    nc, prog = build_program(NB, S, NL)
    f32 = lambda a: np.ascontiguousarray(np.asarray(a), dtype=np.float32)
    cst, strips = make_consts(S, f32(inputs["rel_bias"]))
    shared = {"consts": np.eye(128, dtype=np.float32), "strips": strips, **cst}
    for k in ("router_w", "router_bias", "norm1_g", "norm2_g", "ada_w", "ada_b", "w_in", "nsa_pe_k", "nsa_pe_v", "nsa_cmp_w1_k", "nsa_cmp_w2_k",
              "nsa_cmp_w1_v", "nsa_cmp_w2_v", "sinks", "w_out", "exp_w_gate", "exp_w_up", "exp_w_down", "final_g"):
        shared[k] = f32(inputs[k])
    shared["mix_norm_g"] = f32(inputs["mix_norm_g"]).reshape(NL, D)
    xin = f32(inputs["x"])
    cin = f32(inputs["c"])
    in_maps = []
    for ci in range(N_CORES):
        m = dict(shared)
        m["x"] = xin[ci * NB:(ci + 1) * NB].reshape(NB * S, D)
        m["c"] = cin[ci * NB:(ci + 1) * NB]
        in_maps.append(m)
    res = run_bass_kernel_spmd(nc, in_maps, core_ids=list(range(N_CORES)))
    out = np.concatenate([res.results[ci]["y"].reshape(NB, S, D) for ci in range(N_CORES)], axis=0)
    return out.astype(np.float32)
```

```python
import contextlib
import math
import numpy as np
import concourse.bass as bass
import concourse.mybir as mybir
from concourse.bass_utils import run_bass_kernel_spmd

F32 = mybir.dt.float32
BF16 = mybir.dt.bfloat16
AF = mybir.ActivationFunctionType
ALU = mybir.AluOpType
AX = mybir.AxisListType

D = 2048
KC = D // 128
HD = 64
NEG = -30000.0
IN_SIZES = (512, 512, 512, 512, 128, 128, 512, 512, 512, 512, 128, 128, 128, 128, 128, 128, 24)
IN_OFFS = [0] + list(np.cumsum(IN_SIZES))
(QA, KA, VA, QB, KB_, VB, QC, KC_, VC, QD, KDC, VDC, KDS, VDS, KDW, VDW, GD) = range(17)
FT_ORDER = [QA, KA, QB, KB_, QC, KC_, QD, KDC, VDC, KDS, KDW]
TM_ORDER = [VA, VB, VC, VDS, VDW, GD]
FT_COLS = sum(IN_SIZES[i] for i in FT_ORDER)
TM_COLS = sum(IN_SIZES[i] for i in TM_ORDER)
FT_OFF = {}
_o = 0
for _i in FT_ORDER:
    FT_OFF[_i] = _o
    _o += IN_SIZES[_i]
TM_OFF = {}
_o = 0
for _i in TM_ORDER:
    TM_OFF[_i] = _o
    _o += IN_SIZES[_i]


class Region:
    __slots__ = ("name", "last_w", "readers")

    def __init__(self, name):
        self.name = name
        self.last_w = None
        self.readers = {}


class Prog:
    ENG = ("pe", "act", "dve", "pool", "sp")
    NDMA = 88

    def __init__(self, nc):
        self.nc = nc
        self.ops = {e: [] for e in self.ENG}
        self.cnt = {e: 0 for e in self.ENG}
        self.seen = {e: {} for e in self.ENG}
        self.dma_cnt = [0] * self.NDMA
        self.dma_rr = 0
        self.stack = contextlib.ExitStack()
        self.nreg = 0
        self.out_events = []

    def sbt(self, name, shape, dtype):
        self.nuniq = getattr(self, "nuniq", 0) + 1
        return self.nc.sbuf_tensor(f"{name}_{self.nuniq}", list(shape), dtype)

    def region(self, name=None):
        self.nreg += 1
        return Region(name or f"r{self.nreg}")

    def sb(self, name, shape, dtype):
        t = self.stack.enter_context(self.nc.sbuf_tensor(name, list(shape), dtype))
        return t

    def ps(self, name, shape, dtype=F32):
        return self.stack.enter_context(self.nc.psum_tensor(name, list(shape), dtype))

    def dram(self, name, shape, dtype, kind="Internal"):
        return self.nc.dram_tensor(name, list(shape), dtype, kind=kind).ap()

    def _deps(self, reads, writes, pe_acc=False, eng=None):
        deps = {}

        def add(ev):
            if ev is None:
                return
            k, v = ev
            if deps.get(k, 0) < v:
                deps[k] = v
        for r in reads:
            add(r.last_w)
        for w in writes:
            if not (pe_acc and w.last_w is not None and w.last_w[0] == eng):
                add(w.last_w)
            for k, v in w.readers.items():
                add((k, v))
        return deps

    def _commit(self, ev, reads, writes):
        for r in reads:
            k, v = ev
            if r.readers.get(k, 0) < v:
                r.readers[k] = v
        for w in writes:
            w.last_w = ev
            w.readers = {}

    def _waits(self, eng, deps):
        waits = []
        for k, v in deps.items():
            if self.seen[eng].get(k, 0) < v:
                self.seen[eng][k] = v
                waits.append((k, v))
        return waits

    def op(self, eng, fn, reads=(), writes=(), pe_acc=False):
        deps = self._deps(reads, writes, pe_acc, eng)
        waits = self._waits(eng, deps)
        self.cnt[eng] += 1
        ev = (eng, self.cnt[eng])
        self.ops[eng].append((waits, fn, (eng, 1)))
        self._commit(ev, reads, writes)
        return ev

    def dma(self, q, out, in_, reads=(), writes=(), store=False, **kw):
        if store:
            deps = self._deps(reads, ())
            for w in writes:
                for k, v in w.readers.items():
                    if deps.get(k, 0) < v:
                        deps[k] = v
            reg = reads[0]
        else:
            deps = self._deps(reads, writes)
            reg = writes[0]
        waits = self._waits(q, deps)
        if not hasattr(self, "reg_sem"):
            self.reg_sem = {}
        if id(reg) not in self.reg_sem:
            self.reg_sem[id(reg)] = len(self.reg_sem) % self.NDMA
        j = self.reg_sem[id(reg)]
        self.dma_cnt[j] += 16
        ev = (("dma", j), self.dma_cnt[j])
        self.ops[q].append((waits, lambda e: e.dma_start(out=out, in_=in_, **kw), (("dma", j), 16)))
        self._commit(ev, reads, writes)
        return ev

    def barrier(self):
        evs = {}
        for e in self.ENG:
            if self.cnt[e]:
                evs[e] = self.cnt[e]
        for j in range(self.NDMA):
            if self.dma_cnt[j]:
                evs[("dma", j)] = self.dma_cnt[j]
        for e in self.ENG:
            waits = self._waits(e, {k: v for k, v in evs.items() if k != e})
            if waits:
                self.ops[e].append((waits, None, None))

    def finish(self):
        nc = self.nc
        sems = {}
        for e in self.ENG:
            sems[e] = self.stack.enter_context(nc.semaphore("sem_" + e))
        for j in range(self.NDMA):
            sems[("dma", j)] = self.stack.enter_context(nc.semaphore(f"sem_dma{j}"))
        final = []
        for e in self.ENG:
            if self.cnt[e] and e != "sp":
                final.append((e, self.cnt[e]))
        for j in range(self.NDMA):
            if self.dma_cnt[j]:
                final.append((("dma", j), self.dma_cnt[j]))
        ops = self.ops
        with nc.Block() as block:
            def run(engobj, name):
                for waits, fn, inc in ops[name]:
                    for k, v in waits:
                        engobj.wait_ge(sems[k], v)
                    if fn is None:
                        continue
                    ins = fn(engobj)
                    ins.then_inc(sems[inc[0]], inc[1])

            @block.tensor
            def _(t):
                run(t, "pe")

            @block.scalar
            def _(s):
                run(s, "act")

            @block.vector
            def _(v):
                run(v, "dve")

            @block.gpsimd
            def _(g):
                run(g, "pool")

            @block.sync
            def _(sp):
                run(sp, "sp")
                for k, v in final:
                    sp.wait_ge(sems[k], v)
        self.stack.close()


class Ctx:
    pass


def t5_bucket_np(dist):
    dist = np.maximum(dist, 0)
    max_exact = 16
    with np.errstate(divide="ignore"):
        large = max_exact + (np.log(np.maximum(dist, 1).astype(np.float32) / np.float32(max_exact))
                             / np.float32(math.log(2048 / max_exact)) * np.float32(32 - max_exact)).astype(np.int32)
    large = np.minimum(large, 31)
    return np.where(dist < max_exact, dist, large)


def setup_ctx(p, consts_ap):
    cx = Ctx()
    cx.banks = [p.ps(f"bank{i}", [128, 512], F32) for i in range(8)]
    cx.bank_r = [p.region(f"bank{i}") for i in range(8)]
    cx.ident_f = p.sb("ident_f", [128, 128], F32)
    cx.ident_b = p.sb("ident_b", [128, 128], BF16)
    cx.r_ident = p.region("ident")
    p.dma("sp", cx.ident_f[:], consts_ap[0:128, 0:128], writes=[cx.r_ident])
    p.op("dve", lambda e: e.tensor_copy(out=cx.ident_b[:], in_=cx.ident_f[:]), reads=[cx.r_ident], writes=[cx.r_ident])
    return cx


def stage_mod(p, cx, c_ap, ada_w, ada_b, modD, NL, NB):
    with contextlib.ExitStack() as st:
        def sb(name, shape, dt):
            return st.enter_context(p.sbt(name, list(shape), dt))
        cT = sb("m_cT", [128, KC, NB], F32)
        r_cT = p.region()
        for b in range(NB):
            p.dma("sp", cT[:, :, b], c_ap[b].rearrange("(k p) -> p k", p=128), writes=[r_cT], allow_slow_non_contiguous=True) \
                if False else p.dma("sp", cT[:, :, b:b + 1], c_ap[b:b + 1, :].rearrange("b (k p) -> p k b", p=128), writes=[r_cT],
                                    allow_slow_non_contiguous=True)
        p.op("act", lambda e: e.activation(out=cT[:], in_=cT[:], func=AF.Silu), reads=[r_cT], writes=[r_cT])
        wt = [sb(f"m_w{i}", [128, KC, 512], F32) for i in range(2)]
        r_w = [p.region() for _ in range(2)]
        bt = [sb(f"m_b{i}", [NB, 512], F32) for i in range(2)]
        r_b = [p.region() for _ in range(2)]
        ot = [sb(f"m_o{i}", [NB, 512], F32) for i in range(2)]
        r_o = [p.region() for _ in range(2)]
        it = 0
        for l in range(NL):
            for cc in range(6 * D // 512):
                i = it % 2
                it += 1
                p.dma(("act" if (MOD_Q2 and it % 2 == 0) else "sp"), wt[i][:], ada_w[l, :, cc * 512:(cc + 1) * 512].rearrange("(k p) c -> p k c", p=128), writes=[r_w[i]])
                for b in range(NB):
                    p.dma("sp", bt[i][b:b + 1, :], ada_b[l:l + 1, cc * 512:(cc + 1) * 512], writes=[r_b[i]])
                bk = cx.banks[i]
                for k in range(KC):
                    p.op("pe", lambda e, k=k, i=i, bk=bk: e.matmul(bk[0:NB, :], cT[:, k, :], wt[i][:, k, :], start=(k == 0), stop=(k == KC - 1)),
                         reads=[r_cT, r_w[i]], writes=[cx.bank_r[i]], pe_acc=(k > 0))
                p.op("dve", lambda e, i=i, bk=bk: e.tensor_tensor(out=ot[i][:], in0=bk[0:NB, :], in1=bt[i][:], op=ALU.add),
                     reads=[cx.bank_r[i], r_b[i]], writes=[r_o[i]])
                p.dma("sp", modD[l, :, cc * 512:(cc + 1) * 512], ot[i][:], reads=[r_o[i]], writes=[cx.r_modD], store=True)
        p.barrier()


def load_cols(p, sbt, r, src_row):
    p.dma("sp", sbt[:].rearrange("p (k o) -> p k o", o=1), src_row.rearrange("o (k p) -> p k o", p=128), writes=[r],
          allow_slow_non_contiguous=True)


def stage_norm_T(p, cx, x_src, tok0, ntok, g_row, sc_row, sh_row, hT, hcol0, tag, r_x, r_hT):
    with contextlib.ExitStack() as st:
        def sb(name, shape, dt):
            return st.enter_context(p.sbt(tag + name, list(shape), dt))
        gc = sb("gc", [128, KC], F32)
        sc = sb("sc", [128, KC], F32)
        sh = sb("sh", [128, KC], F32)
        r_c = p.region()
        load_cols(p, gc, r_c, g_row)
        load_cols(p, sc, r_c, sc_row)
        load_cols(p, sh, r_c, sh_row)
        p.op("dve", lambda e: e.scalar_tensor_tensor(out=sc[:], in0=sc[:], scalar=1.0, in1=gc[:], op0=ALU.add, op1=ALU.mult),
             reads=[r_c], writes=[r_c])
        xt = [sb(f"xt{i}", [128, D], F32) for i in range(2)]
        r_xt = [p.region() for _ in range(2)]
        junk = sb("junk", [128, D], BF16)
        r_junk = p.region()
        ss = [sb(f"ss{i}", [128, 1], F32) for i in range(2)]
        r_ss = [p.region() for _ in range(2)]
        xn = [sb(f"xn{i}", [128, D], BF16) for i in range(4)]
        r_xn = [p.region() for _ in range(4)]
        hs = [sb(f"hs{i}", [128, KC, 512], BF16) for i in range(2)]
        r_hs = [p.region() for _ in range(2)]
        nblk = ntok // 512
        it = 0
        for blk in range(nblk):
            hb = blk % 2
            for tt in range(4):
                i = it % 2
                it += 1
                t0 = tok0 + blk * 512 + tt * 128
                p.dma("sp", xt[i][:], x_src[t0:t0 + 128, :], reads=[r_x], writes=[r_xt[i]])
                p.op("act", lambda e, i=i: e.activation(out=junk[:], in_=xt[i][:], func=AF.Square, accum_out=ss[i][:]),
                     reads=[r_xt[i]], writes=[r_junk, r_ss[i]])
                p.op("dve", lambda e, i=i: e.tensor_scalar(out=ss[i][:], in0=ss[i][:], scalar1=1.0 / D, scalar2=1e-6, op0=ALU.mult, op1=ALU.add),
                     reads=[r_ss[i]], writes=[r_ss[i]])
                p.op("act", lambda e, i=i: e.activation(out=ss[i][:], in_=ss[i][:], func=AF.Sqrt), reads=[r_ss[i]], writes=[r_ss[i]])
                p.op("dve", lambda e, i=i: e.reciprocal(out=ss[i][:], in_=ss[i][:]), reads=[r_ss[i]], writes=[r_ss[i]])
                p.op("dve", lambda e, i=i, tt=tt: e.tensor_scalar(out=xn[tt][:], in0=xt[i][:], scalar1=ss[i][:, 0:1], scalar2=None, op0=ALU.mult),
                     reads=[r_ss[i], r_xt[i]], writes=[r_xn[tt]])
            for j in range(KC):
                bk = 4 + j % 4
                for tt in range(4):
                    p.op("pe", lambda e, j=j, tt=tt, bk=bk: e.transpose(
                        out=cx.banks[bk][:].bitcast(BF16)[:, tt * 128:(tt + 1) * 128], in_=xn[tt][:, j * 128:(j + 1) * 128], identity=cx.ident_b[:]),
                        reads=[r_xn[tt], cx.r_ident], writes=[cx.bank_r[bk]], pe_acc=(tt > 0))
                p.op("act", lambda e, j=j, bk=bk, hb=hb: e.activation(
                    out=hs[hb][:, j, :], in_=cx.banks[bk][:].bitcast(BF16)[:, 0:512], func=AF.Identity, scale=sc[:, j:j + 1], bias=sh[:, j:j + 1]),
                    reads=[cx.bank_r[bk], r_c], writes=[r_hs[hb]])
            c0 = hcol0 + blk * 512
            p.dma("pool", hT[:, c0:c0 + 512].rearrange("(k p) t -> p k t", p=128), hs[hb][:], reads=[r_hs[hb]], writes=[r_hT], store=True)
        p.barrier()


def stage_proj(p, cx, w_in_l, hT, FT, TM, ntok, r_hT, r_FT, r_TM):
    with contextlib.ExitStack() as st:
        def sb(name, shape, dt):
            return st.enter_context(p.sbt("B" + name, list(shape), dt))
        wgs = [sb(f"wg{i}", [128, KC, 1024], BF16) for i in range(2)]
        r_wgs = [p.region() for _ in range(2)]
        hb = [sb(f"hb{i}", [128, KC, 512], BF16) for i in range(2)]
        r_hb = [p.region() for _ in range(2)]
        og = [sb(f"og{i}", [128, 512], BF16) for i in range(2)]
        r_og = [p.region() for _ in range(2)]
        nblk = ntok // 512
        chunks = []
        for seg in FT_ORDER:
            for c in range(IN_SIZES[seg] // 128):
                chunks.append((IN_OFFS[seg] + c * 128, seg in (QA, QB, QC, QD)))
        ngrp = (len(chunks) + 7) // 8
        it = 0
        ib = 0
        def load_group(g):
            for j, (c0, isq) in enumerate(chunks[g * 8:(g + 1) * 8]):
                p.dma("pool", wgs[g % 2][:, :, j * 128:(j + 1) * 128], w_in_l[:, c0:c0 + 128].rearrange("(k p) c -> p k c", p=128), writes=[r_wgs[g % 2]])
        wt = sb("wt", [128, KC, TM_COLS], BF16)
        r_wt = p.region()
        load_group(0)
        for g in range(ngrp):
            gch = chunks[g * 8:(g + 1) * 8]
            wg, r_wg = wgs[g % 2], r_wgs[g % 2]
            if g + 1 < ngrp:
                load_group(g + 1)
            else:
                for seg in TM_ORDER:
                    p.dma("pool", wt[:, :, TM_OFF[seg]:TM_OFF[seg] + IN_SIZES[seg]],
                          w_in_l[:, IN_OFFS[seg]:IN_OFFS[seg] + IN_SIZES[seg]].rearrange("(k p) c -> p k c", p=128), writes=[r_wt])
            for blk in range(nblk):
                i = ib % 2
                ib += 1
                p.dma("sp", hb[i][:], hT[:, blk * 512:(blk + 1) * 512].rearrange("(k p) t -> p k t", p=128), reads=[r_hT], writes=[r_hb[i]])
                for j, (c0, isq) in enumerate(gch):
                    bk = it % 4
                    o = it % 2
                    it += 1
                    for k in range(KC):
                        p.op("pe", lambda e, j=j, k=k, i=i, bk=bk, wg=wg: e.matmul(cx.banks[bk][:], wg[:, k, j * 128:(j + 1) * 128], hb[i][:, k, :],
                                                                        start=(k == 0), stop=(k == KC - 1)),
                             reads=[r_wg, r_hb[i]], writes=[cx.bank_r[bk]], pe_acc=(k > 0))
                    p.op("act", lambda e, bk=bk, o=o, isq=isq: e.activation(out=og[o][:], in_=cx.banks[bk][:], func=AF.Copy, scale=(0.125 if isq else 1.0)),
                         reads=[cx.bank_r[bk]], writes=[r_og[o]])
                    row = (g * 8 + j) * 128
                    p.dma("sp", FT[row:row + 128, blk * 512:(blk + 1) * 512], og[o][:], reads=[r_og[o]], writes=[r_FT], store=True)
        ot = [sb(f"ot{i}", [128, TM_COLS], BF16) for i in range(2)]
        r_ot = [p.region() for _ in range(2)]
        cgs = [(0, 512), (512, 512), (1024, TM_COLS - 1024)]
        itt = 0
        for blk in range(nblk):
            i = ib % 2
            ib += 1
            p.dma("sp", hb[i][:], hT[:, blk * 512:(blk + 1) * 512].rearrange("(k p) t -> p k t", p=128), reads=[r_hT], writes=[r_hb[i]])
            for tt in range(4):
                o = itt % 2
                itt += 1
                for (c0, cn) in cgs:
                    bk = it % 4
                    it += 1
                    for k in range(KC):
                        p.op("pe", lambda e, k=k, i=i, bk=bk, tt=tt, c0=c0, cn=cn: e.matmul(
                            cx.banks[bk][:, 0:cn], hb[i][:, k, tt * 128:(tt + 1) * 128], wt[:, k, c0:c0 + cn], start=(k == 0), stop=(k == KC - 1)),
                            reads=[r_wt, r_hb[i]], writes=[cx.bank_r[bk]], pe_acc=(k > 0))
                    p.op("dve", lambda e, bk=bk, o=o, c0=c0, cn=cn: e.tensor_copy(out=ot[o][:, c0:c0 + cn], in_=cx.banks[bk][:, 0:cn]),
                         reads=[cx.bank_r[bk]], writes=[r_ot[o]])
                t0 = blk * 512 + tt * 128
                p.dma("sp", TM[t0:t0 + 128, :], ot[o][:], reads=[r_ot[o]], writes=[r_TM], store=True)
        p.barrier()


W_STRIP = 3072
NSA_ONLY = None
DBG = None
OFF_STRIP = 384


def attn_core(p, cx, pT, r_pT, S, QT, r_q, KT, r_k, Kc, V, r_v, nv, eb, r_eb, clamp, ktiles_fn, skip_fn, out_cb, cmask=None, r_cm=None):
    steps = []
    nqc = S // 512
    for qc in range(nqc):
        kts = ktiles_fn(qc)
        for kt in kts:
            steps.append((qc, kt))
    used = {}
    for qc in range(nqc):
        for tq in range(4):
            used[(qc, tq)] = [kt for kt in ktiles_fn(qc) if not skip_fn(kt, qc * 4 + tq)]

    SB = (0, 1, 6, 7)
    NP = len(pT)

    def qk(s):
        qc, kt = steps[s]
        bk = SB[s % 4]
        extra = BIAS_ON_PE and ((eb is not None) or (cmask is not None))
        p.op("pe", lambda e, qc=qc, kt=kt, bk=bk, extra=extra: e.matmul(cx.banks[bk][:], KT[0:Kc, kt * 128:(kt + 1) * 128], QT[0:Kc, qc * 512:(qc + 1) * 512],
                                                                      start=True, stop=(not extra)),
             reads=[r_q, r_k], writes=[cx.bank_r[bk]])
        if eb is not None and BIAS_ON_PE:
            b = qc * 512 - kt * 128 + OFF_STRIP
            if clamp and b > 2048:
                b = 2048
            p.op("pe", lambda e, bk=bk, b=b: e.matmul(cx.banks[bk][:], cx.ident_b[:], eb[:, b:b + 512], start=False, stop=True),
                 reads=[r_eb, cx.r_ident], writes=[cx.bank_r[bk]], pe_acc=True)
        if cmask is not None and BIAS_ON_PE:
            p.op("pe", lambda e, bk=bk, kt=kt, qc=qc: e.matmul(cx.banks[bk][:], cx.ident_b[:], cmask[:, kt, qc * 512:(qc + 1) * 512], start=False, stop=True),
                 reads=[r_cm, cx.r_ident], writes=[cx.bank_r[bk]], pe_acc=True)
    def front(s):
        qc, kt = steps[s]
        bk = SB[s % 4]
        pb = s % NP
        W_ = 128 if ATT_DIAG == 4 else 512
        p.op("act", lambda e, bk=bk, pb=pb, W_=W_: e.activation(out=pT[pb][:, 0:W_], in_=cx.banks[bk][:, 0:W_], func=AF.Exp), reads=[cx.bank_r[bk]], writes=[r_pT[pb]])
        if not BIAS_ON_PE:
            if eb is not None:
                b = qc * 512 - kt * 128 + OFF_STRIP
                if clamp and b > 2048:
                    b = 2048
                p.op("dve", lambda e, pb=pb, b=b: e.tensor_tensor(out=pT[pb][:], in0=pT[pb][:], in1=eb[:, b:b + 512], op=ALU.mult),
                     reads=[r_eb, r_pT[pb]], writes=[r_pT[pb]])
            if cmask is not None:
                p.op("dve", lambda e, pb=pb, kt=kt, qc=qc: e.tensor_tensor(out=pT[pb][:], in0=pT[pb][:], in1=cmask[:, kt, qc * 512:(qc + 1) * 512], op=ALU.mult),
                     reads=[r_cm, r_pT[pb]], writes=[r_pT[pb]])

    def back(s):
        qc, kt = steps[s]
        pb = s % NP
        for tq in range(4):
            ul = used[(qc, tq)]
            if kt not in ul:
                continue
            ob = 2 + tq
            first = (kt == ul[0])
            last = (kt == ul[-1])
            p.op("pe", lambda e, pb=pb, tq=tq, kt=kt, ob=ob, first=first, last=last: e.matmul(
                cx.banks[ob][:, 0:nv], pT[pb][:, tq * 128:(tq + 1) * 128], V[:, kt, 0:nv], start=first, stop=last),
                reads=[r_pT[pb], r_v], writes=[cx.bank_r[ob]], pe_acc=True)
            if last:
                out_cb(qc * 4 + tq, cx.banks[ob][:, 0:nv], cx.bank_r[ob])

    n = len(steps)
    for s0 in range(min(3, n)):
        qk(s0)
    for s in range(n + PV_DELAY):
        if s + 3 < n:
            qk(s + 3)
        if s < n:
            front(s)
        if s - PV_DELAY >= 0:
            back(s - PV_DELAY)


def stage_eb(p, cx, rel_strips, cst, EBD, r_EBD):
    with contextlib.ExitStack() as st:
        def sb(name, shape, dt):
            return st.enter_context(p.sbt("P" + name, list(shape), dt))
        sbias = [sb(f"sbias{i}", [128, W_STRIP], F32) for i in range(2)]
        smult = sb("smult", [128, W_STRIP], F32)
        eb = [sb(f"eb{i}", [128, W_STRIP], BF16) for i in range(2)]
        r_sb = [p.region() for _ in range(2)]
        r_eb = [p.region() for _ in range(2)]
        r_sm = p.region()
        combos = [(h, 0) for h in range(8)] + [(8 + h, 1) for h in range(8)] + [(16 + h, 2) for h in range(8)] + \
                 [(24 + h, 2) for h in range(8)] + [(24 + h, 3) for h in range(8)]
        last_m = None
        for idx, (si, mi) in enumerate(combos):
            i = idx % 2
            if mi != last_m:
                p.dma("sp", smult[:], cst["mults"][mi], writes=[r_sm])
                last_m = mi
            p.dma("sp", sbias[i][:], rel_strips[si], writes=[r_sb[i]])
            p.op("dve", lambda e, i=i: e.tensor_tensor(out=eb[i][:], in0=sbias[i][:], in1=smult[:], op=ALU.add), reads=[r_sb[i], r_sm], writes=[r_eb[i]])
            p.dma("pool", EBD[idx], eb[i][:], reads=[r_eb[i]], writes=[r_EBD], store=True)
        p.barrier()


def stage_attn(p, cx, cst, FT, TM, OD, S, b, EBD, sinks_l, nsa_w, r_FT, r_TM, r_OD):
    col0 = b * S
    NKT = S // 128
    with contextlib.ExitStack() as st:
        def sb(name, shape, dt):
            return st.enter_context(p.sbt("C" + name, list(shape), dt))
        QT = [sb(f"QT{i}", [128, S], BF16) for i in range(2)]
        KT = [sb(f"KT{i}", [128, S], BF16) for i in range(2)]
        V = [sb(f"V{i}", [128, NKT, 65], BF16) for i in range(2)]
        eb = [sb(f"eb{i}", [128, W_STRIP], BF16) for i in range(2)]
        r_q = [p.region() for _ in range(2)]
        r_k = [p.region() for _ in range(2)]
        r_v = [p.region() for _ in range(2)]
        r_eb = [p.region() for _ in range(2)]
        pT = [sb(f"pT{i}", [128, 512], BF16) for i in range(8)]
        r_pT = [p.region() for _ in range(8)]
        osb = [sb(f"osb{i}", [128, 64], F32) for i in range(8)]
        r_osb = [p.region() for _ in range(8)]
        rden = [sb(f"rden{i}", [128, 1], F32) for i in range(8)]
        r_rd = [p.region() for _ in range(8)]
        esink = sb("esink", [128, 8], F32)
        r_es = p.region()
        p.dma("sp", esink[:], bcast_rows(sinks_l, 128), writes=[r_es])
        p.op("act", lambda e: e.activation(out=esink[:], in_=esink[:], func=AF.Exp), reads=[r_es], writes=[r_es])
        for i in range(2):
            p.op("pool", lambda e, i=i: e.memset(V[i][:, :, 64:65], 1.0), writes=[r_v[i]])
        cnt = [0]
        slot = [-1]

        def nxt():
            slot[0] += 1
            return slot[0] % 2

        def load_eb(ebidx, dst, r_dst):
            p.dma("sp", dst[:], EBD[ebidx], writes=[r_dst])

        def load_ft(dst, rows, seg, idx, width, r_dst):
            r0 = FT_OFF[seg] + idx * width
            p.dma("sp", dst[rows[0]:rows[0] + width, :], FT[r0:r0 + width, col0:col0 + S], reads=[r_FT], writes=[r_dst])

        def load_v(Vt, seg, idx, r_dst):
            if ATT_DIAG == 3:
                return
            c0 = TM_OFF[seg] + idx * 64
            p.dma("sp", Vt[:, :, 0:64], TM[col0:col0 + S, c0:c0 + 64].rearrange("(t p) c -> p t c", p=128), reads=[r_TM], writes=[r_dst])

        def simple_out(mixer, h, extra_den=None):
            def cb(qt, ps, r_bank):
                i = cnt[0] % 8
                cnt[0] += 1
                if extra_den is not None:
                    p.op("dve", lambda e, i=i: e.tensor_tensor(out=rden[i][:], in0=ps[:, 64:65], in1=extra_den, op=ALU.add),
                         reads=[r_bank, r_es], writes=[r_rd[i]])
                    p.op("dve", lambda e, i=i: e.reciprocal(out=rden[i][:], in_=rden[i][:]), reads=[r_rd[i]], writes=[r_rd[i]])
                else:
                    p.op("dve", lambda e, i=i: e.reciprocal(out=rden[i][:], in_=ps[:, 64:65]), reads=[r_bank], writes=[r_rd[i]])
                p.op("dve", lambda e, i=i: e.tensor_scalar(out=osb[i][:], in0=ps[:, 0:64], scalar1=rden[i][:, 0:1], scalar2=None, op0=ALU.mult),
                     reads=[r_bank, r_rd[i]], writes=[r_osb[i]])
                t0 = col0 + qt * 128
                c0 = mixer * 512 + h * 64
                p.dma("pool", OD[t0:t0 + 128, c0:c0 + 64], osb[i][:], reads=[r_osb[i]], writes=[r_OD], store=True)
            return cb

        causal_skip = lambda kt, qt: kt > qt
        for h in range(8):
            i = nxt()
            load_ft(QT[i], (0,), QA, h, 64, r_q[i])
            load_ft(KT[i], (0,), KA, h, 64, r_k[i])
            load_v(V[i], VA, h, r_v[i])
            load_eb(h, eb[i], r_eb[i])
            attn_core(p, cx, pT, r_pT, S, QT[i], r_q[i], KT[i], r_k[i], 64, V[i], r_v[i], 65, eb[i], r_eb[i], False,
                      lambda qc: list(range(max(0, (qc * 512 - 2048) // 128), min(NKT, qc * 4 + 4))), causal_skip, simple_out(0, h))
        for h in range(8):
            i = nxt()
            load_ft(QT[i], (0,), QB, h, 64, r_q[i])
            load_ft(KT[i], (0,), KB_, h // 4, 64, r_k[i])
            load_v(V[i], VB, h // 4, r_v[i])
            load_eb(8 + h, eb[i], r_eb[i])
            attn_core(p, cx, pT, r_pT, S, QT[i], r_q[i], KT[i], r_k[i], 64, V[i], r_v[i], 65, eb[i], r_eb[i], False,
                      lambda qc: list(range(max(0, qc * 4 - 1), min(NKT, qc * 4 + 4))), causal_skip, simple_out(1, h, extra_den=esink[:, h:h + 1]))
        stage_moba(p, cx, cst, sb, S, col0, FT, QT, KT, V, r_q, r_k, r_v, pT, r_pT, eb, r_eb, nxt, load_ft, load_v, load_eb, simple_out, causal_skip, r_FT)
        stage_nsa(p, cx, cst, sb, S, col0, FT, TM, OD, QT, KT, V, r_q, r_k, r_v, pT, r_pT, eb, r_eb, nxt, load_ft, load_v, load_eb,
                  causal_skip, nsa_w, r_FT, r_TM, r_OD, rden, r_rd, osb, r_osb, cnt)
        p.barrier()


def bcast_rows(ap1d, n):
    return bass.AP(tensor=ap1d.tensor, offset=ap1d.offset, ap=[[0, n]] + [list(x) for x in ap1d.ap])


def stage_moba(p, cx, cst, sb, S, col0, FT, QTs, KTs, Vs, r_qs, r_ks, r_vs, pT, r_pT, ebs, r_ebs, nxt, load_ft, load_v, load_eb, simple_out, causal_skip, r_FT):
    NKT = S // 128
    nblk = S // 256
    kmf = sb("kmf", [64, 16], F32)
    kmb = sb("kmb", [64, 16], BF16)
    r_km = p.region()
    mv = sb("mv", [128, 16, 16], F32)
    own = sb("own", [128, 16, 16], F32)
    r_mc = p.region()
    p.dma("sp", mv[:].rearrange("p a b -> p (a b)"), bcast_rows(cst["moba_valid"], 128), writes=[r_mc])
    p.dma("sp", own[:].rearrange("p a b -> p (a b)"), bcast_rows(cst["moba_own"], 128), writes=[r_mc])
    gs = sb("gs", [128, 16], F32)
    gall = sb("gall", [128, 512], F32)
    tall = sb("tall", [128, 512], F32)
    t2all = sb("t2all", [128, 512], F32)
    mx = sb("mx", [128, 32], F32)
    mvq = sb("mvq", [128, 512], F32)
    ownq = sb("ownq", [128, 512], F32)
    wides = [sb(f"wides{i}", [128, 128], F32) for i in range(2)]
    r_wides = [p.region() for _ in range(2)]
    r_t2 = p.region()
    for i in range(2):
        p.op("pool", lambda e, i=i: e.memset(wides[i][:], 0.0), writes=[r_wides[i]])
    p.dma("sp", mvq[:, 0:NKT * 16], bcast_rows(cst["moba_validq"], 128), writes=[r_mc])
    p.dma("sp", ownq[:, 0:NKT * 16], bcast_rows(cst["moba_ownq"], 128), writes=[r_mc])
    m8 = sb("m8", [128, 8], F32)
    sel = sb("sel", [128, 16], F32)
    wide = sb("wide", [128, 128], F32)
    r_gs, r_m8, r_sel, r_wide = p.region(), p.region(), p.region(), p.region()
    p.op("pool", lambda e: e.memset(wide[:], 0.0), writes=[r_wide])
    p.op("pool", lambda e: e.memset(kmf[:], 0.0), writes=[r_km])
    for i in range(2):
        p.dma("sp", KTs[i][64:80, :], cst["moba_koh"], writes=[r_ks[i]])
    for h in range(8):
        i = nxt()
        QT, KT, V, eb, r_q, r_k, r_v, r_eb = QTs[i], KTs[i], Vs[i], ebs[i], r_qs[i], r_ks[i], r_vs[i], r_ebs[i]
        load_ft(QT, (0,), QC, h, 64, r_q)
        load_ft(KT, (0,), KC_, h, 64, r_k)
        load_v(V, VC, h, r_v)
        load_eb(16 + h, eb, r_eb)
        p.op("dve", lambda e, KT=KT: e.tensor_reduce(out=kmf[:, 0:nblk], in_=KT[0:64, :].rearrange("p (n k) -> p n k", k=256), axis=AX.X, op=ALU.add),
             reads=[r_k], writes=[r_km])
        p.op("dve", lambda e: e.tensor_scalar(out=kmb[:], in0=kmf[:], scalar1=1.0 / 256, scalar2=None, op0=ALU.mult), reads=[r_km], writes=[r_km])
        for qt in range(NKT):
            p.op("pe", lambda e, qt=qt, QT=QT: e.matmul(cx.banks[6][:, qt * 16:(qt + 1) * 16], QT[0:64, qt * 128:(qt + 1) * 128], kmb[:], start=True, stop=True),
                 reads=[r_q, r_km], writes=[cx.bank_r[6]], pe_acc=(qt > 0))
        G3 = lambda t: t[:, 0:NKT * 16].rearrange("p (q n) -> p q n", n=16)
        bc = lambda t: t[:, 0:NKT].unsqueeze(2).to_broadcast([128, NKT, 16])
        p.op("dve", lambda e: e.tensor_tensor(out=gall[:, 0:NKT * 16], in0=cx.banks[6][:, 0:NKT * 16], in1=mvq[:, 0:NKT * 16], op=ALU.add),
             reads=[cx.bank_r[6], r_mc], writes=[r_gs])
        p.op("dve", lambda e: e.tensor_reduce(out=mx[:, 0:NKT], in_=G3(gall), axis=AX.X, op=ALU.max), reads=[r_gs], writes=[r_m8])
        p.op("dve", lambda e: e.tensor_tensor(out=G3(tall), in0=G3(gall), in1=bc(mx), op=ALU.is_equal), reads=[r_gs, r_m8], writes=[r_sel])
        p.op("dve", lambda e: e.scalar_tensor_tensor(out=tall[:, 0:NKT * 16], in0=tall[:, 0:NKT * 16], scalar=-1e30, in1=gall[:, 0:NKT * 16], op0=ALU.mult, op1=ALU.add),
             reads=[r_sel, r_gs], writes=[r_sel])
        for _round in range(2):
            p.op("dve", lambda e: e.tensor_reduce(out=mx[:, 0:NKT], in_=G3(tall), axis=AX.X, op=ALU.max), reads=[r_sel], writes=[r_m8])
            if _round == 0:
                p.op("dve", lambda e: e.tensor_tensor(out=G3(t2all), in0=G3(tall), in1=bc(mx), op=ALU.is_equal), reads=[r_sel, r_m8], writes=[r_t2])
                p.op("dve", lambda e: e.scalar_tensor_tensor(out=tall[:, 0:NKT * 16], in0=t2all[:, 0:NKT * 16], scalar=-1e30, in1=tall[:, 0:NKT * 16], op0=ALU.mult, op1=ALU.add),
                     reads=[r_t2, r_sel], writes=[r_sel])
        p.op("dve", lambda e: e.tensor_scalar(out=mx[:, 0:NKT], in0=mx[:, 0:NKT], scalar1=-1e29, scalar2=None, op0=ALU.max), reads=[r_m8], writes=[r_m8])
        p.op("dve", lambda e: e.tensor_tensor(out=G3(tall), in0=G3(gall), in1=bc(mx), op=ALU.is_ge), reads=[r_gs, r_m8], writes=[r_sel])
        p.op("dve", lambda e: e.tensor_tensor(out=tall[:, 0:NKT * 16], in0=tall[:, 0:NKT * 16], in1=ownq[:, 0:NKT * 16], op=ALU.max), reads=[r_sel, r_mc], writes=[r_sel])
        p.op("dve", lambda e: e.tensor_scalar(out=tall[:, 0:NKT * 16], in0=tall[:, 0:NKT * 16], scalar1=-1.0, scalar2=-NEG, op0=ALU.add, op1=ALU.mult),
             reads=[r_sel], writes=[r_sel])
        for qt in range(NKT):
            wi = qt % 2
            p.op("dve", lambda e, qt=qt, wi=wi: e.tensor_copy(out=wides[wi][:, 64:80], in_=tall[:, qt * 16:(qt + 1) * 16]), reads=[r_sel], writes=[r_wides[wi]])
            bkt = 7 if wi == 0 else 6
            p.op("pe", lambda e, wi=wi, bkt=bkt: e.transpose(out=cx.banks[bkt][:, 0:128], in_=wides[wi][:], identity=cx.ident_f[:]),
                 reads=[r_wides[wi], cx.r_ident], writes=[cx.bank_r[bkt]])
            p.op("act", lambda e, qt=qt, QT=QT, bkt=bkt: e.activation(out=QT[64:80, qt * 128:(qt + 1) * 128], in_=cx.banks[bkt][64:80, 0:128], func=AF.Copy),
                 reads=[cx.bank_r[bkt]], writes=[r_q])
        attn_core(p, cx, pT, r_pT, S, QT, r_q, KT, r_k, 80, V, r_v, 65, eb, r_eb, True,
                  lambda qc: list(range(0, min(NKT, qc * 4 + 4))), causal_skip, simple_out(2, h))


def stage_nsa(p, cx, cst, sb, S, col0, FT, TM, OD, QTs, KTs, Vs, r_qs, r_ks, r_vs, pT, r_pT, ebs, r_ebs, nxt, load_ft, load_v, load_eb,
              causal_skip, nsa_w, r_FT, r_TM, r_OD, rden, r_rd, osb, r_osb, cnt):
    NKT = S // 128
    ncmp = (S - 32) // 16 + 1
    w1k, w2k, w1v, w2v, pek, pev = nsa_w
    w1 = [sb(f"w1_{i}", [64, 32, 128], BF16) for i in range(2)]
    w2 = [sb(f"w2_{i}", [128, 64], BF16) for i in range(2)]
    peT = [sb(f"peT{i}", [64, 32], BF16) for i in range(2)]
    r_w = p.region()
    for i, (a, b2, c) in enumerate(((w1k, w2k, pek), (w1v, w2v, pev))):
        p.dma("pool", w1[i][:], a.rearrange("(t d) m -> d t m", d=64), writes=[r_w])
        p.dma("pool", w2[i][:], b2, writes=[r_w])
        p.dma("pool", peT[i][:], c.rearrange("t d -> d t"), writes=[r_w], allow_slow_non_contiguous=True)
    src = sb("csrc", [64, S], BF16)
    r_src = p.region()
    hbias = sb("hbias", [128, 1], F32)
    r_hb = p.region()
    xs = sb("xs", [128, 256], F32)
    u = sb("u", [128, 256], F32)
    hid = sb("hid", [128, 256], BF16)
    r_xs, r_u, r_hid = p.region(), p.region(), p.region()
    KTc = sb("KTc", [64, 256], BF16)
    Vc = sb("Vc", [128, 2, 129], BF16)
    r_ktc, r_vc = p.region(), p.region()
    cmask = sb("cmask", [128, 2, S], BF16)
    r_cm = p.region()
    p.dma("sp", cmask[:], cst["cmp_mask"].rearrange("(t p) s -> p t s", p=128), writes=[r_cm])
    gsig = sb("gsig", [128, NKT, 24], F32)
    r_g = p.region()
    p.dma("sp", gsig[:], TM[col0:col0 + S, TM_OFF[GD]:TM_OFF[GD] + 24].rearrange("(t p) c -> p t c", p=128), reads=[r_TM], writes=[r_g]) \
        if False else None
    gtmp = sb("gtmp", [128, NKT, 24], BF16)
    p.dma("sp", gtmp[:], TM[col0:col0 + S, TM_OFF[GD]:TM_OFF[GD] + 24].rearrange("(t p) c -> p t c", p=128), reads=[r_TM], writes=[r_g])
    p.op("act", lambda e: e.activation(out=gsig[:], in_=gtmp[:], func=AF.Sigmoid), reads=[r_g], writes=[r_g])
    imp = sb("imp", [128, NKT, 64], F32)
    r_imp = p.region()
    oacc = [sb(f"oacc{g}", [128, NKT, 64], F32) for g in range(4)]
    r_oacc = [p.region() for _ in range(4)]
    selmul = sb("selmul", [128, 64], F32)
    seladd = sb("seladd", [128, 64], F32)
    r_sc = p.region()
    selT = sb("selT", [128, S], BF16)
    r_selT = p.region()
    v1 = sb("v1", [128, 64], F32)
    v2 = sb("v2", [128, 64], F32)
    m8a = sb("m8a", [128, 8], F32)
    m8b = sb("m8b", [128, 8], F32)
    wide = sb("nwide", [128, 128], F32)
    r_v1, r_v2, r_m8a, r_m8b, r_wide = p.region(), p.region(), p.region(), p.region(), p.region()
    p.op("pool", lambda e: e.memset(wide[:], 0.0), writes=[r_wide])
    p.op("pool", lambda e: e.memset(hid[:], 0.0), writes=[r_hid])
    p.op("pool", lambda e: e.memset(KTc[:], 0.0), writes=[r_ktc])
    p.op("pool", lambda e: e.memset(Vc[:, :, 64:65], 1.0), writes=[r_vc])
    p.dma("sp", Vc[:, :, 65:129], cst["overlap"].rearrange("(t p) c -> p t c", p=128), writes=[r_vc])
    tmp = [sb(f"ntmp{i}", [128, 64], F32) for i in range(2)]
    r_tmp = [p.region() for _ in range(2)]

    def compress(i, seg, kvh):
        r0 = FT_OFF[seg] + kvh * 64
        p.dma("sp", src[:], FT[r0:r0 + 64, col0:col0 + S], reads=[r_FT], writes=[r_src])
        sv = src[:].rearrange("p (n s) -> p n s", s=16)
        for t in range(32):
            p.op("pe", lambda e, t=t: e.matmul(cx.banks[6][:, 0:ncmp], w1[i][:, t, :], sv[:, t // 16:t // 16 + ncmp, t % 16], start=(t == 0), stop=(t == 31)),
                 reads=[r_w, r_src], writes=[cx.bank_r[6]], pe_acc=(t > 0))
        for t in range(32):
            p.op("pe", lambda e, t=t: e.matmul(cx.banks[7][:, 0:1], w1[i][:, t, :], peT[i][:, t:t + 1], start=(t == 0), stop=(t == 31)),
                 reads=[r_w], writes=[cx.bank_r[7]], pe_acc=(t > 0))
        p.op("dve", lambda e: e.tensor_copy(out=hbias[:], in_=cx.banks[7][:, 0:1]), reads=[cx.bank_r[7]], writes=[r_hb])
        p.op("act", lambda e: e.activation(out=xs[:, 0:ncmp], in_=cx.banks[6][:, 0:ncmp], func=AF.Identity, bias=hbias[:, 0:1]),
             reads=[cx.bank_r[6], r_hb], writes=[r_xs])
        p.op("dve", lambda e: e.tensor_tensor(out=u[:, 0:ncmp], in0=xs[:, 0:ncmp], in1=xs[:, 0:ncmp], op=ALU.mult), reads=[r_xs], writes=[r_u])
        p.op("dve", lambda e: e.tensor_scalar(out=u[:, 0:ncmp], in0=u[:, 0:ncmp], scalar1=0.044715, scalar2=1.0, op0=ALU.mult, op1=ALU.add), reads=[r_u], writes=[r_u])
        p.op("dve", lambda e: e.tensor_tensor(out=u[:, 0:ncmp], in0=u[:, 0:ncmp], in1=xs[:, 0:ncmp], op=ALU.mult), reads=[r_u, r_xs], writes=[r_u])
        p.op("act", lambda e: e.activation(out=u[:, 0:ncmp], in_=u[:, 0:ncmp], func=AF.Sigmoid, scale=2.0 * 0.7978845608028654), reads=[r_u], writes=[r_u])
        p.op("dve", lambda e: e.tensor_tensor(out=hid[:, 0:ncmp], in0=u[:, 0:ncmp], in1=xs[:, 0:ncmp], op=ALU.mult), reads=[r_u, r_xs], writes=[r_hid])

    for i in range(2):
        p.dma("sp", KTs[i][64:128, :], cst["nsa_koh"], writes=[r_ks[i]])
    for kvh in range(2):
        compress(0, KDC, kvh)
        p.op("pe", lambda e: e.matmul(cx.banks[6][0:64, 0:256], w2[0][:], hid[:], start=True, stop=True), reads=[r_w, r_hid], writes=[cx.bank_r[6]])
        p.op("act", lambda e: e.activation(out=KTc[:, 0:ncmp], in_=cx.banks[6][0:64, 0:ncmp], func=AF.Copy), reads=[cx.bank_r[6]], writes=[r_ktc])
        compress(1, VDC, kvh)
        for nt in range((ncmp + 127) // 128):
            p.op("pe", lambda e, nt=nt: e.matmul(cx.banks[7][:, 0:64], hid[:, nt * 128:(nt + 1) * 128], w2[1][:], start=True, stop=True),
                 reads=[r_w, r_hid], writes=[cx.bank_r[7]])
            p.op("act", lambda e, nt=nt: e.activation(out=Vc[:, nt, 0:64], in_=cx.banks[7][:, 0:64], func=AF.Copy), reads=[cx.bank_r[7]], writes=[r_vc])
        p.op("pool", lambda e: e.memset(imp[:], 0.0), writes=[r_imp])
        nct = (ncmp + 127) // 128
        for g in range(4):
            h = kvh * 4 + g
            i = nxt()
            QT, r_q = QTs[i], r_qs[i]
            load_ft(QT, (0,), QD, h, 64, r_q)

            def cmp_cb(qt, ps, r_bank, g=g, h=h):
                i = cnt[0] % 2
                cnt[0] += 1
                p.op("dve", lambda e, i=i: e.tensor_scalar(out=rden[i][:], in0=ps[:, 64:65], scalar1=1e-30, scalar2=None, op0=ALU.max), reads=[r_bank], writes=[r_rd[i]])
                p.op("dve", lambda e, i=i: e.reciprocal(out=rden[i][:], in_=rden[i][:]), reads=[r_rd[i]], writes=[r_rd[i]])
                p.op("dve", lambda e, i=i, qt=qt: e.scalar_tensor_tensor(out=imp[:, qt, :], in0=ps[:, 65:129], scalar=rden[i][:, 0:1], in1=imp[:, qt, :],
                                                                       op0=ALU.mult, op1=ALU.add), reads=[r_bank, r_rd[i], r_imp], writes=[r_imp])
                p.op("dve", lambda e, i=i: e.tensor_scalar(out=tmp[i][:], in0=ps[:, 0:64], scalar1=rden[i][:, 0:1], scalar2=None, op0=ALU.mult),
                     reads=[r_bank, r_rd[i]], writes=[r_tmp[i]])
                p.op("dve", lambda e, i=i, qt=qt: e.tensor_scalar(out=oacc[g][:, qt, :], in0=tmp[i][:], scalar1=(gsig[:, qt, h * 3:h * 3 + 1] if NSA_ONLY in (None, 0) else 0.0), scalar2=None, op0=ALU.mult),
                     reads=[r_tmp[i], r_g], writes=[r_oacc[g]])
            attn_core(p, cx, pT, r_pT, S, QT, r_q, KTc, r_ktc, 64, Vc, r_vc, 129, None, None, False,
                      lambda qc: list(range(nct)), lambda kt, qt: False, cmp_cb, cmask=cmask, r_cm=r_cm)
        for qt in range(NKT):
            p.dma("sp", selmul[:], cst["sel_mul"][qt * 128:(qt + 1) * 128, :], writes=[r_sc])
            p.dma("sp", seladd[:], cst["sel_add"][qt * 128:(qt + 1) * 128, :], writes=[r_sc])
            p.op("dve", lambda e, qt=qt: e.tensor_tensor(out=v1[:], in0=imp[:, qt, :], in1=selmul[:], op=ALU.mult), reads=[r_imp, r_sc], writes=[r_v1])
            p.op("dve", lambda e, qt=qt: e.tensor_tensor(out=v1[:], in0=v1[:], in1=seladd[:], op=ALU.add), reads=[r_v1, r_sc], writes=[r_v1])
            p.op("dve", lambda e: e.max(out=m8a[:], in_=v1[:]), reads=[r_v1], writes=[r_m8a])
            p.op("dve", lambda e: e.match_replace(out=v2[:], in_to_replace=m8a[:], in_values=v1[:], imm_value=-1e30), reads=[r_v1, r_m8a], writes=[r_v2])
            p.op("dve", lambda e: e.max(out=m8b[:], in_=v2[:]), reads=[r_v2], writes=[r_m8b])
            p.op("dve", lambda e: e.tensor_scalar(out=m8b[:, 7:8], in0=m8b[:, 7:8], scalar1=-1e29, scalar2=None, op0=ALU.max), reads=[r_m8b], writes=[r_m8b])
            p.op("dve", lambda e: e.tensor_scalar(out=v2[:], in0=v1[:], scalar1=m8b[:, 7:8], scalar2=None, op0=ALU.is_ge), reads=[r_v1, r_m8b], writes=[r_v2])
            p.op("dve", lambda e: e.tensor_scalar(out=wide[:, 64:128], in0=v2[:], scalar1=-1.0, scalar2=-NEG, op0=ALU.add, op1=ALU.mult), reads=[r_v2], writes=[r_wide])
            p.op("pe", lambda e: e.transpose(out=cx.banks[7][:, 0:128], in_=wide[:], identity=cx.ident_f[:]), reads=[r_wide, cx.r_ident], writes=[cx.bank_r[7]])
            p.op("act", lambda e, qt=qt: e.activation(out=selT[64:128, qt * 128:(qt + 1) * 128], in_=cx.banks[7][64:128, 0:128], func=AF.Copy),
                 reads=[cx.bank_r[7]], writes=[r_selT])
        for g in range(4):
            h = kvh * 4 + g
            for br in (1, 2):
                i = nxt()
                QT, KT, V, eb, r_q, r_k, r_v, r_eb = QTs[i], KTs[i], Vs[i], ebs[i], r_qs[i], r_ks[i], r_vs[i], r_ebs[i]
                load_ft(QT, (0,), QD, h, 64, r_q)
                if br == 1:
                    p.op("act", lambda e, QT=QT: e.activation(out=QT[64:128, :], in_=selT[64:128, :], func=AF.Copy), reads=[r_selT], writes=[r_q])
                    load_ft(KT, (0,), KDS, kvh, 64, r_k)
                    load_v(V, VDS, kvh, r_v)
                    load_eb(24 + h, eb, r_eb)
                    kfn = lambda qc: list(range(0, min(NKT, qc * 4 + 4)))
                    Kc, clamp = 128, True
                else:
                    load_ft(KT, (0,), KDW, kvh, 64, r_k)
                    load_v(V, VDW, kvh, r_v)
                    load_eb(32 + h, eb, r_eb)
                    kfn = lambda qc: list(range(max(0, qc * 4 - 4), min(NKT, qc * 4 + 4)))
                    Kc, clamp = 64, False

                def br_cb(qt, ps, r_bank, g=g, h=h, br=br):
                    if NSA_ONLY is not None and NSA_ONLY != br:
                        return
                    i = cnt[0] % 2
                    cnt[0] += 1
                    p.op("dve", lambda e, i=i: e.reciprocal(out=rden[i][:], in_=ps[:, 64:65]), reads=[r_bank], writes=[r_rd[i]])
                    p.op("dve", lambda e, i=i: e.tensor_scalar(out=tmp[i][:], in0=ps[:, 0:64], scalar1=rden[i][:, 0:1], scalar2=None, op0=ALU.mult),
                         reads=[r_bank, r_rd[i]], writes=[r_tmp[i]])
                    p.op("dve", lambda e, i=i, qt=qt: e.scalar_tensor_tensor(out=oacc[g][:, qt, :], in0=tmp[i][:], scalar=gsig[:, qt, h * 3 + br:h * 3 + br + 1],
                                                                           in1=oacc[g][:, qt, :], op0=ALU.mult, op1=ALU.add),
                         reads=[r_tmp[i], r_g, r_oacc[g]], writes=[r_oacc[g]])
                attn_core(p, cx, pT, r_pT, S, QT, r_q, KT, r_k, Kc, V, r_v, 65, eb, r_eb, clamp, kfn, causal_skip, br_cb)
            c0 = 3 * 512 + h * 64
            p.dma("pool", OD[col0:col0 + S, c0:c0 + 64].rearrange("(t p) c -> p t c", p=128), oacc[g][:], reads=[r_oacc[g]], writes=[r_OD], store=True)


def make_consts(S, rel_bias):
    cst = {}
    j = np.arange(128)[:, None]
    m = np.arange(W_STRIP)[None, :]
    d = m - OFF_STRIP - j
    dd = np.maximum(d, 0)
    bucket = t5_bucket_np(dd)
    strips = np.ascontiguousarray(np.transpose(rel_bias[bucket], (2, 0, 1))).astype(np.float32)
    causal = (d >= 0)
    multA = ((d >= 0) & (d <= 128)).astype(np.float32) + ((d >= 0) & (d % 4 == 0) & (d <= 512)) + ((d >= 0) & (d % 16 == 0) & (d <= 2048))
    mults = np.stack([multA, (causal & (d <= 127)), causal, (causal & (d <= 511))]).astype(np.float32)
    with np.errstate(divide="ignore"):
        cst["mults"] = np.where(mults > 0, np.log(np.maximum(mults, 1e-30)), NEG).astype(np.float32)
    nblk = 16
    cur = np.arange(16)[:, None]
    n = np.arange(16)[None, :]
    cst["moba_valid"] = np.where(n < cur, 0.0, -1e30).astype(np.float32).reshape(-1)
    cst["moba_own"] = (n == cur).astype(np.float32).reshape(-1)
    curq = (np.arange(S // 128) // 2)[:, None]
    cst["moba_validq"] = np.where(n < curq, 0.0, -1e30).astype(np.float32).reshape(-1)
    cst["moba_ownq"] = (n == curq).astype(np.float32).reshape(-1)
    import ml_dtypes
    bf = ml_dtypes.bfloat16
    key = np.arange(S)[None, :]
    cst["moba_koh"] = (key // 256 == np.arange(16)[:, None]).astype(bf)
    cst["nsa_koh"] = (key // 64 == np.arange(64)[:, None]).astype(bf)
    ncmp = (S - 32) // 16 + 1
    nn = np.arange(256)[:, None]
    cst["cmp_mask"] = np.where((nn * 16 + 31 <= key) & (nn < ncmp), 0.0, NEG).astype(bf)
    c_start = nn * 16
    j_start = np.arange(64)[None, :] * 64
    cst["overlap"] = ((c_start < j_start + 64) & (c_start + 32 > j_start) & (nn < ncmp)).astype(bf)
    pos = np.arange(S)[:, None]
    curq = pos // 64
    jj = np.arange(64)[None, :]
    forced = (jj == 0) | (jj == curq) | (jj == curq - 1)
    valid = jj <= curq
    cst["sel_mul"] = (valid & ~forced).astype(np.float32)
    cst["sel_add"] = np.where(forced, 1e30, np.where(valid, 0.0, -1e30)).astype(np.float32)
    return cst, strips


def stage_mix_out(p, cx, OD, x_src, x_dst, mixg_row, w_out_l, modD, l, NB, S, r_OD, r_x):
    with contextlib.ExitStack() as st:
        def sb(name, shape, dt):
            return st.enter_context(p.sbt("E" + name, list(shape), dt))
        wo = sb("wo", [128, KC, D], BF16)
        r_wo = p.region()
        for k4 in range(4):
            p.dma("pool", wo[:, k4 * 4:(k4 + 1) * 4, :], w_out_l[k4 * 512:(k4 + 1) * 512, :].rearrange("(k p) c -> p k c", p=128), writes=[r_wo])
        mg = sb("mg", [128, KC], F32)
        r_mg = p.region()
        load_cols(p, mg, r_mg, mixg_row)
        g1b = sb("g1b", [128, D], F32)
        r_g1 = p.region()
        ot = [sb(f"ot{i}", [128, D], F32) for i in range(2)]
        r_ot = [p.region() for _ in range(2)]
        junk = sb("junk", [128, 512], BF16)
        r_junk = p.region()
        ss = [sb(f"ss{i}", [128, 4], F32) for i in range(2)]
        r_ss = [p.region() for _ in range(2)]
        on = sb("on", [128, D], BF16)
        r_on = p.region()
        cT = [sb(f"cT{i}", [128, KC, 128], BF16) for i in range(2)]
        r_cT = [p.region() for _ in range(2)]
        xt = [sb(f"xt{i}", [128, D], F32) for i in range(2)]
        r_xt = [p.region() for _ in range(2)]
        x1 = [sb(f"x1{i}", [128, D], F32) for i in range(2)]
        r_x1 = [p.region() for _ in range(2)]
        it = 0
        for b in range(NB):
            p.dma("sp", g1b[:], bcast_rows(modD[l, b, 2 * D:3 * D], 128), reads=[cx.r_modD], writes=[r_g1])
            for tt in range(S // 128):
                i = it % 2
                it += 1
                t0 = b * S + tt * 128
                p.dma("sp", ot[i][:], OD[t0:t0 + 128, :], reads=[r_OD], writes=[r_ot[i]])
                p.dma("sp", xt[i][:], x_src[t0:t0 + 128, :], reads=[r_x], writes=[r_xt[i]])
                for m in range(4):
                    p.op("act", lambda e, i=i, m=m: e.activation(out=junk[:], in_=ot[i][:, m * 512:(m + 1) * 512], func=AF.Square, accum_out=ss[i][:, m:m + 1]),
                         reads=[r_ot[i]], writes=[r_junk, r_ss[i]])
                p.op("dve", lambda e, i=i: e.tensor_scalar(out=ss[i][:], in0=ss[i][:], scalar1=1.0 / 512, scalar2=1e-6, op0=ALU.mult, op1=ALU.add),
                     reads=[r_ss[i]], writes=[r_ss[i]])
                p.op("act", lambda e, i=i: e.activation(out=ss[i][:], in_=ss[i][:], func=AF.Sqrt), reads=[r_ss[i]], writes=[r_ss[i]])
                p.op("dve", lambda e, i=i: e.reciprocal(out=ss[i][:], in_=ss[i][:]), reads=[r_ss[i]], writes=[r_ss[i]])
                for m in range(4):
                    p.op("dve", lambda e, i=i, m=m: e.tensor_scalar(out=on[:, m * 512:(m + 1) * 512], in0=ot[i][:, m * 512:(m + 1) * 512],
                                                                  scalar1=ss[i][:, m:m + 1], scalar2=None, op0=ALU.mult),
                         reads=[r_ot[i], r_ss[i]], writes=[r_on])
                for j4 in range(4):
                    bk = 4 + j4
                    for jj in range(4):
                        j = j4 * 4 + jj
                        p.op("pe", lambda e, j=j, jj=jj, bk=bk: e.transpose(out=cx.banks[bk][:].bitcast(BF16)[:, jj * 128:(jj + 1) * 128], in_=on[:, j * 128:(j + 1) * 128],
                                                                          identity=cx.ident_b[:]),
                             reads=[r_on, cx.r_ident], writes=[cx.bank_r[bk]], pe_acc=(jj > 0))
                    for jj in range(4):
                        j = j4 * 4 + jj
                        p.op("act", lambda e, j=j, jj=jj, bk=bk, i=i: e.activation(out=cT[i][:, j, :], in_=cx.banks[bk][:].bitcast(BF16)[:, jj * 128:(jj + 1) * 128],
                                                                             func=AF.Copy, scale=mg[:, j:j + 1]),
                             reads=[cx.bank_r[bk], r_mg], writes=[r_cT[i]])
                for n in range(4):
                    for k in range(KC):
                        p.op("pe", lambda e, n=n, k=k, i=i: e.matmul(cx.banks[n][:], cT[i][:, k, :], wo[:, k, n * 512:(n + 1) * 512], start=(k == 0), stop=(k == KC - 1)),
                             reads=[r_cT[i], r_wo], writes=[cx.bank_r[n]], pe_acc=(k > 0))
                    p.op("dve", lambda e, n=n, i=i: e.tensor_tensor(out=x1[i][:, n * 512:(n + 1) * 512], in0=cx.banks[n][:], in1=g1b[:, n * 512:(n + 1) * 512], op=ALU.mult),
                         reads=[cx.bank_r[n], r_g1], writes=[r_x1[i]])
                    p.op("dve", lambda e, n=n, i=i: e.tensor_tensor(out=x1[i][:, n * 512:(n + 1) * 512], in0=x1[i][:, n * 512:(n + 1) * 512],
                                                                  in1=xt[i][:, n * 512:(n + 1) * 512], op=ALU.add),
                         reads=[r_x1[i], r_xt[i]], writes=[r_x1[i]])
                p.dma("pool", x_dst[t0:t0 + 128, :], x1[i][:], reads=[r_x1[i]], writes=[r_x], store=True)
        p.barrier()


def stage_moe(p, cx, h2T, NT, router_w, router_bias, wg_l, wu_l, wd_l, yacc, r_h2T, r_y):
    NTL = NT // 128
    with contextlib.ExitStack() as st:
        def sb(name, shape, dt):
            return st.enter_context(p.sbt("M" + name, list(shape), dt))
        rwb = sb("rwb", [128, KC, 16], BF16)
        r_rw = p.region()
        p.dma("pool", rwb[:], router_w.rearrange("(k p) e -> p k e", p=128), writes=[r_rw])
        rb = sb("rb", [128, 16], F32)
        p.dma("sp", rb[:], bcast_rows(router_bias, 128), writes=[r_rw])
        comb = sb("comb", [128, NTL, 16], F32)
        r_comb = p.region()
        hb = [sb(f"hb{i}", [128, KC, 512], BF16) for i in range(2)]
        r_hb = [p.region() for _ in range(2)]
        aff = sb("aff", [128, 16], F32)
        sc = sb("sc", [128, 16], F32)
        t16 = sb("t16", [128, 16], F32)
        m1 = sb("m1", [128, 4], F32)
        m2 = sb("m2", [128, 4], F32)
        gm = sb("gm", [128, 1], F32)
        e1 = sb("e1", [128, 1], F32)
        r_aff, r_sc, r_t16, r_m1, r_m2, r_gm, r_e1 = (p.region() for _ in range(7))
        ib = 0

        def v3(t):
            return t[:].rearrange("p (g k) -> p g k", k=4)
        for blk in range(NT // 512):
            i = ib % 2
            ib += 1
            p.dma("sp", hb[i][:], h2T[:, blk * 512:(blk + 1) * 512].rearrange("(k p) t -> p k t", p=128), reads=[r_h2T], writes=[r_hb[i]])
            for tt in range(4):
                tl = blk * 4 + tt
                for k in range(KC):
                    p.op("pe", lambda e, k=k, i=i, tt=tt: e.matmul(cx.banks[6][:, 0:16], hb[i][:, k, tt * 128:(tt + 1) * 128], rwb[:, k, :], start=(k == 0), stop=(k == KC - 1)),
                         reads=[r_hb[i], r_rw], writes=[cx.bank_r[6]], pe_acc=(k > 0))
                p.op("act", lambda e: e.activation(out=aff[:], in_=cx.banks[6][:, 0:16], func=AF.Sigmoid), reads=[cx.bank_r[6]], writes=[r_aff])
                p.op("dve", lambda e: e.tensor_tensor(out=sc[:], in0=aff[:], in1=rb[:], op=ALU.add), reads=[r_aff, r_rw], writes=[r_sc])
                p.op("dve", lambda e: e.tensor_reduce(out=m1[:], in_=v3(sc), axis=AX.X, op=ALU.max), reads=[r_sc], writes=[r_m1])
                p.op("dve", lambda e: e.tensor_tensor(out=v3(t16), in0=v3(sc), in1=m1[:].unsqueeze(2).to_broadcast([128, 4, 4]), op=ALU.is_equal),
                     reads=[r_sc, r_m1], writes=[r_t16])
                p.op("dve", lambda e: e.scalar_tensor_tensor(out=t16[:], in0=t16[:], scalar=-1e30, in1=sc[:], op0=ALU.mult, op1=ALU.add),
                     reads=[r_t16, r_sc], writes=[r_t16])
                p.op("dve", lambda e: e.tensor_reduce(out=m2[:], in_=v3(t16), axis=AX.X, op=ALU.max), reads=[r_t16], writes=[r_m2])
                p.op("dve", lambda e: e.tensor_tensor(out=m1[:], in0=m1[:], in1=m2[:], op=ALU.add), reads=[r_m1, r_m2], writes=[r_m1])
                p.op("dve", lambda e: e.tensor_reduce(out=gm[:], in_=m1[:], axis=AX.X, op=ALU.max), reads=[r_m1], writes=[r_gm])
                p.op("dve", lambda e: e.tensor_scalar(out=m2[:], in0=m1[:], scalar1=gm[:, 0:1], scalar2=None, op0=ALU.is_ge), reads=[r_m1, r_gm], writes=[r_m2])
                p.op("dve", lambda e: e.tensor_scalar(out=m2[:], in0=m2[:], scalar1=-1.0, scalar2=1e30, op0=ALU.add, op1=ALU.mult), reads=[r_m2], writes=[r_m2])
                p.op("dve", lambda e: e.tensor_tensor(out=v3(t16), in0=v3(sc), in1=m2[:].unsqueeze(2).to_broadcast([128, 4, 4]), op=ALU.add),
                     reads=[r_sc, r_m2], writes=[r_t16])
                p.op("dve", lambda e: e.tensor_reduce(out=e1[:], in_=t16[:], axis=AX.X, op=ALU.max), reads=[r_t16], writes=[r_e1])
                p.op("dve", lambda e: e.tensor_scalar(out=sc[:], in0=t16[:], scalar1=e1[:, 0:1], scalar2=None, op0=ALU.is_equal), reads=[r_t16, r_e1], writes=[r_sc])
                p.op("dve", lambda e: e.scalar_tensor_tensor(out=t16[:], in0=sc[:], scalar=-1e30, in1=t16[:], op0=ALU.mult, op1=ALU.add),
                     reads=[r_sc, r_t16], writes=[r_t16])
                p.op("dve", lambda e: e.tensor_reduce(out=e1[:], in_=t16[:], axis=AX.X, op=ALU.max), reads=[r_t16], writes=[r_e1])
                p.op("dve", lambda e: e.tensor_scalar(out=t16[:], in0=t16[:], scalar1=e1[:, 0:1], scalar2=None, op0=ALU.is_equal), reads=[r_t16, r_e1], writes=[r_t16])
                p.op("dve", lambda e: e.tensor_tensor(out=sc[:], in0=sc[:], in1=t16[:], op=ALU.add), reads=[r_sc, r_t16], writes=[r_sc])
                p.op("dve", lambda e: e.tensor_tensor(out=sc[:], in0=sc[:], in1=aff[:], op=ALU.mult), reads=[r_sc, r_aff], writes=[r_sc])
                p.op("dve", lambda e: e.tensor_reduce(out=e1[:], in_=sc[:], axis=AX.X, op=ALU.add), reads=[r_sc], writes=[r_e1])
                p.op("dve", lambda e: e.reciprocal(out=e1[:], in_=e1[:]), reads=[r_e1], writes=[r_e1])
                p.op("dve", lambda e, tl=tl: e.tensor_scalar(out=comb[:, tl, :], in0=sc[:], scalar1=e1[:, 0:1], scalar2=None, op0=ALU.mult),
                     reads=[r_sc, r_e1], writes=[r_comb])
        wgu = [sb(f"wgu{i}", [128, KC, 1024], BF16) for i in range(2)]
        wd = [sb(f"wd{i}", [128, 4, D], BF16) for i in range(2)]
        r_wgu = [p.region() for _ in range(2)]
        r_wd = [p.region() for _ in range(2)]
        sg = [sb(f"sg{i}", [128, 512], F32) for i in range(2)]
        he = [sb(f"he{i}", [128, 512], BF16) for i in range(2)]
        heT = [sb(f"heT{i}", [128, 4, 128], BF16) for i in range(2)]
        yt = [sb(f"yt{i}", [128, D], F32) for i in range(2)]
        r_sg = [p.region() for _ in range(2)]
        r_he = [p.region() for _ in range(2)]
        r_heT = [p.region() for _ in range(2)]
        r_yt = [p.region() for _ in range(2)]
        def load_w(ex):
            w = ex % 2
            p.dma("pool", wgu[w][:, :, 0:512], wg_l[ex].rearrange("(k p) f -> p k f", p=128), writes=[r_wgu[w]])
            p.dma("pool", wgu[w][:, :, 512:1024], wu_l[ex].rearrange("(k p) f -> p k f", p=128), writes=[r_wgu[w]])
            p.dma("pool", wd[w][:], wd_l[ex].rearrange("(k p) c -> p k c", p=128), writes=[r_wd[w]])

        steps = [(ex, blk, tt) for ex in range(16) for blk in range(NT // 512) for tt in range(4)]
        hb_of = {}

        def gate_up(n):
            ex, blk, tt = steps[n]
            w = ex % 2
            if tt == 0:
                i = (ib0[0]) % 2
                ib0[0] += 1
                hb_of[(ex, blk)] = i
                p.dma("sp", hb[i][:], h2T[:, blk * 512:(blk + 1) * 512].rearrange("(k p) t -> p k t", p=128), reads=[r_h2T], writes=[r_hb[i]])
            i = hb_of[(ex, blk)]
            pr = 2 * (n % 2)
            for half in range(2):
                for k in range(KC):
                    p.op("pe", lambda e, k=k, i=i, tt=tt, w=w, half=half, pr=pr: e.matmul(
                        cx.banks[pr + half][:], hb[i][:, k, tt * 128:(tt + 1) * 128], wgu[w][:, k, half * 512:(half + 1) * 512], start=(k == 0), stop=(k == KC - 1)),
                        reads=[r_hb[i], r_wgu[w]], writes=[cx.bank_r[pr + half]], pe_acc=(k > 0))
        ib0 = [ib]
        load_w(0)
        gate_up(0)
        for n, (ex, blk, tt) in enumerate(steps):
            w = ex % 2
            if blk == 0 and tt == 0 and ex + 1 < 16:
                load_w(ex + 1)
            if n + 1 < len(steps):
                gate_up(n + 1)
            tl = blk * 4 + tt
            o = n % 2
            pr = 2 * (n % 2)
            p.op("act", lambda e, o=o, pr=pr: e.activation(out=sg[o][:], in_=cx.banks[pr][:], func=AF.Silu), reads=[cx.bank_r[pr]], writes=[r_sg[o]])
            p.op("dve", lambda e, o=o, tl=tl, ex=ex, pr=pr: e.scalar_tensor_tensor(out=he[o][:], in0=cx.banks[pr + 1][:], scalar=comb[:, tl, ex:ex + 1], in1=sg[o][:],
                                                                             op0=ALU.mult, op1=ALU.mult),
                 reads=[cx.bank_r[pr + 1], r_comb, r_sg[o]], writes=[r_he[o]])
            bt = pr
            for kk in range(4):
                p.op("pe", lambda e, kk=kk, o=o, bt=bt: e.transpose(out=cx.banks[bt][:].bitcast(BF16)[:, kk * 128:(kk + 1) * 128], in_=he[o][:, kk * 128:(kk + 1) * 128],
                                                                  identity=cx.ident_b[:]),
                     reads=[r_he[o], cx.r_ident], writes=[cx.bank_r[bt]], pe_acc=(kk > 0))
            p.op("act", lambda e, o=o, bt=bt: e.activation(out=heT[o][:].rearrange("p k t -> p (k t)"), in_=cx.banks[bt][:].bitcast(BF16)[:, 0:512], func=AF.Copy),
                 reads=[cx.bank_r[bt]], writes=[r_heT[o]])
            for nn in range(4):
                for kk in range(4):
                    p.op("pe", lambda e, nn=nn, kk=kk, o=o, w=w: e.matmul(cx.banks[4 + nn][:], heT[o][:, kk, :], wd[w][:, kk, nn * 512:(nn + 1) * 512],
                                                                        start=(kk == 0), stop=(kk == 3)),
                         reads=[r_heT[o], r_wd[w]], writes=[cx.bank_r[4 + nn]], pe_acc=(kk > 0))
                if nn % 2 == 0:
                    p.op("act", lambda e, nn=nn, o=o: e.activation(out=yt[o][:, nn * 512:(nn + 1) * 512], in_=cx.banks[4 + nn][:], func=AF.Copy),
                         reads=[cx.bank_r[4 + nn]], writes=[r_yt[o]])
                else:
                    p.op("dve", lambda e, nn=nn, o=o: e.tensor_copy(out=yt[o][:, nn * 512:(nn + 1) * 512], in_=cx.banks[4 + nn][:]),
                         reads=[cx.bank_r[4 + nn]], writes=[r_yt[o]])
            if ex == 0 or MOE_DIAG == 2:
                p.dma("pool", yacc[tl * 128:(tl + 1) * 128, :], yt[o][:], reads=[r_yt[o]], writes=[r_y], store=True)
            elif MOE_DIAG == 1:
                pass
            else:
                p.dma("pool", yacc[tl * 128:(tl + 1) * 128, :], yt[o][:], reads=[r_yt[o]], writes=[r_y], accum_op=ALU.add, store=True)
        p.barrier()


def stage_resid(p, cx, xs, yacc, modD, l, goff, NB, S, r_x, r_y, final_g=None, out=None, r_out=None):
    with contextlib.ExitStack() as st:
        def sb(name, shape, dt):
            return st.enter_context(p.sbt("R" + name, list(shape), dt))
        gb = sb("gb", [128, D], F32)
        r_gb = p.region()
        fg = sb("fg", [128, D], F32)
        r_fg = p.region()
        if final_g is not None:
            p.dma("sp", fg[:], bcast_rows(final_g, 128), writes=[r_fg])
        xt = [sb(f"xt{i}", [128, D], F32) for i in range(2)]
        yt = [sb(f"yt{i}", [128, D], F32) for i in range(2)]
        r_xt = [p.region() for _ in range(2)]
        r_yt = [p.region() for _ in range(2)]
        junk = sb("junk", [128, D], BF16)
        r_junk = p.region()
        ss = [sb(f"ss{i}", [128, 1], F32) for i in range(2)]
        r_ss = [p.region() for _ in range(2)]
        it = 0
        for b in range(NB):
            p.dma("sp", gb[:], bcast_rows(modD[l, b, goff:goff + D], 128), reads=[cx.r_modD], writes=[r_gb])
            for tt in range(S // 128):
                i = it % 2
                it += 1
                t0 = b * S + tt * 128
                p.dma("sp", xt[i][:], xs[t0:t0 + 128, :], reads=[r_x], writes=[r_xt[i]])
                p.dma("sp", yt[i][:], yacc[t0:t0 + 128, :], reads=[r_y], writes=[r_yt[i]])
                p.op("dve", lambda e, i=i: e.tensor_tensor(out=yt[i][:], in0=yt[i][:], in1=gb[:], op=ALU.mult), reads=[r_yt[i], r_gb], writes=[r_yt[i]])
                p.op("dve", lambda e, i=i: e.tensor_tensor(out=xt[i][:], in0=xt[i][:], in1=yt[i][:], op=ALU.add), reads=[r_yt[i], r_xt[i]], writes=[r_xt[i]])
                if final_g is None:
                    p.dma("pool", xs[t0:t0 + 128, :], xt[i][:], reads=[r_xt[i]], writes=[r_x], store=True)
                else:
                    p.op("act", lambda e, i=i: e.activation(out=junk[:], in_=xt[i][:], func=AF.Square, accum_out=ss[i][:]),
                         reads=[r_xt[i]], writes=[r_junk, r_ss[i]])
                    p.op("dve", lambda e, i=i: e.tensor_scalar(out=ss[i][:], in0=ss[i][:], scalar1=1.0 / D, scalar2=1e-6, op0=ALU.mult, op1=ALU.add),
                         reads=[r_ss[i]], writes=[r_ss[i]])
                    p.op("act", lambda e, i=i: e.activation(out=ss[i][:], in_=ss[i][:], func=AF.Sqrt), reads=[r_ss[i]], writes=[r_ss[i]])
                    p.op("dve", lambda e, i=i: e.reciprocal(out=ss[i][:], in_=ss[i][:]), reads=[r_ss[i]], writes=[r_ss[i]])
                    p.op("dve", lambda e, i=i: e.scalar_tensor_tensor(out=yt[i][:], in0=xt[i][:], scalar=ss[i][:, 0:1], in1=fg[:], op0=ALU.mult, op1=ALU.mult),
                         reads=[r_xt[i], r_ss[i], r_fg], writes=[r_yt[i]])
                    p.dma("pool", out[t0:t0 + 128, :], yt[i][:], reads=[r_yt[i]], writes=[r_out], store=True)
        p.barrier()


CONST_SPECS = None


def build_program(NB, S, NL):
    NT = NB * S
    nc = bass.Bass("TRN2", target_bir_lowering=False)
    p = Prog(nc)

    def ext(name, shape, dt=F32):
        return nc.dram_tensor(name, list(shape), dt, kind="ExternalInput").ap()

    def internal(name, shape, dt=F32):
        return nc.dram_tensor(name, list(shape), dt, kind="Internal").ap()
    consts = ext("consts", [128, 128])
    x = ext("x", [NT, D])
    c = ext("c", [NB, D])
    strips = ext("strips", [32, 128, W_STRIP])
    cst = {"mults": ext("mults", [4, 128, W_STRIP]), "moba_valid": ext("moba_valid", [256]), "moba_own": ext("moba_own", [256]), "moba_validq": ext("moba_validq", [S // 128 * 16]), "moba_ownq": ext("moba_ownq", [S // 128 * 16]),
           "moba_koh": ext("moba_koh", [16, S], BF16), "nsa_koh": ext("nsa_koh", [64, S], BF16), "cmp_mask": ext("cmp_mask", [256, S], BF16),
           "overlap": ext("overlap", [256, 64], BF16), "sel_mul": ext("sel_mul", [S, 64]), "sel_add": ext("sel_add", [S, 64])}
    router_w = ext("router_w", [D, 16])
    router_bias = ext("router_bias", [16])
    norm1_g = ext("norm1_g", [NL, D])
    norm2_g = ext("norm2_g", [NL, D])
    ada_w = ext("ada_w", [NL, D, 6 * D])
    ada_b = ext("ada_b", [NL, 6 * D])
    w_in = ext("w_in", [NL, D, 5144])
    pek = ext("nsa_pe_k", [NL, 32, 64])
    pev = ext("nsa_pe_v", [NL, 32, 64])
    w1k = ext("nsa_cmp_w1_k", [NL, 2048, 128])
    w2k = ext("nsa_cmp_w2_k", [NL, 128, 64])
    w1v = ext("nsa_cmp_w1_v", [NL, 2048, 128])
    w2v = ext("nsa_cmp_w2_v", [NL, 128, 64])
    sinks = ext("sinks", [NL, 8])
    mixg = ext("mix_norm_g", [NL, D])
    w_out = ext("w_out", [NL, D, D])
    wg = ext("exp_w_gate", [NL, 16, D, 512])
    wu = ext("exp_w_up", [NL, 16, D, 512])
    wd = ext("exp_w_down", [NL, 16, 512, D])
    final_g = ext("final_g", [D])
    y = nc.dram_tensor("y", [NT, D], F32, kind="ExternalOutput").ap()
    modD = internal("modD", [NL, NB, 6 * D])
    xs = internal("xs", [NT, D])
    hT = internal("hT", [D, NT], BF16)
    FT = internal("FT", [FT_COLS, NT], BF16)
    TM = internal("TM", [NT, TM_COLS], BF16)
    OD = internal("OD", [NT, D])
    yacc = internal("yacc", [NT, D])
    EBD = internal("EBD", [40, 128, W_STRIP], BF16)
    cx = setup_ctx(p, consts)
    cx.r_modD = p.region("modD")
    r_x, r_hT, r_FT, r_TM, r_OD, r_y, r_out = (p.region() for _ in range(7))
    stage_mod(p, cx, c, ada_w, ada_b, modD, NL, NB)
    stage_eb(p, cx, strips, cst, EBD, p.region())
    for l in range(NL):
        x_src = x if l == 0 else xs
        for b in range(NB):
            stage_norm_T(p, cx, x_src, b * S, S, norm1_g[l:l + 1, :], modD[l, b:b + 1, D:2 * D], modD[l, b:b + 1, 0:D], hT, b * S, "A", r_x, r_hT)
        if 'proj' not in SKIP:
            stage_proj(p, cx, w_in[l], hT, FT, TM, NT, r_hT, r_FT, r_TM)
        for b in range(NB if 'attn' not in SKIP else 0):
            stage_attn(p, cx, cst, FT, TM, OD, S, b, EBD, sinks[l], (w1k[l], w2k[l], w1v[l], w2v[l], pek[l], pev[l]), r_FT, r_TM, r_OD)
        if 'mix' not in SKIP:
          stage_mix_out(p, cx, OD, x_src, xs, mixg[l:l + 1, :], w_out[l], modD, l, NB, S, r_OD, r_x)
        for b in range(NB):
            stage_norm_T(p, cx, xs, b * S, S, norm2_g[l:l + 1, :], modD[l, b:b + 1, 4 * D:5 * D], modD[l, b:b + 1, 3 * D:4 * D], hT, b * S, "N", r_x, r_hT)
        if 'moe' not in SKIP:
          stage_moe(p, cx, hT, NT, router_w, router_bias, wg[l], wu[l], wd[l], yacc, r_hT, r_y)
        last = (l == NL - 1)
        stage_resid(p, cx, xs, yacc, modD, l, 5 * D, NB, S, r_x, r_y, final_g=(final_g if last else None), out=y, r_out=r_out)
    p.finish()
    return nc, p


N_CORES = 4
SKIP = set()
MOE_DIAG = 0
ATT_DIAG = 0
BIAS_ON_PE = True
PV_DELAY = 2
MOD_Q2 = False


def kernel(**inputs):
    import ml_dtypes
    B, S, _ = inputs["x"].shape
    NL = inputs["ada_w"].shape[0]
    NB = B // N_CORES
    nc, prog = build_program(NB, S, NL)
    f32 = lambda a: np.ascontiguousarray(np.asarray(a), dtype=np.float32)
    cst, strips = make_consts(S, f32(inputs["rel_bias"]))
    shared = {"consts": np.eye(128, dtype=np.float32), "strips": strips, **cst}
    for k in ("router_w", "router_bias", "norm1_g", "norm2_g", "ada_w", "ada_b", "w_in", "nsa_pe_k", "nsa_pe_v", "nsa_cmp_w1_k", "nsa_cmp_w2_k",
              "nsa_cmp_w1_v", "nsa_cmp_w2_v", "sinks", "w_out", "exp_w_gate", "exp_w_up", "exp_w_down", "final_g"):
        shared[k] = f32(inputs[k])
    shared["mix_norm_g"] = f32(inputs["mix_norm_g"]).reshape(NL, D)
    xin = f32(inputs["x"])
    cin = f32(inputs["c"])
    in_maps = []
    for ci in range(N_CORES):
        m = dict(shared)
        m["x"] = xin[ci * NB:(ci + 1) * NB].reshape(NB * S, D)
        m["c"] = cin[ci * NB:(ci + 1) * NB]
        in_maps.append(m)
    res = run_bass_kernel_spmd(nc, in_maps, core_ids=list(range(N_CORES)))
    out = np.concatenate([res.results[ci]["y"].reshape(NB, S, D) for ci in range(N_CORES)], axis=0)
    return out.astype(np.float32)
```
